# Optimizing a Trainium2 kernel written in Bass

```python
import jax
import jax.numpy as jnp
from jax import lax
import numpy as np

D_MODEL = 1024
BATCH = 16
SEQ = 2048
DEPTH = 1

GRID_W = 64
CTX_LEN = 256
EPS = 1e-6
N_MOD = 6

MLA_V = 128
MLA_HEADS = D_MODEL // MLA_V
MLA_NOPE = 128
MLA_ROPE = 64
MLA_QK = MLA_NOPE + MLA_ROPE
MLA_Q_RANK = 3 * D_MODEL // 4
MLA_KV_RANK = D_MODEL // 4
MLA_WIDTH = MLA_HEADS * MLA_V
ROPE_BASE = 10000.0
Q_BLOCK = 128

HG_DK = 128
HG_HEADS = D_MODEL // HG_DK
HG_DV = D_MODEL // HG_HEADS
HG_FDIM = HG_HEADS * HG_DK
HG_WIDTH = HG_HEADS * HG_DV
HG_CHUNK = 64

N_EXPERTS = 32
TOP_K = 4
D_EXPERT = D_MODEL
SWIGLU_LIMIT = 7.0
SWIGLU_ALPHA = 1.702
MOE_BLOCK = 128

IN_SPLITS = (MLA_Q_RANK, MLA_KV_RANK + MLA_ROPE, HG_FDIM, HG_FDIM, HG_FDIM, HG_WIDTH, HG_WIDTH, D_MODEL, D_MODEL)
D_IN = MLA_Q_RANK + MLA_KV_RANK + MLA_ROPE + 3 * HG_FDIM + 2 * HG_WIDTH + 2 * D_MODEL

kernel_name = 'hybrid_mla_hgrn2_moe_diffusion_block'


def rmsnorm(x, g):
    xf = x.astype(jnp.float32)
    y = xf * lax.rsqrt(jnp.mean(xf * xf, axis=-1, keepdims=True) + EPS)
    return (y * g.astype(jnp.float32)).astype(x.dtype)


def split_cols(p):
    return jnp.split(p, np.cumsum(IN_SPLITS)[:-1].tolist(), axis=-1)


def axial_rope_tables(rows):
    row = jnp.repeat(jnp.arange(rows), GRID_W).astype(jnp.float32)
    col = jnp.tile(jnp.arange(GRID_W), rows).astype(jnp.float32)
    n_freq = MLA_ROPE // 4
    inv = ROPE_BASE ** (-jnp.arange(n_freq, dtype=jnp.float32) / n_freq)
    ang = jnp.concatenate([row[:, None] * inv, col[:, None] * inv], axis=-1)
    return jnp.cos(ang), jnp.sin(ang)


def apply_rope(x, cos, sin):
    half = MLA_ROPE // 2
    x1 = x[..., :half].astype(jnp.float32)
    x2 = x[..., half:].astype(jnp.float32)
    return jnp.concatenate([x1 * cos - x2 * sin, x1 * sin + x2 * cos], axis=-1).astype(x.dtype)


def mla_queries(q_in, q_norm_g, w_q_b, cos, sin):
    b, n, _ = q_in.shape
    q = (rmsnorm(q_in, q_norm_g) @ w_q_b).reshape(b, n, MLA_HEADS, MLA_QK)
    q_nope, q_rope = q[..., :MLA_NOPE], q[..., MLA_NOPE:]
    if cos is not None:
        q_rope = apply_rope(q_rope, cos[:, None, :], sin[:, None, :])
    return jnp.concatenate([q_nope, q_rope], axis=-1)


def mla_keys_values(kv_in, kv_norm_g, w_kv_b, cos, sin):
    b, n, _ = kv_in.shape
    c_kv, k_rope = kv_in[..., :MLA_KV_RANK], kv_in[..., MLA_KV_RANK:]
    kv = (rmsnorm(c_kv, kv_norm_g) @ w_kv_b).reshape(b, n, MLA_HEADS, MLA_NOPE + MLA_V)
    k_nope, v = kv[..., :MLA_NOPE], kv[..., MLA_NOPE:]
    if cos is not None:
        k_rope = apply_rope(k_rope, cos, sin)
    k_rope = jnp.broadcast_to(k_rope[:, :, None, :], (b, n, MLA_HEADS, MLA_ROPE))
    return jnp.concatenate([k_nope, k_rope], axis=-1), v


def block_attention(q, k, v):
    b, n, h, dk = q.shape
    nb = n // Q_BLOCK
    scale = dk ** -0.5
    q_blocks = q.reshape(b, nb, Q_BLOCK, h, dk).transpose(1, 0, 2, 3, 4)

    def one_block(qb):
        s = jnp.einsum('bqhd,bkhd->bhqk', qb, k, preferred_element_type=jnp.float32) * scale
        p = jax.nn.softmax(s, axis=-1).astype(v.dtype)
        return jnp.einsum('bhqk,bkhd->bqhd', p, v)

    out = lax.map(one_block, q_blocks)
    return out.transpose(1, 0, 2, 3, 4).reshape(b, n, h * v.shape[-1])


def hgrn2_chunk_scan(q, k, v, log_f, s0):
    b, n, h, dk = q.shape
    dv = v.shape[-1]
    nc = n // HG_CHUNK

    def chunks(a):
        return a.reshape(b, nc, HG_CHUNK, h, a.shape[-1]).transpose(1, 0, 3, 2, 4)

    lower_tri = jnp.tril(jnp.ones((HG_CHUNK, HG_CHUNK), dtype=bool))[:, :, None]

    def step(state, inp):
        qc, kc, vc, gc = inp
        cum = jnp.cumsum(gc, axis=2)
        o_inter = jnp.einsum('bhtk,bhkv->bhtv', qc * jnp.exp(cum), state)
        rel = cum[:, :, :, None, :] - cum[:, :, None, :, :]
        decay = jnp.where(lower_tri, jnp.exp(jnp.minimum(rel, 0.0)), 0.0)
        att = jnp.einsum('bhtk,bhtsk,bhsk->bhts', qc, decay, kc)
        o_intra = jnp.einsum('bhts,bhsv->bhtv', att, vc)
        last = cum[:, :, -1:, :]
        state = jnp.exp(last[:, :, 0, :])[..., None] * state + jnp.einsum('bhsk,bhsv->bhkv', kc * jnp.exp(last - cum), vc)
        return state, o_inter + o_intra

    s_final, o = lax.scan(step, s0, (chunks(q), chunks(k), chunks(v), chunks(log_f)))
    return o.transpose(1, 0, 3, 2, 4).reshape(b, n, h, dv), s_final


def hgrn2_gates(z, lb):
    b, n = z.shape[:2]
    f = lb + (1.0 - lb) * jax.nn.sigmoid(z.astype(jnp.float32))
    return (1.0 - f).reshape(b, n, HG_HEADS, HG_DK), jnp.log(f).reshape(b, n, HG_HEADS, HG_DK)


def hgrn2_qv(p):
    b, n = p[2].shape[:2]
    q = (jax.nn.silu(p[2].astype(jnp.float32)) * HG_DK ** -0.5).reshape(b, n, HG_HEADS, HG_DK)
    v = p[5].astype(jnp.float32).reshape(b, n, HG_HEADS, HG_DV)
    return q, v


def hgrn2_readout(o, g, hg_norm_g):
    b, n = g.shape[:2]
    gate = jax.nn.silu(g.astype(jnp.float32)).reshape(b, n, HG_HEADS, HG_DV)
    return (rmsnorm(o, hg_norm_g) * gate).reshape(b, n, HG_WIDTH).astype(g.dtype)


def merge_branches(p, y_mla, y_hg, w_out):
    g_mla = jax.nn.sigmoid(p[7].astype(jnp.float32))
    g_hg = jax.nn.sigmoid(p[8].astype(jnp.float32))
    y = g_mla * y_mla.astype(jnp.float32) + g_hg * y_hg.astype(jnp.float32)
    return y.astype(p[7].dtype) @ w_out


def token_mixer(h_lat, h_ctx, cos, sin, lb_fwd, lb_bwd, w_in, q_norm_g, w_q_b, kv_norm_g, w_kv_b, hg_norm_g, w_out, with_ctx_out):
    lat = split_cols(h_lat @ w_in)
    ctx = split_cols(h_ctx @ w_in)
    k_lat, v_lat = mla_keys_values(lat[1], kv_norm_g, w_kv_b, cos, sin)
    k_ctx, v_ctx = mla_keys_values(ctx[1], kv_norm_g, w_kv_b, None, None)
    q_lat = mla_queries(lat[0], q_norm_g, w_q_b, cos, sin)
    y_mla_lat = block_attention(q_lat, jnp.concatenate([k_lat, k_ctx], axis=1), jnp.concatenate([v_lat, v_ctx], axis=1))
    q_l, v_l = hgrn2_qv(lat)
    q_c, v_c = hgrn2_qv(ctx)
    kf_l, gf_l = hgrn2_gates(lat[3], lb_fwd)
    kb_l, gb_l = hgrn2_gates(lat[4], lb_bwd)
    kf_c, gf_c = hgrn2_gates(ctx[3], lb_fwd)
    kb_c, gb_c = hgrn2_gates(ctx[4], lb_bwd)
    b = h_lat.shape[0]
    s0 = jnp.zeros((b, HG_HEADS, HG_DK, HG_DV), jnp.float32)
    flip = lambda a: jnp.flip(a, axis=1)
    o_cf, s_cf = hgrn2_chunk_scan(q_c, kf_c, v_c, gf_c, s0)
    o_cb, s_cb = hgrn2_chunk_scan(flip(q_c), flip(kb_c), flip(v_c), flip(gb_c), s0)
    o_lf, _ = hgrn2_chunk_scan(q_l, kf_l, v_l, gf_l, s_cf)
    o_lb, _ = hgrn2_chunk_scan(flip(q_l), flip(kb_l), flip(v_l), flip(gb_l), s_cb)
    y_hg_lat = hgrn2_readout(o_lf + flip(o_lb), lat[6], hg_norm_g)
    out_lat = merge_branches(lat, y_mla_lat, y_hg_lat, w_out)
    if not with_ctx_out:
        return out_lat, None
    q_ctx = mla_queries(ctx[0], q_norm_g, w_q_b, None, None)
    y_mla_ctx = block_attention(q_ctx, k_ctx, v_ctx)
    y_hg_ctx = hgrn2_readout(o_cf + flip(o_cb), ctx[6], hg_norm_g)
    return out_lat, merge_branches(ctx, y_mla_ctx, y_hg_ctx, w_out)


def moe_ffn(h, w_router, b_router, w_gate_up, b_gate_up, w_down, b_down):
    shp = h.shape
    t = h.reshape(-1, shp[-1])
    n_tok = t.shape[0]
    n_pair = n_tok * TOP_K
    logits = (t @ w_router + b_router).astype(jnp.float32)
    top_val, top_idx = lax.top_k(logits, TOP_K)
    probs = jax.nn.softmax(top_val, axis=-1)
    flat_e = top_idx.reshape(-1)
    flat_tok = jnp.arange(n_pair, dtype=jnp.int32) // TOP_K
    order = jnp.argsort(flat_e)
    sorted_e = flat_e[order]
    counts = jnp.bincount(flat_e, length=N_EXPERTS)
    padded = (counts + MOE_BLOCK - 1) // MOE_BLOCK * MOE_BLOCK
    start = jnp.cumsum(counts) - counts
    pad_end = jnp.cumsum(padded)
    pad_start = pad_end - padded
    dest_sorted = pad_start[sorted_e] + jnp.arange(n_pair, dtype=jnp.int32) - start[sorted_e]
    dest = jnp.zeros((n_pair,), jnp.int32).at[order].set(dest_sorted.astype(jnp.int32))
    n_rows = n_pair + N_EXPERTS * MOE_BLOCK
    n_blocks = n_rows // MOE_BLOCK
    row_tok = jnp.full((n_rows,), n_tok, jnp.int32).at[dest].set(flat_tok)
    t_ext = jnp.concatenate([t, jnp.zeros((1, t.shape[-1]), t.dtype)], axis=0)
    x_rows = t_ext[row_tok].reshape(n_blocks, MOE_BLOCK, -1)
    block_start = jnp.arange(n_blocks, dtype=jnp.int32) * MOE_BLOCK
    block_e = jnp.minimum(jnp.searchsorted(pad_end, block_start, side='right'), N_EXPERTS - 1)

    def expert_block(args):
        xb, e = args
        gu = xb @ w_gate_up[e] + b_gate_up[e]
        x_glu = jnp.minimum(gu[:, 0::2], SWIGLU_LIMIT)
        x_lin = jnp.clip(gu[:, 1::2], -SWIGLU_LIMIT, SWIGLU_LIMIT)
        act = x_glu * jax.nn.sigmoid(SWIGLU_ALPHA * x_glu) * (x_lin + 1)
        return act @ w_down[e] + b_down[e]

    y_rows = lax.map(expert_block, (x_rows, block_e)).reshape(n_rows, -1)
    y_pairs = y_rows[dest].reshape(n_tok, TOP_K, -1)
    out = jnp.einsum('tk,tkd->td', probs.astype(y_pairs.dtype), y_pairs)
    return out.reshape(shp)


def setup_inputs(seed: int = 0) -> dict:
    key = jax.random.key(seed)
    ks = jax.random.split(key, 24)
    f32 = jnp.float32
    nrm = lambda k, shape, scale: jax.random.normal(k, shape, f32) * scale
    gain = lambda k, shape: 1.0 + 0.02 * jax.random.normal(k, shape, f32)
    return {
        'x': nrm(ks[0], (BATCH, SEQ, D_MODEL), 1.0),
        'c': nrm(ks[1], (BATCH, D_MODEL), 1.0),
        'ctx': nrm(ks[2], (BATCH, CTX_LEN, D_MODEL), 1.0),
        'c_ctx': nrm(ks[3], (D_MODEL,), 1.0),
        'w_mod': nrm(ks[4], (DEPTH, D_MODEL, N_MOD * D_MODEL), 0.5 * D_MODEL ** -0.5),
        'b_mod': nrm(ks[5], (DEPTH, N_MOD * D_MODEL), 0.02),
        'norm_mix_g': gain(ks[6], (DEPTH, D_MODEL)),
        'w_in': nrm(ks[7], (DEPTH, D_MODEL, D_IN), D_MODEL ** -0.5),
        'mla_q_norm_g': gain(ks[8], (DEPTH, MLA_Q_RANK)),
        'w_q_b': nrm(ks[9], (DEPTH, MLA_Q_RANK, MLA_HEADS * MLA_QK), MLA_Q_RANK ** -0.5),
        'mla_kv_norm_g': gain(ks[10], (DEPTH, MLA_KV_RANK)),
        'w_kv_b': nrm(ks[11], (DEPTH, MLA_KV_RANK, MLA_HEADS * (MLA_NOPE + MLA_V)), MLA_KV_RANK ** -0.5),
        'hg_lb_logits': nrm(ks[12], (2, DEPTH + 1, HG_FDIM), 1.0),
        'hg_norm_g': gain(ks[13], (DEPTH, HG_DV)),
        'w_out': nrm(ks[14], (DEPTH, D_MODEL, D_MODEL), D_MODEL ** -0.5),
        'norm_ffn_g': gain(ks[15], (DEPTH, D_MODEL)),
        'w_router': nrm(ks[16], (DEPTH, D_MODEL, N_EXPERTS), D_MODEL ** -0.5),
        'b_router': nrm(ks[17], (DEPTH, N_EXPERTS), 0.01),
        'w_gate_up': nrm(ks[18], (DEPTH, N_EXPERTS, D_MODEL, 2 * D_EXPERT), D_MODEL ** -0.5),
        'b_gate_up': nrm(ks[19], (DEPTH, N_EXPERTS, 2 * D_EXPERT), 0.01),
        'w_down': nrm(ks[20], (DEPTH, N_EXPERTS, D_EXPERT, D_MODEL), D_EXPERT ** -0.5),
        'b_down': nrm(ks[21], (DEPTH, N_EXPERTS, D_MODEL), 0.01),
        'final_norm_g': gain(ks[22], (D_MODEL,)),
    }


def reference(x, c, ctx, c_ctx, w_mod, b_mod, norm_mix_g, w_in, mla_q_norm_g, w_q_b, mla_kv_norm_g, w_kv_b, hg_lb_logits, hg_norm_g, w_out, norm_ffn_g, w_router, b_router, w_gate_up, b_gate_up, w_down, b_down, final_norm_g):
    rows = x.shape[1] // GRID_W
    cos, sin = axial_rope_tables(rows)
    lb = jnp.cumsum(jax.nn.softmax(hg_lb_logits.astype(jnp.float32), axis=1), axis=1)
    silu_c = jax.nn.silu(c)
    silu_cc = jax.nn.silu(c_ctx)
    n_ctx = ctx.shape[1]
    for l in range(DEPTH):
        last = l == DEPTH - 1
        mod_lat = jnp.split((silu_c @ w_mod[l] + b_mod[l])[:, None, :], N_MOD, axis=-1)
        mod_ctx = jnp.split((silu_cc @ w_mod[l] + b_mod[l])[None, None, :], N_MOD, axis=-1)
        h_lat = rmsnorm(x, norm_mix_g[l]) * (1 + mod_lat[1]) + mod_lat[0]
        h_ctx = rmsnorm(ctx, norm_mix_g[l]) * (1 + mod_ctx[1]) + mod_ctx[0]
        mix_lat, mix_ctx = token_mixer(h_lat, h_ctx, cos, sin, lb[0, l], lb[1, l], w_in[l], mla_q_norm_g[l], w_q_b[l], mla_kv_norm_g[l], w_kv_b[l], hg_norm_g[l], w_out[l], not last)
        x = x + mod_lat[2] * mix_lat
        h_lat = rmsnorm(x, norm_ffn_g[l]) * (1 + mod_lat[4]) + mod_lat[3]
        if last:
            x = x + mod_lat[5] * moe_ffn(h_lat, w_router[l], b_router[l], w_gate_up[l], b_gate_up[l], w_down[l], b_down[l])
        else:
            ctx = ctx + mod_ctx[2] * mix_ctx
            h_ctx = rmsnorm(ctx, norm_ffn_g[l]) * (1 + mod_ctx[4]) + mod_ctx[3]
            y = moe_ffn(jnp.concatenate([h_ctx, h_lat], axis=1), w_router[l], b_router[l], w_gate_up[l], b_gate_up[l], w_down[l], b_down[l])
            ctx = ctx + mod_ctx[5] * y[:, :n_ctx]
            x = x + mod_lat[5] * y[:, n_ctx:]
    return rmsnorm(x, final_norm_g)
```

```python
from contextlib import ExitStack
import numpy as np
import ml_dtypes
import concourse.bass as bass
import concourse.mybir as mybir
from concourse.bass_utils import run_bass_kernel_spmd

F32 = mybir.dt.float32
BF16 = mybir.dt.bfloat16
I32 = mybir.dt.int32
U32 = mybir.dt.uint32
AF = mybir.ActivationFunctionType
ALU = mybir.AluOpType
AX = mybir.AxisListType

NCORES = 8
BL = 2
NLAT = 2048
NCTX = 256
NTOK = NLAT + NCTX
D = 1024
DIN = 8256
EPS = 1e-6
NEXP = 32
HC = 32
NCH = 128 // HC
CAP = 4096
SLAB = 256
NB = SLAB // 128
NSL = BL * NLAT * 4 // SLAB + NEXP


class T:
    __slots__ = ("name", "w", "r")

    def __init__(self, name=""):
        self.name = name
        self.w = None
        self.r = {}


class Sched:
    def __init__(self, nc, n_dma_sems=8):
        self.nc = nc
        self.eng = {}
        for nm, h in (("pe", nc.tensor), ("act", nc.scalar), ("dve", nc.vector), ("pool", nc.gpsimd), ("sp", nc.sync)):
            sem = nc.alloc_semaphore(f"s_{nm}")
            self.eng[nm] = dict(h=h, sem=sem, cnt=0, seen={}, name=nm)
        self.dq = {}
        for nm in ("sp", "pool", "act"):
            sems = [nc.alloc_semaphore(f"d_{nm}{i}") for i in range(n_dma_sems)]
            self.dq[nm] = dict(sems=sems, tgt=[0] * n_dma_sems, i=0)
        self.nwaits = 0
        self.nops = 0

    def _wait(self, E, deps, skip_same=False):
        best = {}
        for d in deps:
            if d is None:
                continue
            sem, cnt = d
            if skip_same and sem is E["sem"]:
                continue
            k = id(sem)
            if k not in best or best[k][1] < cnt:
                best[k] = (sem, cnt)
        for k, (sem, cnt) in best.items():
            if E["seen"].get(k, 0) >= cnt:
                continue
            E["h"].wait_ge(sem, cnt)
            E["seen"][k] = cnt
            self.nwaits += 1

    @staticmethod
    def _deps(reads, writes):
        deps = []
        for t in reads:
            deps.append(t.w)
        for t in writes:
            deps.append(t.w)
            deps.extend(t.r.values())
        return deps

    @staticmethod
    def _commit(tok, reads, writes):
        for t in writes:
            t.w = tok
            t.r = {}
        for t in reads:
            if t.w is not tok:
                t.r[id(tok[0])] = tok

    def op(self, e, fn, reads=(), writes=()):
        E = self.eng[e]
        self._wait(E, self._deps(reads, writes), skip_same=(e == "pe"))
        ins = fn(E["h"])
        E["cnt"] += 1
        ins.then_inc(E["sem"], 1)
        self.nops += 1
        self._commit((E["sem"], E["cnt"]), reads, writes)
        return ins

    def dma(self, q, out, in_, reads=(), writes=(), fn=None, **kw):
        E = self.eng[q]
        Q = self.dq[q]
        i = Q["i"] % len(Q["sems"])
        Q["i"] += 1
        sem = Q["sems"][i]
        deps = self._deps(reads, writes)
        if Q["tgt"][i] > 0:
            deps.append((sem, Q["tgt"][i]))
        self._wait(E, deps)
        if fn is not None:
            ins = fn(E["h"])
        else:
            ins = E["h"].dma_start(out=out, in_=in_, **kw)
        Q["tgt"][i] += 16
        ins.then_inc(sem, 16)
        self.nops += 1
        self._commit((sem, Q["tgt"][i]), reads, writes)
        return ins

    def barrier(self):
        toks = [(E["sem"], E["cnt"]) for E in self.eng.values() if E["cnt"] > 0]
        for Q in self.dq.values():
            for s, t in zip(Q["sems"], Q["tgt"]):
                if t > 0:
                    toks.append((s, t))
        for E in self.eng.values():
            self._wait(E, toks)

    def finish(self, tiles):
        self._wait(self.eng["sp"], [t.w for t in tiles])


class Buf:
    def __init__(self, h, name):
        self.h = h
        self.t = T(name)

    def ap(self):
        return self.h.ap()


def host_consts():
    c = {}
    c["ident_f"] = np.eye(128, dtype=np.float32)
    rows = NLAT // 64
    row = np.repeat(np.arange(rows), 64).astype(np.float32)
    col = np.tile(np.arange(64), rows).astype(np.float32)
    inv = (np.float32(10000.0) ** (-np.arange(16, dtype=np.float32) / np.float32(16))).astype(np.float32)
    ang = np.concatenate([row[:, None] * inv, col[:, None] * inv], axis=-1).astype(np.float32)
    cos = np.cos(ang).astype(np.float32).T
    sin = np.sin(ang).astype(np.float32).T
    c["rope_cc"] = np.ascontiguousarray(np.concatenate([cos, cos], 0))
    c["rope_ss"] = np.ascontiguousarray(np.concatenate([sin, sin], 0))
    s = np.arange(128)[:, None]
    t = np.arange(128)[None, :]
    same = (s // HC) == (t // HC)
    tri = np.stack([
        same & (s <= t),
        same & (s > t),
        same & (s >= t),
        same & (s < t),
    ]).astype(np.float32)
    c["tri"] = np.ascontiguousarray(tri.transpose(1, 0, 2))
    ind = np.zeros((128, NCH), np.float32)
    for cix in range(NCH):
        ind[cix * HC:(cix + 1) * HC, cix] = 1
    c["chunk_ind"] = ind
    c["ustrict"] = (s < t).astype(np.float32)
    c["iota_e"] = np.tile(np.arange(NEXP, dtype=np.float32)[None, :], (128, 1))
    c["iota_cap"] = np.ascontiguousarray(c["iota_e"] * CAP)
    c["jota"] = np.tile(np.arange(NSL, dtype=np.float32)[None, :], (128, 1))
    c["pidx"] = np.tile(np.arange(128, dtype=np.float32)[:, None], (1, NSL))
    c["off4"] = (np.arange(128, dtype=np.float32)[:, None] + 128.0 * np.arange(NB, dtype=np.float32)[None, :])
    return c


C_QA, C_KV, C_KR, C_HQ, C_FF, C_FB, C_HI, C_HG, C_MM, C_MH = 0, 768, 1024, 1088, 2112, 3136, 4160, 5184, 6208, 7232


class Prog:
    def __init__(self, stop_after=None, dbg=False):
        self.stop_after = stop_after
        self.dbg = dbg
        nc = self.nc = bass.Bass("TRN2", target_bir_lowering=False)
        self.S = Sched(nc)
        self.inp = {}
        self.dbg_outs = {}
        self.psn = 0
        self.scopes = []

    def din(self, name, shape, dt=F32):
        ap = self.nc.dram_tensor(name, list(shape), dt, kind="ExternalInput").ap()
        self.inp[name] = ap
        return ap

    def dscr(self, name, shape, dt=F32):
        return self.nc.dram_tensor(name, list(shape), dt, kind="Internal").ap()

    def dout(self, name, shape, dt=F32):
        return self.nc.dram_tensor(name, list(shape), dt, kind="ExternalOutput").ap()

    def sb(self, name, shape, dt=F32):
        self.uid = getattr(self, "uid", 0) + 1
        nm = f"s_{name}_{self.uid}"
        if self.scopes:
            h = self.scopes[-1].enter_context(self.nc.sbuf_tensor(nm, list(shape), dt))
        else:
            h = self.nc.alloc_sbuf_tensor(nm, list(shape), dt)
        return Buf(h, name)

    def open_scope(self):
        self.scopes.append(ExitStack())

    def close_scope(self):
        self.S.barrier()
        self.scopes.pop().close()

    def dump(self, name, buf, shape, dt=F32):
        if not self.dbg:
            return
        o = self.dout("dbg_" + name, shape, dt)
        t = T("dbg_" + name)
        self.S.dma("sp", o, buf.ap(), reads=[buf.t], writes=[t])
        self.dbg_outs[name] = t

    def build(self):
        nc, S = self.nc, self.S
        op, dma = S.op, S.dma
        x_d = self.din("x", [BL * NLAT, D])
        ctx_d = self.din("ctx", [BL * NCTX, D])
        cvec_d = self.din("cvec", [128, 8, 3])
        wmod_d = self.din("w_mod", [D, 6 * D])
        bmodT_d = self.din("b_modT", [128, 48])
        gmixT_d = self.din("g_mixT", [128, 8])
        win_d = self.din("w_in", [D, DIN])
        gqT_d = self.din("g_qT", [128, 6])
        wqb_d = self.din("w_q_b", [768, 1536])
        gkvT_d = self.din("g_kvT", [128, 2])
        wkvb_d = self.din("w_kv_b", [256, 2048])
        lbl_d = self.din("lb_logits", [2, 2, D])
        ghgT_d = self.din("g_hgT", [128, 1])
        wout_d = self.din("w_out", [D, D])
        gffn_d = self.din("g_ffn", [1, D])
        wr_d = self.din("w_router", [D, NEXP])
        br_d = self.din("b_router", [1, NEXP])
        wgu_d = self.din("w_gate_up", [NEXP, D, 2 * D])
        bguT_d = self.din("b_guT", [NEXP, 128, 2, 8])
        bgur_d = self.din("b_gu_rows", [NEXP, 2 * D])
        wd_d = self.din("w_down", [NEXP, D, D])
        bd_d = self.din("b_down", [NEXP, D])
        gfin_d = self.din("g_fin", [1, D])
        identf_d = self.din("ident_f", [128, 128])
        cc_d = self.din("rope_cc", [64, NLAT])
        ss_d = self.din("rope_ss", [64, NLAT])
        tri_d = self.din("tri", [128, 4, 128])
        cind_d = self.din("chunk_ind", [128, NCH])
        ustr_d = self.din("ustrict", [128, 128])
        iotae_d = self.din("iota_e", [128, NEXP])
        iotac_d = self.din("iota_cap", [128, NEXP])
        jota_d = self.din("jota", [128, NSL])
        pidx_d = self.din("pidx", [128, NSL])
        off4_d = self.din("off4", [128, NB])
        out_d = self.dout("out", [BL * NLAT, D])
        self.t_out = T("out")
        modD = self.dscr("modD", [3, 6 * D])
        ymlaD = self.dscr("ymlaD", [BL, 128, 8, NLAT], BF16)
        self.t_ymlaD = T("ymlaD")
        yhD = self.dscr("yhD", [BL, 128, 8, NLAT], BF16)
        self.t_yhD = T("yhD")
        x1D = self.dscr("x1D", [BL * NLAT, D])
        self.t_x1D = T("x1D")
        XeD = self.dscr("XeD", [NEXP * CAP, D], BF16)
        self.t_XeD = T("XeD")
        YeD = self.dscr("YeD", [NSL * SLAB, D])
        self.t_YeD = T("YeD")
        self.tl_Xe, self.tl_Ye, self.tl_out = [], [], []
        self.tl_x1 = [T(f"x1D{i}") for i in range(32)]

        ident_f = self.sb("ident_f_sb", [128, 128])
        ident_b = self.sb("ident_b", [128, 128], BF16)
        ones_f = self.sb("ones_f", [128, 128])
        ones_b = self.sb("ones_b", [128, 128], BF16)
        dma("sp", ident_f.ap(), identf_d, writes=[ident_f.t])
        op("dve", lambda e: e.tensor_copy(out=ident_b.ap(), in_=ident_f.ap()), reads=[ident_f.t], writes=[ident_b.t])
        op("pool", lambda e: e.memset(ones_f.ap(), 1.0), writes=[ones_f.t])
        op("pool", lambda e: e.memset(ones_b.ap(), 1.0), writes=[ones_b.t])
        self.ident_f, self.ident_b, self.ones_f, self.ones_b = ident_f, ident_b, ones_f, ones_b
        self.ps = [Buf(nc.alloc_psum_tensor(f"ps{i}", [128, 512], F32), f"ps{i}") for i in range(8)]

        bmodT = self.sb("bmodT", [128, 48])
        modT = self.sb("modT", [128, 48, 3])
        gmixT = self.sb("gmixT", [128, 8])
        G1 = self.sb("G1", [128, 8, 3])
        self.open_scope()
        zt = self.sb("zeros", [128, 2048], BF16)
        op("pool", lambda e: e.memset(zt.ap(), 0.0), writes=[zt.t])
        zsem = nc.alloc_semaphore("zfill")
        Xv = XeD.rearrange("(a p j) d -> a p (j d)", p=128, j=2)
        S._wait(S.eng["act"], [zt.t.w])
        NZ = SLAB // 256
        for a in range(NZ):
            nc.scalar.dma_start(out=Xv[a], in_=zt.ap()).then_inc(zsem, 16)
        self.t_XeD.w = (zsem, 16 * NZ)
        cvec = self.sb("cvec", [128, 8, 3])
        sig = self.sb("csig", [128, 8, 3])
        scb = self.sb("scb", [128, 8, 3], BF16)
        dma("sp", cvec.ap(), cvec_d, writes=[cvec.t])
        op("act", lambda e: e.activation(out=sig.ap(), in_=cvec.ap(), func=AF.Sigmoid), reads=[cvec.t], writes=[sig.t])
        op("dve", lambda e: e.tensor_mul(out=scb.ap(), in0=cvec.ap(), in1=sig.ap()), reads=[cvec.t, sig.t], writes=[scb.t])
        dma("sp", bmodT.ap(), bmodT_d, writes=[bmodT.t])
        wm = [self.sb(f"wmod{i}", [128, 8, 1536], BF16) for i in range(2)]
        pm = self.ps[0]
        wmod_v = wmod_d.rearrange("(k p) c -> p k c", p=128)
        for pc in range(4):
            w = wm[pc % 2]
            dma("pool", w.ap(), wmod_v[:, :, pc * 1536:(pc + 1) * 1536], writes=[w.t])
            for jj in range(12):
                j = pc * 12 + jj
                for k in range(8):
                    op("pe", lambda e: e.matmul(pm.ap()[:, j * 3:(j + 1) * 3], lhsT=w.ap()[:, k, jj * 128:(jj + 1) * 128],
                                                rhs=scb.ap()[:, k, :], start=(k == 0), stop=(k == 7)),
                       reads=[w.t, scb.t], writes=[pm.t])
        op("dve", lambda e: e.tensor_tensor(out=modT.ap(), in0=pm.ap()[:, 0:144].rearrange("p (j v) -> p j v", v=3),
                                            in1=bmodT.ap().unsqueeze(2).to_broadcast([128, 48, 3]), op=ALU.add),
           reads=[pm.t, bmodT.t], writes=[modT.t])
        t_modD = T("modD")
        with nc.allow_non_contiguous_dma(reason="one-time tiny modulation transpose"):
            for v in range(3):
                dma("sp", modD[v, :].rearrange("(j p) -> p j", p=128), modT.ap()[:, :, v], reads=[modT.t], writes=[t_modD])
        self.modD, self.t_modD = modD, t_modD
        dma("sp", gmixT.ap(), gmixT_d, writes=[gmixT.t])
        op("dve", lambda e: e.scalar_tensor_tensor(out=G1.ap(), in0=modT.ap()[:, 8:16, :], scalar=1.0,
                                                   in1=gmixT.ap().unsqueeze(2).to_broadcast([128, 8, 3]), op0=ALU.add, op1=ALU.mult),
           reads=[modT.t, gmixT.t], writes=[G1.t])
        self.dump("modT", modT, [128, 48, 3])
        for E in S.eng.values():
            S._wait(E, [self.t_XeD.w])
        self.close_scope()
        if self.stop_after == "P0":
            return self.finalize()

        gqT = self.sb("gqT", [128, 6]); dma("sp", gqT.ap(), gqT_d, writes=[gqT.t])
        gkvT = self.sb("gkvT", [128, 2]); dma("sp", gkvT.ap(), gkvT_d, writes=[gkvT.t])
        ghgT = self.sb("ghgT", [128, 1]); dma("sp", ghgT.ap(), ghgT_d, writes=[ghgT.t])

        self.base_bc = self.sb("base_bc", [128, NEXP])
        op("pool", lambda e: e.memset(self.base_bc.ap(), 0.0), writes=[self.base_bc.t])
        self.destAll = self.sb("destAll", [128, 32, 4], I32)
        self.probAll = self.sb("probAll", [128, 32, 4])
        self.dstfAll = self.sb("dstfAll", [128, 32, 4])
        self.eidAll = self.sb("eidAll", [128, 32, 4])
        self.ustr = self.sb("ustr", [128, 128], BF16)
        tmpu = self.sb("tmpu", [128, 128])
        dma("sp", tmpu.ap(), ustr_d, writes=[tmpu.t])
        op("dve", lambda e: e.tensor_copy(out=self.ustr.ap(), in_=tmpu.ap()), reads=[tmpu.t], writes=[self.ustr.t])
        self.iota_e = self.sb("iota_e", [128, NEXP]); dma("sp", self.iota_e.ap(), iotae_d, writes=[self.iota_e.t])
        self.iota_c = self.sb("iota_c", [128, NEXP]); dma("sp", self.iota_c.ap(), iotac_d, writes=[self.iota_c.t])
        for b in range(BL):
            self.batch(b, locals())
            if self.stop_after is not None and self.stop_after.startswith("B0"):
                return self.finalize()
        self.moe(locals())
        return self.finalize()

    def finalize(self):
        S = self.S
        tl = list(self.dbg_outs.values())
        tl.extend(self.tl_out)
        S.finish(tl)
        return self.nc


def make_in_maps(inputs):
    f = lambda a: np.ascontiguousarray(np.asarray(a, dtype=np.float32))
    x, c, ctx, c_ctx = f(inputs["x"]), f(inputs["c"]), f(inputs["ctx"]), f(inputs["c_ctx"])
    consts = host_consts()
    featT = lambda v, nk: np.ascontiguousarray(v.reshape(nk, 128).T)
    shared = {
        "w_mod": f(inputs["w_mod"][0]),
        "b_modT": featT(f(inputs["b_mod"][0]), 48),
        "g_mixT": featT(f(inputs["norm_mix_g"][0]), 8),
        "w_in": f(inputs["w_in"][0]),
        "g_qT": featT(f(inputs["mla_q_norm_g"][0]), 6),
        "w_q_b": f(inputs["w_q_b"][0]),
        "g_kvT": featT(f(inputs["mla_kv_norm_g"][0]), 2),
        "w_kv_b": f(inputs["w_kv_b"][0]),
        "lb_logits": f(inputs["hg_lb_logits"]),
        "g_hgT": featT(f(inputs["hg_norm_g"][0]), 1),
        "w_out": f(inputs["w_out"][0]),
        "g_ffn": f(inputs["norm_ffn_g"][0]).reshape(1, D),
        "w_router": f(inputs["w_router"][0]),
        "b_router": f(inputs["b_router"][0]).reshape(1, NEXP),
        "w_gate_up": f(inputs["w_gate_up"][0]),
        "b_guT": np.ascontiguousarray(f(inputs["b_gate_up"][0]).reshape(NEXP, 128, 8, 2).transpose(0, 1, 3, 2)),
        "b_gu_rows": f(inputs["b_gate_up"][0]),
        "w_down": f(inputs["w_down"][0]),
        "b_down": f(inputs["b_down"][0]),
        "g_fin": f(inputs["final_norm_g"]).reshape(1, D),
    }
    shared.update(consts)
    maps = []
    for core in range(NCORES):
        b0 = core * BL
        cv = np.stack([c[b0], c[b0 + 1], c_ctx], axis=-1)
        m = dict(shared)
        m["x"] = np.ascontiguousarray(x[b0:b0 + BL].reshape(BL * NLAT, D))
        m["ctx"] = np.ascontiguousarray(ctx[b0:b0 + BL].reshape(BL * NCTX, D))
        m["cvec"] = np.ascontiguousarray(cv.reshape(8, 128, 3).transpose(1, 0, 2))
        maps.append(m)
    return maps


def kernel(**inputs):
    prog = Prog()
    nc = prog.build()
    in_maps = make_in_maps(inputs)
    in_maps = [{k: v for k, v in m.items() if k in prog.inp} for m in in_maps]
    res = run_bass_kernel_spmd(nc, in_maps, core_ids=list(range(NCORES)))
    outs = [np.asarray(r["out"], dtype=np.float32).reshape(BL, NLAT, D) for r in res.results]
    return np.concatenate(outs, axis=0)


def _batch(self, b, L):
    nc, S = self.nc, self.S
    op, dma = S.op, S.dma
    ps = self.ps
    ident_f, ident_b, ones_f, ones_b = self.ident_f, self.ident_b, self.ones_f, self.ones_b
    G1, modT = L["G1"], L["modT"]
    x_d, ctx_d, win_d = L["x_d"], L["ctx_d"], L["win_d"]
    win_v = win_d.rearrange("(k p) c -> p k c", p=128)
    first = (b == 0)

    def psum(i):
        return ps[i % 8]

    self.open_scope()
    if True:
        self.hT = self.sb("hT", [128, 8, NTOK], BF16)
        self.hT_t = [T(f"hT{i}") for i in range(18)]
        self.open_scope()
        self.xt = [self.sb(f"xt{i}", [128, D]) for i in range(2)]
        self.sq = self.sb("sqscr", [128, D])
        self.st = [self.sb(f"st{i}", [128, 4]) for i in range(2)]
    hT, hT_t, xt, sq, st = self.hT, self.hT_t, self.xt, self.sq, self.st
    for i in range(18):
        xb, sb_ = xt[i % 2], st[i % 2]
        if i < 16:
            src, v = x_d[b * NLAT + i * 128: b * NLAT + (i + 1) * 128, :], b
        else:
            src, v = ctx_d[b * NCTX + (i - 16) * 128: b * NCTX + (i - 15) * 128, :], 2
        dma("sp", xb.ap(), src, writes=[xb.t])
        op("act", lambda e: e.activation(out=sq.ap(), in_=xb.ap(), func=AF.Square, accum_out=sb_.ap()[:, 0:1]),
           reads=[xb.t], writes=[sq.t, sb_.t])
        op("act", lambda e: e.activation(out=sb_.ap()[:, 1:2], in_=sb_.ap()[:, 0:1], func=AF.Ln, scale=1.0 / D, bias=EPS),
           reads=[sb_.t], writes=[sb_.t])
        op("act", lambda e: e.activation(out=sb_.ap()[:, 2:3], in_=sb_.ap()[:, 1:2], func=AF.Exp, scale=-0.5),
           reads=[sb_.t], writes=[sb_.t])
        op("dve", lambda e: e.tensor_scalar(out=xb.ap(), in0=xb.ap(), scalar1=sb_.ap()[:, 2:3], scalar2=None, op0=ALU.mult),
           reads=[sb_.t, xb.t], writes=[xb.t])
        for half in range(2):
            p = psum(i * 2 + half)
            for kk in range(4):
                k = half * 4 + kk
                op("pe", lambda e: e.transpose(out=p.ap()[:, kk * 128:(kk + 1) * 128], in_=xb.ap()[:, k * 128:(k + 1) * 128],
                                               identity=ident_f.ap()), reads=[xb.t, ident_f.t], writes=[p.t])
            for kk in range(4):
                k = half * 4 + kk
                eng = "act" if kk % 2 == 0 else "dve"
                if eng == "act":
                    op("act", lambda e: e.activation(out=hT.ap()[:, k, i * 128:(i + 1) * 128], in_=p.ap()[:, kk * 128:(kk + 1) * 128],
                                                     func=AF.Identity, scale=G1.ap()[:, k, v:v + 1], bias=modT.ap()[:, k, v:v + 1]),
                       reads=[p.t, G1.t, modT.t], writes=[hT_t[i]])
                else:
                    op("dve", lambda e: e.tensor_scalar(out=hT.ap()[:, k, i * 128:(i + 1) * 128], in0=p.ap()[:, kk * 128:(kk + 1) * 128],
                                                        scalar1=G1.ap()[:, k, v:v + 1], scalar2=modT.ap()[:, k, v:v + 1],
                                                        op0=ALU.mult, op1=ALU.add),
                       reads=[p.t, G1.t, modT.t], writes=[hT_t[i]])
    if self.dbg and first:
        allh = T("allh")
        o = self.dout("dbg_hT", [128, 8, NTOK], BF16)
        S.dma("sp", o, hT.ap(), reads=hT_t, writes=[allh])
        self.dbg_outs["hT"] = allh
    if self.stop_after == "B0P1":
        return
    self.close_scope()

    self.open_scope()
    if True:
        self.wA = self.sb("wA", [128, 8, 1088], BF16)
        self.wAs = self.sb("wAs", [128, 8, 64], BF16)
        dma("pool", self.wA.ap(), win_v[:, :, 0:1088], writes=[self.wA.t])
        op("act", lambda e: e.mul(out=self.wAs.ap()[:, :, 0:32], in_=self.wA.ap()[:, :, 1056:1088], mul=-1.0),
           reads=[self.wA.t], writes=[self.wAs.t])
        op("act", lambda e: e.copy(out=self.wAs.ap()[:, :, 32:64], in_=self.wA.ap()[:, :, 1024:1056]),
           reads=[self.wA.t], writes=[self.wAs.t])
        self.qaT = self.sb("qaT", [128, 6, NLAT], BF16)
        self.ckvT = self.sb("ckvT", [128, 2, NTOK], BF16)
        self.krT = self.sb("krT", [64, NTOK], BF16)
        self.cc = self.sb("cc", [64, NLAT]); dma("sp", self.cc.ap(), L["cc_d"], writes=[self.cc.t])
        self.ss = self.sb("ss", [64, NLAT]); dma("sp", self.ss.ap(), L["ss_d"], writes=[self.ss.t])
        self.sqf = [self.sb(f"sqf{i}", [128, 512]) for i in range(2)]
        self.rstd_bc = self.sb("rstd_bc", [128, 512])
        self.rtmp = [self.sb(f"rtmp{i}", [64, 512]) for i in range(2)]
        self.wkvb = self.sb("wkvb", [128, 2, 2048], BF16)
        dma("pool", self.wkvb.ap(), L["wkvb_d"].rearrange("(k p) c -> p k c", p=128), writes=[self.wkvb.t])
    wA, wAs, qaT, ckvT, krT, cc, ss, sqf, rstd_bc, rtmp, wkvb = (self.wA, self.wAs, self.qaT, self.ckvT, self.krT, self.cc,
                                                                 self.ss, self.sqf, self.rstd_bc, self.rtmp, self.wkvb)
    gqT, gkvT = L["gqT"], L["gkvT"]
    slabs = [(s * 512, 512) for s in range(4)] + [(NLAT, NCTX)]
    hdeps = lambda c0, n: hT_t[c0 // 128:(c0 + n) // 128]

    def norm_group(cols0, nch, dst, g, c0, n, nfeat, pbase):
        pbs = [psum(pbase + m) for m in range(nch)]
        pss = psum(pbase + nch)
        for m in range(nch):
            for k in range(8):
                op("pe", lambda e: e.matmul(pbs[m].ap()[:, 0:n], lhsT=wA.ap()[:, k, cols0 + m * 128: cols0 + (m + 1) * 128],
                                            rhs=hT.ap()[:, k, c0:c0 + n], start=(k == 0), stop=(k == 7)),
                   reads=[wA.t] + hdeps(c0, n), writes=[pbs[m].t])
        for m in range(nch):
            q = sqf[m % 2]
            op("act", lambda e: e.activation(out=q.ap()[:, 0:n], in_=pbs[m].ap()[:, 0:n], func=AF.Square), reads=[pbs[m].t], writes=[q.t])
            op("pe", lambda e: e.matmul(pss.ap()[:, 0:n], lhsT=ones_f.ap(), rhs=q.ap()[:, 0:n], start=(m == 0), stop=(m == nch - 1)),
               reads=[ones_f.t, q.t], writes=[pss.t])
        op("act", lambda e: e.activation(out=rstd_bc.ap()[:, 0:n], in_=pss.ap()[:, 0:n], func=AF.Ln, scale=1.0 / nfeat, bias=EPS),
           reads=[pss.t], writes=[rstd_bc.t])
        op("act", lambda e: e.activation(out=rstd_bc.ap()[:, 0:n], in_=rstd_bc.ap()[:, 0:n], func=AF.Exp, scale=-0.5),
           reads=[rstd_bc.t], writes=[rstd_bc.t])
        for m in range(nch):
            op("dve", lambda e: e.scalar_tensor_tensor(out=dst.ap()[:, m, c0:c0 + n], in0=pbs[m].ap()[:, 0:n], scalar=g.ap()[:, m:m + 1],
                                                       in1=rstd_bc.ap()[:, 0:n], op0=ALU.mult, op1=ALU.mult),
               reads=[pbs[m].t, g.t, rstd_bc.t], writes=[dst.t])

    for (c0, n) in slabs:
        if c0 < NLAT:
            norm_group(C_QA, 6, qaT, gqT, c0, n, 768, 0)
        norm_group(C_KV, 2, ckvT, gkvT, c0, n, 256, 0)
        pk, pks = psum(3), psum(4)
        for k in range(8):
            op("pe", lambda e: e.matmul(pk.ap()[0:64, 0:n], lhsT=wA.ap()[:, k, C_KR:C_KR + 64], rhs=hT.ap()[:, k, c0:c0 + n],
                                        start=(k == 0), stop=(k == 7)), reads=[wA.t] + hdeps(c0, n), writes=[pk.t])
        if c0 < NLAT:
            for k in range(8):
                op("pe", lambda e: e.matmul(pks.ap()[0:64, 0:n], lhsT=wAs.ap()[:, k, :], rhs=hT.ap()[:, k, c0:c0 + n],
                                            start=(k == 0), stop=(k == 7)), reads=[wAs.t] + hdeps(c0, n), writes=[pks.t])
            op("dve", lambda e: e.tensor_tensor(out=rtmp[0].ap()[:, 0:n], in0=pk.ap()[0:64, 0:n], in1=cc.ap()[:, c0:c0 + n], op=ALU.mult),
               reads=[pk.t, cc.t], writes=[rtmp[0].t])
            op("dve", lambda e: e.tensor_tensor(out=rtmp[1].ap()[:, 0:n], in0=pks.ap()[0:64, 0:n], in1=ss.ap()[:, c0:c0 + n], op=ALU.mult),
               reads=[pks.t, ss.t], writes=[rtmp[1].t])
            op("dve", lambda e: e.tensor_tensor(out=krT.ap()[:, c0:c0 + n], in0=rtmp[0].ap()[:, 0:n], in1=rtmp[1].ap()[:, 0:n], op=ALU.add),
               reads=[rtmp[0].t, rtmp[1].t], writes=[krT.t])
        else:
            op("act", lambda e: e.copy(out=krT.ap()[:, c0:c0 + n], in_=pk.ap()[0:64, 0:n]), reads=[pk.t], writes=[krT.t])
    if self.dbg and first:
        self.dump("qaT", qaT, [128, 6, NLAT], BF16)
        self.dump("ckvT", ckvT, [128, 2, NTOK], BF16)
        self.dump("krT", krT, [64, NTOK], BF16)
    if self.stop_after == "B0P2a":
        return

    if True:
        self.wq = [self.sb(f"wq{i}", [128, 6, 192], BF16) for i in range(2)]
        self.wqs = [self.sb(f"wqs{i}", [128, 6, 64], BF16) for i in range(2)]
        self.qT = self.sb("qT", [128, NLAT], BF16)
        self.qrT = self.sb("qrT", [64, NLAT], BF16)
        self.kT = self.sb("kT", [128, NTOK], BF16)
        self.vh = self.sb("vh", [128, 18, 128], BF16)
        self.PT = [self.sb(f"PT{i}", [128, 512], BF16) for i in range(4)]
        self.rs = self.sb("rs", [128, 512])
        self.yT = self.sb("yT", [128, 8, NLAT], BF16)
    wq, wqs, qT, qrT, kT, vh, PT, rs, yT = self.wq, self.wqs, self.qT, self.qrT, self.kT, self.vh, self.PT, self.rs, self.yT
    wqb_v = L["wqb_d"].rearrange("(k p) c -> p k c", p=128)
    scale = float(192 ** -0.5)
    for h in range(8):
        w, ws = wq[h % 2], wqs[h % 2]
        dma("pool", w.ap(), wqb_v[:, :, h * 192:(h + 1) * 192], writes=[w.t])
        op("act", lambda e: e.mul(out=ws.ap()[:, :, 0:32], in_=w.ap()[:, :, 160:192], mul=-1.0), reads=[w.t], writes=[ws.t])
        op("act", lambda e: e.copy(out=ws.ap()[:, :, 32:64], in_=w.ap()[:, :, 128:160]), reads=[w.t], writes=[ws.t])
        for s in range(4):
            c0 = s * 512
            pq, pr, prs = psum(0), psum(1), psum(2)
            for k in range(6):
                op("pe", lambda e: e.matmul(pq.ap(), lhsT=w.ap()[:, k, 0:128], rhs=qaT.ap()[:, k, c0:c0 + 512], start=(k == 0), stop=(k == 5)),
                   reads=[w.t, qaT.t], writes=[pq.t])
            for k in range(6):
                op("pe", lambda e: e.matmul(pr.ap()[0:64, :], lhsT=w.ap()[:, k, 128:192], rhs=qaT.ap()[:, k, c0:c0 + 512], start=(k == 0), stop=(k == 5)),
                   reads=[w.t, qaT.t], writes=[pr.t])
            for k in range(6):
                op("pe", lambda e: e.matmul(prs.ap()[0:64, :], lhsT=ws.ap()[:, k, :], rhs=qaT.ap()[:, k, c0:c0 + 512], start=(k == 0), stop=(k == 5)),
                   reads=[ws.t, qaT.t], writes=[prs.t])
            op("act", lambda e: e.copy(out=qT.ap()[:, c0:c0 + 512], in_=pq.ap()), reads=[pq.t], writes=[qT.t])
            op("dve", lambda e: e.tensor_tensor(out=rtmp[0].ap(), in0=pr.ap()[0:64, :], in1=cc.ap()[:, c0:c0 + 512], op=ALU.mult),
               reads=[pr.t, cc.t], writes=[rtmp[0].t])
            op("dve", lambda e: e.tensor_tensor(out=rtmp[1].ap(), in0=prs.ap()[0:64, :], in1=ss.ap()[:, c0:c0 + 512], op=ALU.mult),
               reads=[prs.t, ss.t], writes=[rtmp[1].t])
            op("dve", lambda e: e.tensor_tensor(out=qrT.ap()[:, c0:c0 + 512], in0=rtmp[0].ap(), in1=rtmp[1].ap(), op=ALU.add),
               reads=[rtmp[0].t, rtmp[1].t], writes=[qrT.t])
        for si, (c0, n) in enumerate(slabs):
            pk = psum(3 + si % 2)
            for k in range(2):
                op("pe", lambda e: e.matmul(pk.ap()[:, 0:n], lhsT=wkvb.ap()[:, k, h * 256:h * 256 + 128], rhs=ckvT.ap()[:, k, c0:c0 + n],
                                            start=(k == 0), stop=(k == 1)), reads=[wkvb.t, ckvT.t], writes=[pk.t])
            op("act", lambda e: e.copy(out=kT.ap()[:, c0:c0 + n], in_=pk.ap()[:, 0:n]), reads=[pk.t], writes=[kT.t])
        for g in range(5):
            pv = psum(5 + g % 2)
            nj = 4 if g < 4 else 2
            for jj in range(nj):
                j = g * 4 + jj
                for k in range(2):
                    op("pe", lambda e: e.matmul(pv.ap()[:, jj * 128:(jj + 1) * 128], lhsT=ckvT.ap()[:, k, j * 128:(j + 1) * 128],
                                                rhs=wkvb.ap()[:, k, h * 256 + 128:h * 256 + 256], start=(k == 0), stop=(k == 1)),
                       reads=[wkvb.t, ckvT.t], writes=[pv.t])
            op("dve", lambda e: e.tensor_copy(out=vh.ap()[:, g * 4:g * 4 + nj, :], in_=pv.ap()[:, 0:nj * 128].rearrange("p (j d) -> p j d", d=128)),
               reads=[pv.t], writes=[vh.t])
        for s in range(4):
            c0 = s * 512
            po, psm = psum(4 + s % 2), psum(6 + s % 2)

            def smm(kc):
                p = psum(kc % 4)
                op("pe", lambda e: e.matmul(p.ap(), lhsT=kT.ap()[:, kc * 128:(kc + 1) * 128], rhs=qT.ap()[:, c0:c0 + 512], start=True, stop=False),
                   reads=[kT.t, qT.t], writes=[p.t])
                op("pe", lambda e: e.matmul(p.ap(), lhsT=krT.ap()[:, kc * 128:(kc + 1) * 128], rhs=qrT.ap()[:, c0:c0 + 512], start=False, stop=True),
                   reads=[krT.t, qrT.t], writes=[p.t])
                pt = PT[kc % 4]
                op("act", lambda e: e.activation(out=pt.ap(), in_=p.ap(), func=AF.Exp, scale=scale), reads=[p.t], writes=[pt.t])

            def omm(kc):
                pt = PT[kc % 4]
                op("pe", lambda e: e.matmul(po.ap(), lhsT=vh.ap()[:, kc, :], rhs=pt.ap(), start=(kc == 0), stop=(kc == 17)),
                   reads=[vh.t, pt.t], writes=[po.t])
                op("pe", lambda e: e.matmul(psm.ap(), lhsT=ones_b.ap(), rhs=pt.ap(), start=(kc == 0), stop=(kc == 17)),
                   reads=[ones_b.t, pt.t], writes=[psm.t])

            smm(0); smm(1)
            for kc in range(18):
                if kc + 2 < 18:
                    smm(kc + 2)
                omm(kc)
            op("dve", lambda e: e.reciprocal(out=rs.ap(), in_=psm.ap()), reads=[psm.t], writes=[rs.t])
            op("dve", lambda e: e.tensor_tensor(out=yT.ap()[:, h, c0:c0 + 512], in0=po.ap(), in1=rs.ap(), op=ALU.mult),
               reads=[po.t, rs.t], writes=[yT.t])
    if self.dbg and first:
        self.dump("ymlaT", yT, [128, 8, NLAT], BF16)
    if self.stop_after == "B0P2":
        return
    ymlaD = L["ymlaD"]
    dma("sp", ymlaD[b], yT.ap(), reads=[yT.t], writes=[self.t_ymlaD])
    self.close_scope()
    self.hgrn(b, L)
    if self.stop_after == "B0P3":
        return
    self.merge_ffn_in(b, L)
    if self.stop_after == "B0P4":
        return
    self.close_scope()


def _hgrn(self, b, L):
    nc, S = self.nc, self.S
    op, dma = S.op, S.dma
    ps = self.ps
    ident_b, ones_f = self.ident_b, self.ones_f
    hT, hT_t = self.hT, self.hT_t
    win_v = L["win_d"].rearrange("(k p) c -> p k c", p=128)
    self.open_scope()
    wS = self.sb("wS", [128, 8, 4096], BF16)
    for pc in range(4):
        dma("pool", wS.ap()[:, :, pc * 1024:(pc + 1) * 1024], win_v[:, :, C_HQ + pc * 1024:C_HQ + (pc + 1) * 1024], writes=[wS.t])
    tri = self.sb("tri", [128, 4, 128]); dma("sp", tri.ap(), L["tri_d"], writes=[tri.t])
    cind = self.sb("cind", [128, NCH]); dma("sp", cind.ap(), L["cind_d"], writes=[cind.t])
    lb = [self.sb(f"lb{d}", [128, D]) for d in range(2)]
    lnoml = [self.sb(f"lnoml{d}", [128, D]) for d in range(2)]
    uL = [self.sb(f"u{d}", [128, D]) for d in range(2)]; L2L = [self.sb(f"L2{d}", [128, D]) for d in range(2)]
    tA_ = self.sb("tA", [128, D]); tB_ = self.sb("tB", [128, D])
    tAL = [tA_, tA_]; tBL = [tB_, tB_]
    u, L2, tA, tB = uL[0], L2L[0], tAL[0], tBL[0]
    lbl = L["lbl_d"]
    for d in range(2):
        dma("sp", u.ap(), lbl[d, 0, :].partition_broadcast(128), writes=[u.t])
        dma("sp", L2.ap(), lbl[d, 1, :].partition_broadcast(128), writes=[L2.t])
        op("dve", lambda e: e.tensor_tensor(out=tA.ap(), in0=L2.ap(), in1=u.ap(), op=ALU.subtract), reads=[u.t, L2.t], writes=[tA.t])
        op("act", lambda e: e.activation(out=tB.ap(), in_=tA.ap(), func=AF.Exp), reads=[tA.t], writes=[tB.t])
        op("act", lambda e: e.activation(out=tB.ap(), in_=tB.ap(), func=AF.Ln, bias=1.0), reads=[tB.t], writes=[tB.t])
        op("act", lambda e: e.activation(out=lb[d].ap(), in_=tB.ap(), func=AF.Exp, scale=-1.0), reads=[tB.t], writes=[lb[d].t])
        op("dve", lambda e: e.tensor_tensor(out=lnoml[d].ap(), in0=tA.ap(), in1=tB.ap(), op=ALU.subtract), reads=[tA.t, tB.t], writes=[lnoml[d].t])
    Kt = [self.sb(f"Kt{d}", [128, D], BF16) for d in range(2)]
    Kh = [self.sb(f"Kh{d}", [128, D], BF16) for d in range(2)]
    Qt = [self.sb(f"Qt{d}", [128, D], BF16) for d in range(2)]
    vt = [self.sb(f"vt{d}", [128, D], BF16) for d in range(2)]
    QT = [self.sb(f"QT{d}", [128, 8, 128], BF16) for d in range(2)]
    KT = [self.sb(f"KT{d}", [128, 8, 128], BF16) for d in range(2)]
    ATm = [self.sb(f"ATm{d}", [128, 8, 128], BF16) for d in range(2)]
    dec = [self.sb(f"dec{d}", [128, 8, NCH]) for d in range(2)]
    vt3 = [self.sb(f"vt3{d}", [128, D], BF16) for d in range(2)]
    Sf = [self.sb(f"Sf{d}", [128, 8, 128]) for d in range(2)]
    Sb = [self.sb(f"Sb{d}", [128, 8, 128], BF16) for d in range(2)]
    otL = [self.sb(f"ot{d}", [128, 8, 128]) for d in range(2)]
    o1 = self.sb("o1", [128, 8, 128])
    sqo = o1
    yh = self.sb("yh", [128, 8, 128], BF16)
    oD = self.dscr(f"oD{b}", [16, 128, 8, 128])
    t_oD = [T(f"oD{i}") for i in range(16)]
    yhD = L["yhD"]
    for d in range(2):
        op("pool", lambda e: e.memset(Sf[d].ap(), 0.0), writes=[Sf[d].t])
        op("pool", lambda e: e.memset(Sb[d].ap(), 0.0), writes=[Sb[d].t])
    ghgT = L["ghgT"]
    A2 = lambda i: (ps[i], ps[i + 1])

    def tokproj(tile, col0, pair):
        for half in range(2):
            p = pair[half]
            for k in range(8):
                op("pe", lambda e: e.matmul(p.ap(), lhsT=hT.ap()[:, k, tile * 128:(tile + 1) * 128],
                                            rhs=wS.ap()[:, k, col0 + half * 512: col0 + (half + 1) * 512], start=(k == 0), stop=(k == 7)),
                   reads=[hT_t[tile], wS.t], writes=[p.t])

    def two(fn, pair, reads, writes, eng):
        for half in range(2):
            p = pair[half]
            op(eng, lambda e: fn(e, p.ap(), slice(half * 512, (half + 1) * 512)), reads=[p.t] + reads, writes=writes)

    def prep(tile, d, need_o):
        pA, pB, pC, pD = A2(0), A2(2), A2(4), A2(6)
        u, L2, tA, tB = uL[d], L2L[d], tAL[d], tBL[d]
        ti_incl, ti_excl = (0, 1) if d == 0 else (2, 3)
        tokproj(tile, 1024 * (1 + d), pA)
        two(lambda e, p, c: e.activation(out=u.ap()[:, c], in_=p, func=AF.Exp, scale=-1.0), pA, [], [u.t], "act")
        op("act", lambda e: e.activation(out=L2.ap(), in_=u.ap(), func=AF.Ln, bias=1.0), reads=[u.t], writes=[L2.t])
        op("dve", lambda e: e.tensor_tensor(out=u.ap(), in0=u.ap(), in1=lb[d].ap(), op=ALU.mult), reads=[u.t, lb[d].t], writes=[u.t])
        op("act", lambda e: e.activation(out=u.ap(), in_=u.ap(), func=AF.Ln, bias=1.0), reads=[u.t], writes=[u.t])
        op("dve", lambda e: e.tensor_tensor(out=u.ap(), in0=u.ap(), in1=L2.ap(), op=ALU.subtract), reads=[u.t, L2.t], writes=[u.t])
        two(lambda e, p, c: e.scalar_tensor_tensor(out=L2.ap()[:, c], in0=p, scalar=-1.0, in1=L2.ap()[:, c], op0=ALU.mult, op1=ALU.subtract),
            pA, [L2.t], [L2.t], "dve")
        op("dve", lambda e: e.tensor_tensor(out=L2.ap(), in0=L2.ap(), in1=lnoml[d].ap(), op=ALU.add), reads=[L2.t, lnoml[d].t], writes=[L2.t])
        for half in range(2):
            op("pe", lambda e: e.matmul(pB[half].ap(), lhsT=tri.ap()[:, ti_incl, :], rhs=u.ap()[:, half * 512:(half + 1) * 512], start=True, stop=True),
               reads=[tri.t, u.t], writes=[pB[half].t])
            op("pe", lambda e: e.matmul(pC[half].ap(), lhsT=tri.ap()[:, ti_excl, :], rhs=u.ap()[:, half * 512:(half + 1) * 512], start=True, stop=True),
               reads=[tri.t, u.t], writes=[pC[half].t])
        for h in range(8):
            op("pe", lambda e: e.matmul(pD[0].ap()[:, h * NCH:(h + 1) * NCH], lhsT=u.ap()[:, h * 128:(h + 1) * 128], rhs=cind.ap(), start=True, stop=True),
               reads=[u.t, cind.t], writes=[pD[0].t])
        op("act", lambda e: e.activation(out=dec[d].ap(), in_=pD[0].ap()[:, 0:8 * NCH].rearrange("p (h c) -> p h c", c=NCH), func=AF.Exp),
           reads=[pD[0].t], writes=[dec[d].t])
        if need_o:
            two(lambda e, p, c: e.tensor_tensor(out=tA.ap()[:, c], in0=L2.ap()[:, c], in1=p, op=ALU.subtract), pB, [L2.t], [tA.t], "dve")
            op("act", lambda e: e.activation(out=Kt[d].ap(), in_=tA.ap(), func=AF.Exp), reads=[tA.t], writes=[Kt[d].t])
        two(lambda e, p, c: e.tensor_tensor(out=tB.ap()[:, c], in0=L2.ap()[:, c], in1=p, op=ALU.add), pC, [L2.t], [tB.t], "dve")
        op("act", lambda e: e.activation(out=Kh[d].ap(), in_=tB.ap(), func=AF.Exp), reads=[tB.t], writes=[Kh[d].t])
        if need_o:
            two(lambda e, p, c: e.activation(out=tA.ap()[:, c], in_=p, func=AF.Exp), pB, [], [tA.t], "act")
            tokproj(tile, 0, pA)
            two(lambda e, p, c: e.activation(out=tB.ap()[:, c], in_=p, func=AF.Exp, scale=-1.0), pA, [], [tB.t], "act")
            op("act", lambda e: e.activation(out=tB.ap(), in_=tB.ap(), func=AF.Ln, bias=1.0), reads=[tB.t], writes=[tB.t])
            op("act", lambda e: e.activation(out=tB.ap(), in_=tB.ap(), func=AF.Exp, scale=-1.0), reads=[tB.t], writes=[tB.t])
            two(lambda e, p, c: e.scalar_tensor_tensor(out=tB.ap()[:, c], in0=p, scalar=float(128 ** -0.5), in1=tB.ap()[:, c], op0=ALU.mult, op1=ALU.mult),
                pA, [tB.t], [tB.t], "dve")
            op("dve", lambda e: e.tensor_tensor(out=Qt[d].ap(), in0=tB.ap(), in1=tA.ap(), op=ALU.mult), reads=[tA.t, tB.t], writes=[Qt[d].t])
        tokproj(tile, 3072, pC)
        two(lambda e, p, c: e.copy(out=vt[d].ap()[:, c], in_=p), pC, [], [vt[d].t], "act")
        two(lambda e, p, c: e.activation(out=vt3[d].ap()[:, c], in_=p, func=AF.Identity, scale=cind.ap()[:, NCH - 1:NCH]), pC, [cind.t], [vt3[d].t], "act")
        if need_o:
            for (src, dst, pp) in ((Qt[d], QT[d], pB[0]), (Kt[d], KT[d], pB[1])):
                pv = pp.ap().bitcast(BF16)
                for h in range(8):
                    op("pe", lambda e: e.transpose(out=pv[:, h * 128:(h + 1) * 128], in_=src.ap()[:, h * 128:(h + 1) * 128], identity=ident_b.ap()),
                       reads=[src.t, ident_b.t], writes=[pp.t])
                op("act", lambda e: e.copy(out=dst.ap(), in_=pv.rearrange("p (h t) -> p h t", t=128)), reads=[pp.t], writes=[dst.t])
            for h in range(8):
                p = pA[h // 4]
                op("pe", lambda e: e.matmul(p.ap()[:, (h % 4) * 128:(h % 4 + 1) * 128], lhsT=KT[d].ap()[:, h, :], rhs=QT[d].ap()[:, h, :], start=True, stop=True),
                   reads=[KT[d].t, QT[d].t], writes=[p.t])
            for half in range(2):
                op("dve", lambda e: e.tensor_tensor(out=ATm[d].ap()[:, half * 4:(half + 1) * 4, :],
                                                    in0=pA[half].ap().rearrange("p (h t) -> p h t", t=128),
                                                    in1=tri.ap()[:, ti_incl:ti_incl + 1, :].to_broadcast([128, 4, 128]), op=ALU.mult),
                   reads=[pA[half].t, tri.t], writes=[ATm[d].t])

    def rec_parts(tile, d, need_o):
        pO = A2(6) if d == 0 else A2(0)
        pS = A2(2) if d == 0 else A2(4)
        ot = otL[d]
        corder = tuple(range(NCH)) if d == 0 else tuple(range(NCH - 1, -1, -1))

        def pre():
            if not need_o:
                return
            for h in range(8):
                p = pO[h // 4]
                cs = slice((h % 4) * 128, (h % 4 + 1) * 128)
                op("pe", lambda e: e.matmul(p.ap()[:, cs], lhsT=vt[d].ap()[:, h * 128:(h + 1) * 128], rhs=ATm[d].ap()[:, h, :], start=True, stop=True),
                   reads=[vt[d].t, ATm[d].t], writes=[p.t])
            for half in range(2):
                op("act", lambda e: e.copy(out=ot.ap()[:, half * 4:(half + 1) * 4, :], in_=pO[half].ap().rearrange("p (h t) -> p h t", t=128)),
                   reads=[pO[half].t], writes=[ot.t])

        def chunk(c):
            if need_o:
                for h in range(8):
                    p = pO[h // 4]
                    cs = slice((h % 4) * 128 + c * HC, (h % 4) * 128 + (c + 1) * HC)
                    op("pe", lambda e: e.matmul(p.ap()[:, cs], lhsT=Sb[d].ap()[:, h, :], rhs=QT[d].ap()[:, h, c * HC:(c + 1) * HC], start=True, stop=True),
                       reads=[Sb[d].t, QT[d].t], writes=[p.t])
            for h in range(8):
                p = pS[h // 4]
                if c < NCH - 1:
                    op("pe", lambda e: e.matmul(p.ap()[:, (h % 4) * 128:(h % 4 + 1) * 128], lhsT=Kh[d].ap()[c * HC:(c + 1) * HC, h * 128:(h + 1) * 128],
                                                rhs=vt[d].ap()[c * HC:(c + 1) * HC, h * 128:(h + 1) * 128], start=True, stop=True),
                       reads=[Kh[d].t, vt[d].t], writes=[p.t])
                else:
                    op("pe", lambda e: e.matmul(p.ap()[:, (h % 4) * 128:(h % 4 + 1) * 128], lhsT=Kh[d].ap()[:, h * 128:(h + 1) * 128],
                                                rhs=vt3[d].ap()[:, h * 128:(h + 1) * 128], start=True, stop=True),
                       reads=[Kh[d].t, vt3[d].t], writes=[p.t])
            op("dve", lambda e: e.tensor_tensor(out=Sf[d].ap(), in0=Sf[d].ap(), in1=dec[d].ap()[:, :, c:c + 1].to_broadcast([128, 8, 128]), op=ALU.mult),
               reads=[Sf[d].t, dec[d].t], writes=[Sf[d].t])
            for half in range(2):
                op("dve", lambda e: e.tensor_tensor(out=Sf[d].ap()[:, half * 4:(half + 1) * 4, :], in0=Sf[d].ap()[:, half * 4:(half + 1) * 4, :],
                                                    in1=pS[half].ap().rearrange("p (h v) -> p h v", v=128), op=ALU.add),
                   reads=[Sf[d].t, pS[half].t], writes=[Sf[d].t])
            op("act", lambda e: e.copy(out=Sb[d].ap(), in_=Sf[d].ap()), reads=[Sf[d].t], writes=[Sb[d].t])

        def post():
            if not need_o:
                return
            lt = tile
            second = (d == 0 and lt >= 8) or (d == 1 and lt < 8)
            for half in range(2):
                op("dve", lambda e: e.tensor_tensor(out=ot.ap()[:, half * 4:(half + 1) * 4, :], in0=ot.ap()[:, half * 4:(half + 1) * 4, :],
                                                    in1=pO[half].ap().rearrange("p (h t) -> p h t", t=128), op=ALU.add),
                   reads=[ot.t, pO[half].t], writes=[ot.t])
            if not second:
                dma("sp", oD[lt], ot.ap(), reads=[ot.t], writes=[t_oD[lt]])
                return
            dma("sp", o1.ap(), oD[lt], reads=[t_oD[lt]], writes=[o1.t])
            op("dve", lambda e: e.tensor_tensor(out=ot.ap(), in0=ot.ap(), in1=o1.ap(), op=ALU.add), reads=[ot.t, o1.t], writes=[ot.t])
            op("act", lambda e: e.activation(out=sqo.ap(), in_=ot.ap(), func=AF.Square), reads=[ot.t], writes=[sqo.t])
            pN = pO
            for half in range(2):
                op("pe", lambda e: e.matmul(pN[half].ap(), lhsT=ones_f.ap(), rhs=sqo.ap()[:, half * 4:(half + 1) * 4, :], start=True, stop=True),
                   reads=[ones_f.t, sqo.t], writes=[pN[half].t])
                op("act", lambda e: e.activation(out=sqo.ap()[:, half * 4:(half + 1) * 4, :], in_=pN[half].ap().rearrange("p (h t) -> p h t", t=128),
                                                 func=AF.Ln, scale=1.0 / 128, bias=EPS), reads=[pN[half].t], writes=[sqo.t])
            op("act", lambda e: e.activation(out=sqo.ap(), in_=sqo.ap(), func=AF.Exp, scale=-0.5), reads=[sqo.t], writes=[sqo.t])
            op("dve", lambda e: e.scalar_tensor_tensor(out=yh.ap(), in0=ot.ap(), scalar=ghgT.ap()[:, 0:1], in1=sqo.ap(), op0=ALU.mult, op1=ALU.mult),
               reads=[ot.t, sqo.t, ghgT.t], writes=[yh.t])
            dma("sp", yhD[b, :, :, lt * 128:(lt + 1) * 128], yh.ap(), reads=[yh.t], writes=[self.t_yhD])

        return [pre] + [(lambda c=c: chunk(c)) for c in corder] + [post]

    forder = [16, 17] + list(range(16))
    border = [17, 16] + list(range(15, -1, -1))
    for step in range(18):
        tf, tb = forder[step], border[step]
        prep(tf, 0, tf < 16)
        prep(tb, 1, tb < 16)
        for fa, fb in zip(rec_parts(tf, 0, tf < 16), rec_parts(tb, 1, tb < 16)):
            fa()
            fb()
    if self.dbg and b == 0:
        o = self.dout("dbg_yh", [128, 8, NLAT], BF16)
        t = T("dbg_yh")
        S.dma("sp", o, yhD[0], reads=[self.t_yhD], writes=[t])
        self.dbg_outs["yh"] = t
    self.close_scope()


def _merge_ffn_in(self, b, L):
    nc, S = self.nc, self.S
    op, dma = S.op, S.dma
    ps = self.ps
    ident_b, ones_b = self.ident_b, self.ones_b
    hT, hT_t = self.hT, self.hT_t
    win_v = L["win_d"].rearrange("(k p) c -> p k c", p=128)
    modD = self.modD
    self.open_scope()
    yT = self.sb("yTm", [128, 8, NLAT], BF16)
    self.open_scope()
    wG = self.sb("wG", [128, 8, 3072], BF16)
    for pc in range(3):
        dma("pool", wG.ap()[:, :, pc * 1024:(pc + 1) * 1024], win_v[:, :, C_HG + pc * 1024:C_HG + (pc + 1) * 1024], writes=[wG.t])
    ym = [self.sb(f"ym{i}", [128, 8, 512], BF16) for i in range(2)]
    yhs = [self.sb(f"yhs{i}", [128, 8, 512], BF16) for i in range(2)]
    sg = [self.sb(f"sg{i}", [128, 512]) for i in range(3)]
    t1 = self.sb("t1", [128, 512]); t2 = self.sb("t2", [128, 512])
    for s in range(4):
        c0 = s * 512
        dma("sp", ym[s % 2].ap(), L["ymlaD"][b, :, :, c0:c0 + 512], reads=[self.t_ymlaD], writes=[ym[s % 2].t])
        dma("sp", yhs[s % 2].ap(), L["yhD"][b, :, :, c0:c0 + 512], reads=[self.t_yhD], writes=[yhs[s % 2].t])
        for m in range(8):
            pg = [ps[(m * 3 + g) % 8] for g in range(3)]
            for g in range(3):
                for k in range(8):
                    op("pe", lambda e: e.matmul(pg[g].ap(), lhsT=wG.ap()[:, k, g * 1024 + m * 128: g * 1024 + (m + 1) * 128],
                                                rhs=hT.ap()[:, k, c0:c0 + 512], start=(k == 0), stop=(k == 7)),
                       reads=[wG.t] + hT_t[c0 // 128:(c0 + 512) // 128], writes=[pg[g].t])
                op("act", lambda e: e.activation(out=sg[g].ap(), in_=pg[g].ap(), func=AF.Sigmoid), reads=[pg[g].t], writes=[sg[g].t])
            op("dve", lambda e: e.tensor_tensor(out=t1.ap(), in0=pg[0].ap(), in1=sg[0].ap(), op=ALU.mult), reads=[pg[0].t, sg[0].t], writes=[t1.t])
            op("dve", lambda e: e.tensor_tensor(out=t1.ap(), in0=t1.ap(), in1=yhs[s % 2].ap()[:, m, :], op=ALU.mult), reads=[t1.t, yhs[s % 2].t], writes=[t1.t])
            op("dve", lambda e: e.tensor_tensor(out=t1.ap(), in0=t1.ap(), in1=sg[2].ap(), op=ALU.mult), reads=[t1.t, sg[2].t], writes=[t1.t])
            op("dve", lambda e: e.tensor_tensor(out=t2.ap(), in0=sg[1].ap(), in1=ym[s % 2].ap()[:, m, :], op=ALU.mult), reads=[sg[1].t, ym[s % 2].t], writes=[t2.t])
            op("dve", lambda e: e.tensor_tensor(out=yT.ap()[:, m, c0:c0 + 512], in0=t1.ap(), in1=t2.ap(), op=ALU.add), reads=[t1.t, t2.t], writes=[yT.t])
    self.close_scope()
    wo = self.sb("wo", [128, 8, D], BF16)
    dma("pool", wo.ap(), L["wout_d"].rearrange("(k p) c -> p k c", p=128), writes=[wo.t])
    wr = self.sb("wr", [128, 8, NEXP], BF16)
    dma("pool", wr.ap(), L["wr_d"].rearrange("(k p) c -> p k c", p=128), writes=[wr.t])
    brb = self.sb("brb", [1, NEXP], BF16)
    dma("pool", brb.ap(), L["br_d"], writes=[brb.t])
    mod2 = self.sb("mod2", [128, D]); dma("sp", mod2.ap(), modD[b, 2 * D:3 * D].partition_broadcast(128), reads=[self.t_modD], writes=[mod2.t])
    S2 = self.sb("S2", [128, D]); dma("sp", S2.ap(), modD[b, 3 * D:4 * D].partition_broadcast(128), reads=[self.t_modD], writes=[S2.t])
    G2 = self.sb("G2", [128, D]); dma("sp", G2.ap(), modD[b, 4 * D:5 * D].partition_broadcast(128), reads=[self.t_modD], writes=[G2.t])
    gf = self.sb("gf", [128, D]); dma("sp", gf.ap(), L["gffn_d"][0, :].partition_broadcast(128), writes=[gf.t])
    op("dve", lambda e: e.scalar_tensor_tensor(out=G2.ap(), in0=G2.ap(), scalar=1.0, in1=gf.ap(), op0=ALU.add, op1=ALU.mult),
       reads=[G2.t, gf.t], writes=[G2.t])
    xr = [self.sb(f"xr{i}", [128, D]) for i in range(2)]
    tm = self.sb("tm", [128, D])
    sq = self.sb("sq4", [128, D])
    st = self.sb("st4", [128, 4])
    h2 = [self.sb(f"h2_{i}", [128, D], BF16) for i in range(2)]
    h2T = self.sb("h2T", [128, 8, 128], BF16)
    lg = self.sb("lg", [128, NEXP])
    mx = self.sb("mx", [128, 8]); ix = self.sb("ix", [128, 8], U32); ixf = self.sb("ixf", [128, 8])
    oh = [self.sb(f"oh{k}", [128, NEXP]) for k in range(4)]
    msk = self.sb("msk", [128, NEXP]); mskb = self.sb("mskb", [128, NEXP], BF16)
    ex = self.sb("ex", [128, 4]); sm = self.sb("sm", [128, 2])
    pos = self.sb("pos", [128, NEXP]); tmp32 = self.sb("tmp32", [128, NEXP]); dstf = self.sb("dstf", [128, 4])
    x_d, x1D, XeD = L["x_d"], L["x1D"], L["XeD"]
    base_bc, destAll, probAll, ustr, iota_e, iota_c = self.base_bc, self.destAll, self.probAll, self.ustr, self.iota_e, self.iota_c
    for i in range(16):
        gt = b * 16 + i
        r0 = b * NLAT + i * 128
        xb, hb = xr[i % 2], h2[i % 2]
        dma("sp", xb.ap(), x_d[r0:r0 + 128, :], writes=[xb.t])
        pm = (ps[0], ps[1])
        for half in range(2):
            for k in range(8):
                op("pe", lambda e: e.matmul(pm[half].ap(), lhsT=yT.ap()[:, k, i * 128:(i + 1) * 128], rhs=wo.ap()[:, k, half * 512:(half + 1) * 512],
                                            start=(k == 0), stop=(k == 7)), reads=[yT.t, wo.t], writes=[pm[half].t])
            op("dve", lambda e: e.tensor_tensor(out=tm.ap()[:, half * 512:(half + 1) * 512], in0=pm[half].ap(), in1=mod2.ap()[:, half * 512:(half + 1) * 512], op=ALU.mult),
               reads=[pm[half].t, mod2.t], writes=[tm.t])
        op("dve", lambda e: e.tensor_tensor(out=xb.ap(), in0=xb.ap(), in1=tm.ap(), op=ALU.add), reads=[xb.t, tm.t], writes=[xb.t])
        dma("sp", x1D[r0:r0 + 128, :], xb.ap(), reads=[xb.t], writes=[self.tl_x1[gt]])
        op("act", lambda e: e.activation(out=sq.ap(), in_=xb.ap(), func=AF.Square, accum_out=st.ap()[:, 0:1]), reads=[xb.t], writes=[sq.t, st.t])
        op("act", lambda e: e.activation(out=st.ap()[:, 1:2], in_=st.ap()[:, 0:1], func=AF.Ln, scale=1.0 / D, bias=EPS), reads=[st.t], writes=[st.t])
        op("act", lambda e: e.activation(out=st.ap()[:, 2:3], in_=st.ap()[:, 1:2], func=AF.Exp, scale=-0.5), reads=[st.t], writes=[st.t])
        op("dve", lambda e: e.scalar_tensor_tensor(out=tm.ap(), in0=xb.ap(), scalar=st.ap()[:, 2:3], in1=G2.ap(), op0=ALU.mult, op1=ALU.mult),
           reads=[xb.t, st.t, G2.t], writes=[tm.t])
        op("dve", lambda e: e.tensor_tensor(out=hb.ap(), in0=tm.ap(), in1=S2.ap(), op=ALU.add), reads=[tm.t, S2.t], writes=[hb.t])
        pt = ps[2]
        ptv = pt.ap().bitcast(BF16)
        for k in range(8):
            op("pe", lambda e: e.transpose(out=ptv[:, k * 128:(k + 1) * 128], in_=hb.ap()[:, k * 128:(k + 1) * 128], identity=ident_b.ap()),
               reads=[hb.t, ident_b.t], writes=[pt.t])
        op("act", lambda e: e.copy(out=h2T.ap(), in_=ptv.rearrange("p (k t) -> p k t", t=128)), reads=[pt.t], writes=[h2T.t])
        pl = ps[3]
        for k in range(8):
            op("pe", lambda e: e.matmul(pl.ap()[:, 0:NEXP], lhsT=h2T.ap()[:, k, :], rhs=wr.ap()[:, k, :], start=(k == 0), stop=False),
               reads=[h2T.t, wr.t], writes=[pl.t])
        op("pe", lambda e: e.matmul(pl.ap()[:, 0:NEXP], lhsT=ones_b.ap()[0:1, :], rhs=brb.ap(), start=False, stop=True),
           reads=[ones_b.t, brb.t], writes=[pl.t])
        op("act", lambda e: e.copy(out=lg.ap(), in_=pl.ap()[:, 0:NEXP]), reads=[pl.t], writes=[lg.t])
        op("dve", lambda e: e.max(out=mx.ap(), in_=lg.ap()), reads=[lg.t], writes=[mx.t])
        op("dve", lambda e: e.max_index(out=ix.ap(), in_max=mx.ap(), in_values=lg.ap()), reads=[mx.t, lg.t], writes=[ix.t])
        op("dve", lambda e: e.tensor_copy(out=ixf.ap(), in_=ix.ap()), reads=[ix.t], writes=[ixf.t])
        for k in range(4):
            op("dve", lambda e: e.tensor_scalar(out=oh[k].ap(), in0=iota_e.ap(), scalar1=ixf.ap()[:, k:k + 1], scalar2=None, op0=ALU.is_equal),
               reads=[iota_e.t, ixf.t], writes=[oh[k].t])
        op("dve", lambda e: e.tensor_tensor(out=msk.ap(), in0=oh[0].ap(), in1=oh[1].ap(), op=ALU.add), reads=[oh[0].t, oh[1].t], writes=[msk.t])
        op("dve", lambda e: e.tensor_tensor(out=msk.ap(), in0=msk.ap(), in1=oh[2].ap(), op=ALU.add), reads=[msk.t, oh[2].t], writes=[msk.t])
        op("dve", lambda e: e.tensor_tensor(out=mskb.ap(), in0=msk.ap(), in1=oh[3].ap(), op=ALU.add), reads=[msk.t, oh[3].t], writes=[mskb.t])
        op("dve", lambda e: e.tensor_scalar(out=sm.ap()[:, 0:1], in0=mx.ap()[:, 0:1], scalar1=-1.0, scalar2=None, op0=ALU.mult), reads=[mx.t], writes=[sm.t])
        op("act", lambda e: e.activation(out=ex.ap(), in_=mx.ap()[:, 0:4], func=AF.Exp, bias=sm.ap()[:, 0:1], accum_out=sm.ap()[:, 1:2]),
           reads=[mx.t, sm.t], writes=[ex.t, sm.t])
        op("dve", lambda e: e.reciprocal(out=sm.ap()[:, 1:2], in_=sm.ap()[:, 1:2]), reads=[sm.t], writes=[sm.t])
        op("dve", lambda e: e.tensor_scalar(out=probAll.ap()[:, gt, :], in0=ex.ap(), scalar1=sm.ap()[:, 1:2], scalar2=None, op0=ALU.mult),
           reads=[ex.t, sm.t], writes=[probAll.t])
        pp, pc = ps[4], ps[5]
        op("pe", lambda e: e.matmul(pp.ap()[:, 0:NEXP], lhsT=ustr.ap(), rhs=mskb.ap(), start=True, stop=True), reads=[ustr.t, mskb.t], writes=[pp.t])
        op("pe", lambda e: e.matmul(pc.ap()[:, 0:NEXP], lhsT=ones_b.ap(), rhs=mskb.ap(), start=True, stop=True), reads=[ones_b.t, mskb.t], writes=[pc.t])
        op("dve", lambda e: e.tensor_tensor(out=pos.ap(), in0=pp.ap()[:, 0:NEXP], in1=base_bc.ap(), op=ALU.add), reads=[pp.t, base_bc.t], writes=[pos.t])
        op("dve", lambda e: e.tensor_tensor(out=base_bc.ap(), in0=pc.ap()[:, 0:NEXP], in1=base_bc.ap(), op=ALU.add), reads=[pc.t, base_bc.t], writes=[base_bc.t])
        op("dve", lambda e: e.tensor_tensor(out=pos.ap(), in0=pos.ap(), in1=iota_c.ap(), op=ALU.add), reads=[pos.t, iota_c.t], writes=[pos.t])
        for k in range(4):
            op("dve", lambda e: e.tensor_tensor(out=tmp32.ap(), in0=pos.ap(), in1=oh[k].ap(), op=ALU.mult), reads=[pos.t, oh[k].t], writes=[tmp32.t])
            op("dve", lambda e: e.reduce_sum(out=dstf.ap()[:, k:k + 1], in_=tmp32.ap(), axis=AX.X), reads=[tmp32.t], writes=[dstf.t])
        op("dve", lambda e: e.tensor_copy(out=destAll.ap()[:, gt, :], in_=dstf.ap()), reads=[dstf.t], writes=[destAll.t])
        op("dve", lambda e: e.tensor_copy(out=self.dstfAll.ap()[:, gt, :], in_=dstf.ap()), reads=[dstf.t], writes=[self.dstfAll.t])
        op("dve", lambda e: e.tensor_copy(out=self.eidAll.ap()[:, gt, :], in_=ixf.ap()[:, 0:4]), reads=[ixf.t], writes=[self.eidAll.t])
        for k in range(4):
            tsc = T("Xsc"); self.tl_Xe.append(tsc)
            dma("pool", None, None, reads=[hb.t, destAll.t, self.t_XeD], writes=[tsc],
                fn=lambda e: e.indirect_dma_start(out=XeD, out_offset=bass.IndirectOffsetOnAxis(ap=destAll.ap()[:, gt, k:k + 1], axis=0),
                                                  in_=hb.ap(), in_offset=None))
    if self.dbg and b == 0:
        o = self.dout("dbg_x1", [NLAT, D]); t = T("dbg_x1")
        S.dma("sp", o, x1D[0:NLAT, :], reads=self.tl_x1[0:16], writes=[t]); self.dbg_outs["x1"] = t
        self.dump("yTm", yT, [128, 8, NLAT], BF16)
        self.dump("destAll", destAll, [128, 32, 4], I32)
        self.dump("probAll", probAll, [128, 32, 4])
        self.dump("base_bc", base_bc, [128, NEXP])
        self.dump("lg", lg, [128, NEXP])
        self.dump("h2last", h2[1], [128, D], BF16)
    self.close_scope()


def _moe(self, L):
    nc, S = self.nc, self.S
    op, dma = S.op, S.dma
    ps = self.ps
    ident_b, ones_b = self.ident_b, self.ones_b
    XeD, YeD, x1D, modD = L["XeD"], L["YeD"], L["x1D"], self.modD
    cnt, iota_e = self.base_bc, self.iota_e
    eI = self.sb("eI", [128, NSL], I32)
    rI = self.sb("rI", [128, NSL], I32)
    wI = self.sb("wI", [128, NSL], I32)
    xI = self.sb("xI", [128, NSL, NB], I32)
    ydest = self.sb("ydest", [128, 32, 4], I32)
    self.open_scope()
    jota = self.sb("jota", [128, NSL]); dma("sp", jota.ap(), L["jota_d"], writes=[jota.t])
    nsl = self.sb("nsl", [128, NEXP]); tmp = self.sb("tmpe", [128, NEXP])
    ca = self.sb("ca", [128, NEXP]); cb = self.sb("cb", [128, NEXP]); cstart = self.sb("cstart", [128, NEXP]); adj = self.sb("adj", [128, NEXP])
    op("dve", lambda e: e.tensor_scalar(out=nsl.ap(), in0=cnt.ap(), scalar1=0.0, scalar2=None, op0=ALU.is_gt), reads=[cnt.t], writes=[nsl.t])
    for m in range(1, CAP // SLAB):
        op("dve", lambda e: e.tensor_scalar(out=tmp.ap(), in0=cnt.ap(), scalar1=float(m * SLAB), scalar2=None, op0=ALU.is_gt), reads=[cnt.t], writes=[tmp.t])
        op("dve", lambda e: e.tensor_tensor(out=nsl.ap(), in0=nsl.ap(), in1=tmp.ap(), op=ALU.add), reads=[nsl.t, tmp.t], writes=[nsl.t])
    op("dve", lambda e: e.tensor_copy(out=ca.ap(), in_=nsl.ap()), reads=[nsl.t], writes=[ca.t])
    src, dst = ca, cb
    sh = 1
    while sh < NEXP:
        op("dve", lambda e: e.tensor_copy(out=dst.ap()[:, 0:sh], in_=src.ap()[:, 0:sh]), reads=[src.t], writes=[dst.t])
        op("dve", lambda e: e.tensor_tensor(out=dst.ap()[:, sh:], in0=src.ap()[:, sh:], in1=src.ap()[:, 0:NEXP - sh], op=ALU.add), reads=[src.t], writes=[dst.t])
        src, dst = dst, src
        sh *= 2
    cend = src
    op("dve", lambda e: e.tensor_tensor(out=cstart.ap(), in0=cend.ap(), in1=nsl.ap(), op=ALU.subtract), reads=[cend.t, nsl.t], writes=[cstart.t])
    big = self.sb("big3", [128, 128, NEXP])
    ej = self.sb("ej", [128, NSL]); csj = self.sb("csj", [128, NSL]); vj = self.sb("vj", [128, NSL]); rj = self.sb("rj", [128, NSL])
    b3 = big.ap()[:, 0:NSL, :]
    op("dve", lambda e: e.tensor_tensor(out=b3, in0=cend.ap().unsqueeze(1).to_broadcast([128, NSL, NEXP]),
                                        in1=jota.ap().unsqueeze(2).to_broadcast([128, NSL, NEXP]), op=ALU.is_le), reads=[cend.t, jota.t], writes=[big.t])
    op("dve", lambda e: e.reduce_sum(out=ej.ap(), in_=b3, axis=AX.X), reads=[big.t], writes=[ej.t])
    op("dve", lambda e: e.tensor_scalar_min(out=ej.ap(), in0=ej.ap(), scalar1=float(NEXP - 1)), reads=[ej.t], writes=[ej.t])
    op("dve", lambda e: e.tensor_tensor(out=b3, in0=iota_e.ap().unsqueeze(1).to_broadcast([128, NSL, NEXP]),
                                        in1=ej.ap().unsqueeze(2).to_broadcast([128, NSL, NEXP]), op=ALU.is_equal), reads=[iota_e.t, ej.t], writes=[big.t])
    op("dve", lambda e: e.tensor_tensor(out=b3, in0=b3, in1=cstart.ap().unsqueeze(1).to_broadcast([128, NSL, NEXP]), op=ALU.mult), reads=[big.t, cstart.t], writes=[big.t])
    op("dve", lambda e: e.reduce_sum(out=csj.ap(), in_=b3, axis=AX.X), reads=[big.t], writes=[csj.t])
    op("dve", lambda e: e.tensor_scalar(out=vj.ap(), in0=jota.ap(), scalar1=cend.ap()[:, NEXP - 1:NEXP], scalar2=None, op0=ALU.is_lt), reads=[jota.t, cend.t], writes=[vj.t])
    op("dve", lambda e: e.tensor_tensor(out=rj.ap(), in0=jota.ap(), in1=csj.ap(), op=ALU.subtract), reads=[jota.t, csj.t], writes=[rj.t])
    op("dve", lambda e: e.tensor_scalar(out=rj.ap(), in0=rj.ap(), scalar1=float(SLAB), scalar2=None, op0=ALU.mult), reads=[rj.t], writes=[rj.t])
    cntj = self.sb("cntj", [128, NSL]); off4 = self.sb("off4", [128, NB]); dma("sp", off4.ap(), L["off4_d"], writes=[off4.t])
    op("dve", lambda e: e.tensor_tensor(out=b3, in0=iota_e.ap().unsqueeze(1).to_broadcast([128, NSL, NEXP]),
                                        in1=ej.ap().unsqueeze(2).to_broadcast([128, NSL, NEXP]), op=ALU.is_equal), reads=[iota_e.t, ej.t], writes=[big.t])
    op("dve", lambda e: e.tensor_tensor(out=b3, in0=b3, in1=cnt.ap().unsqueeze(1).to_broadcast([128, NSL, NEXP]), op=ALU.mult), reads=[big.t, cnt.t], writes=[big.t])
    op("dve", lambda e: e.reduce_sum(out=cntj.ap(), in_=b3, axis=AX.X), reads=[big.t], writes=[cntj.t])
    op("dve", lambda e: e.tensor_tensor(out=cntj.ap(), in0=cntj.ap(), in1=vj.ap(), op=ALU.mult), reads=[cntj.t, vj.t], writes=[cntj.t])
    op("dve", lambda e: e.tensor_tensor(out=ej.ap(), in0=ej.ap(), in1=vj.ap(), op=ALU.mult), reads=[ej.t, vj.t], writes=[ej.t])
    posb = self.sb("posb", [128, NSL, NB]); val4 = self.sb("val4", [128, NSL, NB])
    op("dve", lambda e: e.tensor_tensor(out=posb.ap(), in0=rj.ap().unsqueeze(2).to_broadcast([128, NSL, NB]),
                                        in1=off4.ap().unsqueeze(1).to_broadcast([128, NSL, NB]), op=ALU.add), reads=[rj.t, off4.t], writes=[posb.t])
    op("dve", lambda e: e.tensor_tensor(out=val4.ap(), in0=posb.ap(), in1=cntj.ap().unsqueeze(2).to_broadcast([128, NSL, NB]), op=ALU.is_lt),
       reads=[posb.t, cntj.t], writes=[val4.t])
    op("dve", lambda e: e.tensor_tensor(out=posb.ap(), in0=posb.ap(), in1=val4.ap(), op=ALU.mult), reads=[posb.t, val4.t], writes=[posb.t])
    op("dve", lambda e: e.tensor_scalar(out=rj.ap(), in0=ej.ap(), scalar1=float(CAP), scalar2=None, op0=ALU.mult), reads=[ej.t], writes=[rj.t])
    op("dve", lambda e: e.tensor_tensor(out=posb.ap(), in0=posb.ap(), in1=rj.ap().unsqueeze(2).to_broadcast([128, NSL, NB]), op=ALU.add), reads=[posb.t, rj.t], writes=[posb.t])
    op("dve", lambda e: e.tensor_copy(out=xI.ap(), in_=posb.ap()), reads=[posb.t], writes=[xI.t])
    pidx = self.sb("pidx", [128, NSL]); dma("sp", pidx.ap(), L["pidx_d"], writes=[pidx.t])
    OOB = 1.0e6
    ld = self.sb("ld", [128, NSL]); tmpj = self.sb("tmpj", [128, NSL])
    op("dve", lambda e: e.memset(ld.ap(), 1.0), writes=[ld.t])
    op("dve", lambda e: e.tensor_tensor(out=ld.ap()[:, 2:], in0=ej.ap()[:, 2:], in1=ej.ap()[:, 0:NSL - 2], op=ALU.not_equal), reads=[ej.t], writes=[ld.t])
    op("dve", lambda e: e.tensor_scalar(out=tmpj.ap(), in0=ej.ap(), scalar1=-OOB, scalar2=None, op0=ALU.add), reads=[ej.t], writes=[tmpj.t])
    op("dve", lambda e: e.tensor_tensor(out=tmpj.ap(), in0=tmpj.ap(), in1=ld.ap(), op=ALU.mult), reads=[tmpj.t, ld.t], writes=[tmpj.t])
    op("dve", lambda e: e.tensor_scalar(out=tmpj.ap(), in0=tmpj.ap(), scalar1=OOB, scalar2=None, op0=ALU.add), reads=[tmpj.t], writes=[tmpj.t])
    op("dve", lambda e: e.tensor_copy(out=eI.ap(), in_=tmpj.ap()), reads=[tmpj.t], writes=[eI.t])
    op("dve", lambda e: e.scalar_tensor_tensor(out=ej.ap(), in0=ej.ap(), scalar=128.0, in1=pidx.ap(), op0=ALU.mult, op1=ALU.add), reads=[ej.t, pidx.t], writes=[ej.t])
    op("dve", lambda e: e.tensor_scalar(out=tmpj.ap(), in0=ej.ap(), scalar1=-OOB, scalar2=None, op0=ALU.add), reads=[ej.t], writes=[tmpj.t])
    op("dve", lambda e: e.tensor_tensor(out=tmpj.ap(), in0=tmpj.ap(), in1=ld.ap(), op=ALU.mult), reads=[tmpj.t, ld.t], writes=[tmpj.t])
    op("dve", lambda e: e.tensor_scalar(out=tmpj.ap(), in0=tmpj.ap(), scalar1=OOB, scalar2=None, op0=ALU.add), reads=[tmpj.t], writes=[tmpj.t])
    op("dve", lambda e: e.tensor_copy(out=wI.ap(), in_=tmpj.ap()), reads=[tmpj.t], writes=[wI.t])
    op("dve", lambda e: e.tensor_scalar(out=adj.ap(), in0=cstart.ap(), scalar1=float(SLAB), scalar2=None, op0=ALU.mult), reads=[cstart.t], writes=[adj.t])
    op("dve", lambda e: e.tensor_tensor(out=adj.ap(), in0=adj.ap(), in1=self.iota_c.ap(), op=ALU.subtract), reads=[adj.t, self.iota_c.t], writes=[adj.t])
    eid2 = self.eidAll.ap().rearrange("p g k -> p (g k)")
    op("dve", lambda e: e.tensor_tensor(out=big.ap(), in0=iota_e.ap().unsqueeze(1).to_broadcast([128, 128, NEXP]),
                                        in1=eid2.unsqueeze(2).to_broadcast([128, 128, NEXP]), op=ALU.is_equal), reads=[iota_e.t, self.eidAll.t], writes=[big.t])
    op("dve", lambda e: e.tensor_tensor(out=big.ap(), in0=big.ap(), in1=adj.ap().unsqueeze(1).to_broadcast([128, 128, NEXP]), op=ALU.mult), reads=[big.t, adj.t], writes=[big.t])
    adjp = self.sb("adjp", [128, 128])
    op("dve", lambda e: e.reduce_sum(out=adjp.ap(), in_=big.ap(), axis=AX.X), reads=[big.t], writes=[adjp.t])
    op("dve", lambda e: e.tensor_tensor(out=adjp.ap(), in0=adjp.ap(), in1=self.dstfAll.ap().rearrange("p g k -> p (g k)"), op=ALU.add), reads=[adjp.t, self.dstfAll.t], writes=[adjp.t])
    op("dve", lambda e: e.tensor_copy(out=ydest.ap().rearrange("p g k -> p (g k)"), in_=adjp.ap()), reads=[adjp.t], writes=[ydest.t])
    if self.dbg:
        self.dump("eI", eI, [128, NSL], I32); self.dump("xI", xI, [128, NSL, NB], I32); self.dump("ydest", ydest, [128, 32, 4], I32)
    self.close_scope()
    self.ydest = ydest
    self.open_scope()
    wgu = [self.sb(f"wgu{i}", [128, 8, 2 * D], BF16) for i in range(2)]
    wd = [self.sb(f"wd{i}", [128, 8, D], BF16) for i in range(2)]
    bgbL = [self.sb(f"bgb{i}", [128, 2 * D]) for i in range(2)]
    ybuf = [self.sb(f"ybuf{i}", [128, 512]) for i in range(2)]
    actb = [self.sb(f"actb{i}", [128, D], BF16) for i in range(2)]
    xblk = [self.sb(f"xblk{i}", [128, NB, D], BF16) for i in range(2)]
    xT = self.sb("xTe", [128, 8, SLAB], BF16)
    aT = self.sb("aTe", [128, 8, SLAB], BF16)
    xg = self.sb("xg", [128, SLAB]); sgm = self.sb("sgm", [128, SLAB]); xl = self.sb("xl", [128, SLAB])
    yo = [self.sb(f"yo{i}", [128, D]) for i in range(2)]
    Yv = YeD.rearrange("(n p) d -> n p d", p=128)
    bdf = [self.sb(f"bdf{i}", [128, D]) for i in range(2)]
    Xg = XeD.rearrange("(r j) d -> r (j d)", j=4)
    Wg = L["wgu_d"].rearrange("e (p k) c -> (e p) (k c)", k=8)
    Wd = L["wd_d"].rearrange("e (p k) c -> (e p) (k c)", k=8)
    Bg = L["bguT_d"].rearrange("e p t c -> (e p) (t c)")
    IOA = bass.IndirectOffsetOnAxis
    bregs = {}

    def gather(dst_ap, src, idx_ap, reads, writes):
        bound = src.shape[0] - 1
        if bound not in bregs:
            bregs[bound] = nc.gpsimd.alloc_register(f"bound{bound}")
            nc.gpsimd.reg_mov(bregs[bound], bound)
        dma("pool", None, None, reads=reads, writes=writes,
            fn=lambda e: e.indirect_dma_start(out=dst_ap, out_offset=None, in_=src, in_offset=IOA(ap=idx_ap, axis=0),
                                              bounds_check=bregs[bound], oob_is_err=False))

    for j in range(NSL):
        wg, wdn, bd, bgb, xb = wgu[j % 2], wd[j % 2], bdf[j % 2], bgbL[j % 2], xblk[j % 2]
        for jb in range(NB):
            gather(xb.ap()[:, jb, :], XeD, xI.ap()[:, j, jb:jb + 1], [self.t_XeD, xI.t] + self.tl_Xe, [xb.t])
        gather(wg.ap().rearrange("p k c -> p (k c)"), Wg, wI.ap()[:, j:j + 1], [wI.t], [wg.t])
        gather(wdn.ap().rearrange("p k c -> p (k c)"), Wd, wI.ap()[:, j:j + 1], [wI.t], [wdn.t])
        gather(bgb.ap(), L["bgur_d"], eI.ap()[:, j:j + 1], [eI.t], [bgb.t])
        gather(bd.ap(), L["bd_d"], eI.ap()[:, j:j + 1], [eI.t], [bd.t])
        for jb in range(NB):
            pt = ps[jb % 2]
            ptv = pt.ap().bitcast(BF16)
            for k in range(8):
                op("pe", lambda e: e.transpose(out=ptv[:, k * 128:(k + 1) * 128], in_=xb.ap()[:, jb, k:D:8], identity=ident_b.ap()),
                   reads=[xb.t, ident_b.t], writes=[pt.t])
            if jb % 2 == 0:
                op("act", lambda e: e.copy(out=xT.ap()[:, :, jb * 128:(jb + 1) * 128], in_=ptv.rearrange("p (k t) -> p k t", t=128)), reads=[pt.t], writes=[xT.t])
            else:
                op("dve", lambda e: e.tensor_copy(out=xT.ap()[:, :, jb * 128:(jb + 1) * 128], in_=ptv.rearrange("p (k t) -> p k t", t=128)), reads=[pt.t], writes=[xT.t])
        gi = 0
        for jb in range(NB):
            ab = actb[jb % 2]
            for cc in range(4):
                pgu = ps[2 + gi % 4]
                yy = ybuf[gi % 2]
                gi += 1
                for k in range(8):
                    op("pe", lambda e: e.matmul(pgu.ap(), lhsT=xT.ap()[:, k, jb * 128:(jb + 1) * 128], rhs=wg.ap()[:, k, cc * 512:(cc + 1) * 512],
                                                start=(k == 0), stop=(k == 7)), reads=[wg.t, xT.t], writes=[pgu.t])
                op("dve", lambda e: e.tensor_tensor(out=yy.ap(), in0=pgu.ap(), in1=bgb.ap()[:, cc * 512:(cc + 1) * 512], op=ALU.add),
                   reads=[pgu.t, bgb.t], writes=[yy.t])
                op("dve", lambda e: e.tensor_scalar(out=xg.ap(), in0=yy.ap()[:, 0:512:2], scalar1=7.0, scalar2=None, op0=ALU.min), reads=[yy.t], writes=[xg.t])
                op("act", lambda e: e.activation(out=sgm.ap(), in_=xg.ap(), func=AF.Sigmoid, scale=1.702), reads=[xg.t], writes=[sgm.t])
                op("dve", lambda e: e.tensor_scalar(out=xl.ap(), in0=yy.ap()[:, 1:512:2], scalar1=7.0, scalar2=-7.0, op0=ALU.min, op1=ALU.max), reads=[yy.t], writes=[xl.t])
                op("dve", lambda e: e.tensor_tensor(out=xg.ap(), in0=xg.ap(), in1=sgm.ap(), op=ALU.mult), reads=[xg.t, sgm.t], writes=[xg.t])
                op("dve", lambda e: e.scalar_tensor_tensor(out=ab.ap()[:, cc * 256:(cc + 1) * 256], in0=xl.ap(), scalar=1.0, in1=xg.ap(), op0=ALU.add, op1=ALU.mult),
                   reads=[xg.t, xl.t], writes=[ab.t])
            pt = ps[jb % 2]
            ptv = pt.ap().bitcast(BF16)
            for k in range(8):
                op("pe", lambda e: e.transpose(out=ptv[:, k * 128:(k + 1) * 128], in_=ab.ap()[:, k:D:8], identity=ident_b.ap()),
                   reads=[ab.t, ident_b.t], writes=[pt.t])
            op("act", lambda e: e.copy(out=aT.ap()[:, :, jb * 128:(jb + 1) * 128], in_=ptv.rearrange("p (k t) -> p k t", t=128)), reads=[pt.t], writes=[aT.t])
        for jb in range(NB):
            y = yo[jb % 2]
            py = (ps[6], ps[7])
            for half in range(2):
                for k in range(8):
                    op("pe", lambda e: e.matmul(py[half].ap(), lhsT=aT.ap()[:, k, jb * 128:(jb + 1) * 128], rhs=wdn.ap()[:, k, half * 512:(half + 1) * 512],
                                                start=(k == 0), stop=(k == 7)), reads=[aT.t, wdn.t], writes=[py[half].t])
                op("dve", lambda e: e.tensor_tensor(out=y.ap()[:, half * 512:(half + 1) * 512], in0=py[half].ap(), in1=bd.ap()[:, half * 512:(half + 1) * 512], op=ALU.add),
                   reads=[py[half].t, bd.t], writes=[y.t])
            tys = T("Yst"); self.tl_Ye.append(tys)
            dma("sp", Yv[j * NB + jb], y.ap(), reads=[y.t], writes=[tys])
    self.close_scope()
    if self.dbg:
        for nm, src, n, tt, dt in (("Xe0", XeD, 1024, self.tl_Xe, BF16), ("Ye0", YeD, 1024, self.tl_Ye, F32), ("x1all", x1D, BL * NLAT, self.tl_x1, F32)):
            o = self.dout("dbg_" + nm, [n, D], dt); t = T("dbg_" + nm)
            S.dma("sp", o, src[0:n, :], reads=tt, writes=[t]); self.dbg_outs[nm] = t
        self.dump("destAll2", self.destAll, [128, 32, 4], I32)
        self.dump("probAll2", self.probAll, [128, 32, 4])
    self.open_scope()
    gfin = self.sb("gfin", [128, D]); dma("sp", gfin.ap(), L["gfin_d"][0, :].partition_broadcast(128), writes=[gfin.t])
    mod5 = self.sb("mod5", [128, D])
    x1 = [self.sb(f"x1_{i}", [128, D]) for i in range(2)]
    yk = [self.sb(f"yk{i}", [128, D]) for i in range(4)]
    acc = self.sb("acc", [128, D]); sq = self.sb("sqF", [128, D]); st = self.sb("stF", [128, 4])
    ob = [self.sb(f"ob{i}", [128, D]) for i in range(2)]
    destAll, probAll = self.destAll, self.probAll
    out_d = L["out_d"]
    for gt in range(32):
        b, i = gt // 16, gt % 16
        if i == 0:
            dma("sp", mod5.ap(), modD[b, 5 * D:6 * D].partition_broadcast(128), reads=[self.t_modD], writes=[mod5.t])
        r0 = gt * 128
        xb, o = x1[gt % 2], ob[gt % 2]
        dma("sp", xb.ap(), x1D[r0:r0 + 128, :], reads=[self.tl_x1[gt]], writes=[xb.t])
        for k in range(4):
            dma("pool", None, None, reads=self.tl_Ye + [self.ydest.t], writes=[yk[k].t],
                fn=lambda e: e.indirect_dma_start(out=yk[k].ap(), out_offset=None, in_=YeD,
                                                  in_offset=bass.IndirectOffsetOnAxis(ap=self.ydest.ap()[:, gt, k:k + 1], axis=0)))
        op("dve", lambda e: e.tensor_scalar(out=acc.ap(), in0=yk[0].ap(), scalar1=probAll.ap()[:, gt, 0:1], scalar2=None, op0=ALU.mult),
           reads=[yk[0].t, probAll.t], writes=[acc.t])
        for k in range(1, 4):
            op("dve", lambda e: e.scalar_tensor_tensor(out=acc.ap(), in0=yk[k].ap(), scalar=probAll.ap()[:, gt, k:k + 1], in1=acc.ap(), op0=ALU.mult, op1=ALU.add),
               reads=[yk[k].t, probAll.t, acc.t], writes=[acc.t])
        op("dve", lambda e: e.tensor_tensor(out=acc.ap(), in0=acc.ap(), in1=mod5.ap(), op=ALU.mult), reads=[acc.t, mod5.t], writes=[acc.t])
        op("dve", lambda e: e.tensor_tensor(out=xb.ap(), in0=xb.ap(), in1=acc.ap(), op=ALU.add), reads=[acc.t, xb.t], writes=[xb.t])
        op("act", lambda e: e.activation(out=sq.ap(), in_=xb.ap(), func=AF.Square, accum_out=st.ap()[:, 0:1]), reads=[xb.t], writes=[sq.t, st.t])
        op("act", lambda e: e.activation(out=st.ap()[:, 1:2], in_=st.ap()[:, 0:1], func=AF.Ln, scale=1.0 / D, bias=EPS), reads=[st.t], writes=[st.t])
        op("act", lambda e: e.activation(out=st.ap()[:, 2:3], in_=st.ap()[:, 1:2], func=AF.Exp, scale=-0.5), reads=[st.t], writes=[st.t])
        op("dve", lambda e: e.scalar_tensor_tensor(out=o.ap(), in0=xb.ap(), scalar=st.ap()[:, 2:3], in1=gfin.ap(), op0=ALU.mult, op1=ALU.mult),
           reads=[xb.t, st.t, gfin.t], writes=[o.t])
        tos = T("outst"); self.tl_out.append(tos)
        dma("sp", out_d[r0:r0 + 128, :], o.ap(), reads=[o.t], writes=[tos])


Prog.batch = _batch
Prog.hgrn = _hgrn
Prog.merge_ffn_in = _merge_ffn_in
Prog.moe = _moe
```

```python
from contextlib import ExitStack
import numpy as np
import ml_dtypes
import concourse.bass as bass
import concourse.mybir as mybir
from concourse.bass_utils import run_bass_kernel_spmd

F32 = mybir.dt.float32
BF16 = mybir.dt.bfloat16
I32 = mybir.dt.int32
U32 = mybir.dt.uint32
AF = mybir.ActivationFunctionType
ALU = mybir.AluOpType
AX = mybir.AxisListType

NCORES = 8
BL = 2
NLAT = 2048
NCTX = 256
NTOK = NLAT + NCTX
D = 1024
DIN = 8256
EPS = 1e-6
NEXP = 32
HC = 32
NCH = 128 // HC
CAP = 4096
SLAB = 256
NB = SLAB // 128
NSL = BL * NLAT * 4 // SLAB + NEXP


class T:
    __slots__ = ("name", "w", "r")

    def __init__(self, name=""):
        self.name = name
        self.w = None
        self.r = {}


class Sched:
    def __init__(self, nc, n_dma_sems=16):
        self.nc = nc
        self.eng = {}
        for nm, h in (("pe", nc.tensor), ("act", nc.scalar), ("dve", nc.vector), ("pool", nc.gpsimd), ("sp", nc.sync)):
            sem = nc.alloc_semaphore(f"s_{nm}")
            self.eng[nm] = dict(h=h, sem=sem, cnt=0, seen={}, name=nm)
        self.dq = {}
        for nm in ("sp", "pool", "act"):
            sems = [nc.alloc_semaphore(f"d_{nm}{i}") for i in range(n_dma_sems)]
            self.dq[nm] = dict(sems=sems, tgt=[0] * n_dma_sems, i=0)
        self.nwaits = 0
        self.nops = 0

    def _wait(self, E, deps, skip_same=False):
        best = {}
        for d in deps:
            if d is None:
                continue
            sem, cnt = d
            if skip_same and sem is E["sem"]:
                continue
            k = id(sem)
            if k not in best or best[k][1] < cnt:
                best[k] = (sem, cnt)
        for k, (sem, cnt) in best.items():
            if E["seen"].get(k, 0) >= cnt:
                continue
            E["h"].wait_ge(sem, cnt)
            E["seen"][k] = cnt
            self.nwaits += 1

    @staticmethod
    def _deps(reads, writes):
        deps = []
        for t in reads:
            deps.append(t.w)
        for t in writes:
            deps.append(t.w)
            deps.extend(t.r.values())
        return deps

    @staticmethod
    def _commit(tok, reads, writes):
        for t in writes:
            t.w = tok
            t.r = {}
        for t in reads:
            if t.w is not tok:
                t.r[id(tok[0])] = tok

    def op(self, e, fn, reads=(), writes=()):
        E = self.eng[e]
        self._wait(E, self._deps(reads, writes), skip_same=(e == "pe"))
        ins = fn(E["h"])
        E["cnt"] += 1
        ins.then_inc(E["sem"], 1)
        self.nops += 1
        self._commit((E["sem"], E["cnt"]), reads, writes)
        return ins

    def dma(self, q, out, in_, reads=(), writes=(), fn=None, **kw):
        E = self.eng[q]
        Q = self.dq[q]
        i = Q["i"] % len(Q["sems"])
        Q["i"] += 1
        sem = Q["sems"][i]
        deps = self._deps(reads, writes)
        if Q["tgt"][i] > 0:
            deps.append((sem, Q["tgt"][i]))
        self._wait(E, deps)
        if fn is not None:
            ins = fn(E["h"])
        else:
            ins = E["h"].dma_start(out=out, in_=in_, **kw)
        Q["tgt"][i] += 16
        ins.then_inc(sem, 16)
        self.nops += 1
        self._commit((sem, Q["tgt"][i]), reads, writes)
        return ins

    def barrier(self):
        toks = [(E["sem"], E["cnt"]) for E in self.eng.values() if E["cnt"] > 0]
        for Q in self.dq.values():
            for s, t in zip(Q["sems"], Q["tgt"]):
                if t > 0:
                    toks.append((s, t))
        for E in self.eng.values():
            self._wait(E, toks)

    def finish(self, tiles):
        self._wait(self.eng["sp"], [t.w for t in tiles])


class Buf:
    def __init__(self, h, name):
        self.h = h
        self.t = T(name)

    def ap(self):
        return self.h.ap()


def host_consts():
    c = {}
    c["ident_f"] = np.eye(128, dtype=np.float32)
    rows = NLAT // 64
    row = np.repeat(np.arange(rows), 64).astype(np.float32)
    col = np.tile(np.arange(64), rows).astype(np.float32)
    inv = (np.float32(10000.0) ** (-np.arange(16, dtype=np.float32) / np.float32(16))).astype(np.float32)
    ang = np.concatenate([row[:, None] * inv, col[:, None] * inv], axis=-1).astype(np.float32)
    cos = np.cos(ang).astype(np.float32).T
    sin = np.sin(ang).astype(np.float32).T
    c["rope_cc"] = np.ascontiguousarray(np.concatenate([cos, cos], 0))
    c["rope_ss"] = np.ascontiguousarray(np.concatenate([sin, sin], 0))
    s = np.arange(128)[:, None]
    t = np.arange(128)[None, :]
    same = (s // HC) == (t // HC)
    tri = np.stack([
        same & (s <= t),
        same & (s > t),
        same & (s >= t),
        same & (s < t),
    ]).astype(np.float32)
    c["tri"] = np.ascontiguousarray(tri.transpose(1, 0, 2))
    ind = np.zeros((128, NCH), np.float32)
    for cix in range(NCH):
        ind[cix * HC:(cix + 1) * HC, cix] = 1
    c["chunk_ind"] = ind
    c["ustrict"] = (s < t).astype(np.float32)
    c["iota_e"] = np.tile(np.arange(NEXP, dtype=np.float32)[None, :], (128, 1))
    c["iota_cap"] = np.ascontiguousarray(c["iota_e"] * CAP)
    c["jota"] = np.tile(np.arange(NSL, dtype=np.float32)[None, :], (128, 1))
    c["pidx"] = np.tile(np.arange(128, dtype=np.float32)[:, None], (1, NSL))
    c["off4"] = (np.arange(128, dtype=np.float32)[:, None] + 128.0 * np.arange(NB, dtype=np.float32)[None, :])
    return c


C_QA, C_KV, C_KR, C_HQ, C_FF, C_FB, C_HI, C_HG, C_MM, C_MH = 0, 768, 1024, 1088, 2112, 3136, 4160, 5184, 6208, 7232


class Prog:
    def __init__(self, stop_after=None, dbg=False):
        self.stop_after = stop_after
        self.dbg = dbg
        nc = self.nc = bass.Bass("TRN2", target_bir_lowering=False)
        self.S = Sched(nc)
        self.inp = {}
        self.dbg_outs = {}
        self.psn = 0
        self.scopes = []

    def din(self, name, shape, dt=F32):
        ap = self.nc.dram_tensor(name, list(shape), dt, kind="ExternalInput").ap()
        self.inp[name] = ap
        return ap

    def dscr(self, name, shape, dt=F32):
        return self.nc.dram_tensor(name, list(shape), dt, kind="Internal").ap()

    def dout(self, name, shape, dt=F32):
        return self.nc.dram_tensor(name, list(shape), dt, kind="ExternalOutput").ap()

    def sb(self, name, shape, dt=F32):
        self.uid = getattr(self, "uid", 0) + 1
        nm = f"s_{name}_{self.uid}"
        if self.scopes:
            h = self.scopes[-1].enter_context(self.nc.sbuf_tensor(nm, list(shape), dt))
        else:
            h = self.nc.alloc_sbuf_tensor(nm, list(shape), dt)
        return Buf(h, name)

    def open_scope(self):
        self.scopes.append(ExitStack())

    def close_scope(self):
        self.S.barrier()
        self.scopes.pop().close()

    def dump(self, name, buf, shape, dt=F32):
        if not self.dbg:
            return
        o = self.dout("dbg_" + name, shape, dt)
        t = T("dbg_" + name)
        self.S.dma("sp", o, buf.ap(), reads=[buf.t], writes=[t])
        self.dbg_outs[name] = t

    def build(self):
        nc, S = self.nc, self.S
        op, dma = S.op, S.dma
        x_d = self.din("x", [BL * NLAT, D])
        ctx_d = self.din("ctx", [BL * NCTX, D])
        cvec_d = self.din("cvec", [128, 8, 3])
        wmod_d = self.din("w_mod", [D, 6 * D])
        bmodT_d = self.din("b_modT", [128, 48])
        gmixT_d = self.din("g_mixT", [128, 8])
        win_d = self.din("w_in", [D, DIN])
        gqT_d = self.din("g_qT", [128, 6])
        wqb_d = self.din("w_q_b", [768, 1536])
        gkvT_d = self.din("g_kvT", [128, 2])
        wkvb_d = self.din("w_kv_b", [256, 2048])
        lbl_d = self.din("lb_logits", [2, 2, D])
        ghgT_d = self.din("g_hgT", [128, 1])
        wout_d = self.din("w_out", [D, D])
        gffn_d = self.din("g_ffn", [1, D])
        wr_d = self.din("w_router", [D, NEXP])
        br_d = self.din("b_router", [1, NEXP])
        wgu_d = self.din("w_gate_up", [NEXP, D, 2 * D])
        bguT_d = self.din("b_guT", [NEXP, 128, 2, 8])
        wd_d = self.din("w_down", [NEXP, D, D])
        bd_d = self.din("b_down", [NEXP, D])
        gfin_d = self.din("g_fin", [1, D])
        identf_d = self.din("ident_f", [128, 128])
        cc_d = self.din("rope_cc", [64, NLAT])
        ss_d = self.din("rope_ss", [64, NLAT])
        tri_d = self.din("tri", [128, 4, 128])
        cind_d = self.din("chunk_ind", [128, NCH])
        ustr_d = self.din("ustrict", [128, 128])
        iotae_d = self.din("iota_e", [128, NEXP])
        iotac_d = self.din("iota_cap", [128, NEXP])
        jota_d = self.din("jota", [128, NSL])
        pidx_d = self.din("pidx", [128, NSL])
        off4_d = self.din("off4", [128, NB])
        out_d = self.dout("out", [BL * NLAT, D])
        self.t_out = T("out")
        modD = self.dscr("modD", [3, 6 * D])
        ymlaD = self.dscr("ymlaD", [BL, 128, 8, NLAT], BF16)
        self.t_ymlaD = T("ymlaD")
        yhD = self.dscr("yhD", [BL, 128, 8, NLAT], BF16)
        self.t_yhD = T("yhD")
        x1D = self.dscr("x1D", [BL * NLAT, D])
        self.t_x1D = T("x1D")
        XeD = self.dscr("XeD", [NEXP * CAP, D], BF16)
        self.t_XeD = T("XeD")
        YeD = self.dscr("YeD", [NSL * SLAB, D])
        self.t_YeD = T("YeD")
        self.tl_Xe, self.tl_Ye, self.tl_out = [], [], []
        self.tl_x1 = [T(f"x1D{i}") for i in range(32)]

        ident_f = self.sb("ident_f_sb", [128, 128])
        ident_b = self.sb("ident_b", [128, 128], BF16)
        ones_f = self.sb("ones_f", [128, 128])
        ones_b = self.sb("ones_b", [128, 128], BF16)
        dma("sp", ident_f.ap(), identf_d, writes=[ident_f.t])
        op("dve", lambda e: e.tensor_copy(out=ident_b.ap(), in_=ident_f.ap()), reads=[ident_f.t], writes=[ident_b.t])
        op("pool", lambda e: e.memset(ones_f.ap(), 1.0), writes=[ones_f.t])
        op("pool", lambda e: e.memset(ones_b.ap(), 1.0), writes=[ones_b.t])
        self.ident_f, self.ident_b, self.ones_f, self.ones_b = ident_f, ident_b, ones_f, ones_b
        self.ps = [Buf(nc.alloc_psum_tensor(f"ps{i}", [128, 512], F32), f"ps{i}") for i in range(8)]

        bmodT = self.sb("bmodT", [128, 48])
        modT = self.sb("modT", [128, 48, 3])
        gmixT = self.sb("gmixT", [128, 8])
        G1 = self.sb("G1", [128, 8, 3])
        self.open_scope()
        zt = self.sb("zeros", [128, 2048], BF16)
        op("pool", lambda e: e.memset(zt.ap(), 0.0), writes=[zt.t])
        zsem = nc.alloc_semaphore("zfill")
        Xv = XeD.rearrange("(a p j) d -> a p (j d)", p=128, j=2)
        S._wait(S.eng["act"], [zt.t.w])
        NZ = SLAB // 256
        for a in range(NZ):
            nc.scalar.dma_start(out=Xv[a], in_=zt.ap()).then_inc(zsem, 16)
        self.t_XeD.w = (zsem, 16 * NZ)
        cvec = self.sb("cvec", [128, 8, 3])
        sig = self.sb("csig", [128, 8, 3])
        scb = self.sb("scb", [128, 8, 3], BF16)
        dma("sp", cvec.ap(), cvec_d, writes=[cvec.t])
        op("act", lambda e: e.activation(out=sig.ap(), in_=cvec.ap(), func=AF.Sigmoid), reads=[cvec.t], writes=[sig.t])
        op("dve", lambda e: e.tensor_mul(out=scb.ap(), in0=cvec.ap(), in1=sig.ap()), reads=[cvec.t, sig.t], writes=[scb.t])
        dma("sp", bmodT.ap(), bmodT_d, writes=[bmodT.t])
        wm = [self.sb(f"wmod{i}", [128, 8, 1536], BF16) for i in range(2)]
        pm = self.ps[0]
        wmod_v = wmod_d.rearrange("(k p) c -> p k c", p=128)
        for pc in range(4):
            w = wm[pc % 2]
            dma("pool", w.ap(), wmod_v[:, :, pc * 1536:(pc + 1) * 1536], writes=[w.t])
            for jj in range(12):
                j = pc * 12 + jj
                for k in range(8):
                    op("pe", lambda e: e.matmul(pm.ap()[:, j * 3:(j + 1) * 3], lhsT=w.ap()[:, k, jj * 128:(jj + 1) * 128],
                                                rhs=scb.ap()[:, k, :], start=(k == 0), stop=(k == 7)),
                       reads=[w.t, scb.t], writes=[pm.t])
        op("dve", lambda e: e.tensor_tensor(out=modT.ap(), in0=pm.ap()[:, 0:144].rearrange("p (j v) -> p j v", v=3),
                                            in1=bmodT.ap().unsqueeze(2).to_broadcast([128, 48, 3]), op=ALU.add),
           reads=[pm.t, bmodT.t], writes=[modT.t])
        t_modD = T("modD")
        with nc.allow_non_contiguous_dma(reason="one-time tiny modulation transpose"):
            for v in range(3):
                dma("sp", modD[v, :].rearrange("(j p) -> p j", p=128), modT.ap()[:, :, v], reads=[modT.t], writes=[t_modD])
        self.modD, self.t_modD = modD, t_modD
        dma("sp", gmixT.ap(), gmixT_d, writes=[gmixT.t])
        op("dve", lambda e: e.scalar_tensor_tensor(out=G1.ap(), in0=modT.ap()[:, 8:16, :], scalar=1.0,
                                                   in1=gmixT.ap().unsqueeze(2).to_broadcast([128, 8, 3]), op0=ALU.add, op1=ALU.mult),
           reads=[modT.t, gmixT.t], writes=[G1.t])
        self.dump("modT", modT, [128, 48, 3])
        for E in S.eng.values():
            S._wait(E, [self.t_XeD.w])
        self.close_scope()
        if self.stop_after == "P0":
            return self.finalize()

        gqT = self.sb("gqT", [128, 6]); dma("sp", gqT.ap(), gqT_d, writes=[gqT.t])
        gkvT = self.sb("gkvT", [128, 2]); dma("sp", gkvT.ap(), gkvT_d, writes=[gkvT.t])
        ghgT = self.sb("ghgT", [128, 1]); dma("sp", ghgT.ap(), ghgT_d, writes=[ghgT.t])

        self.base_bc = self.sb("base_bc", [128, NEXP])
        op("pool", lambda e: e.memset(self.base_bc.ap(), 0.0), writes=[self.base_bc.t])
        self.destAll = self.sb("destAll", [128, 32, 4], I32)
        self.probAll = self.sb("probAll", [128, 32, 4])
        self.dstfAll = self.sb("dstfAll", [128, 32, 4])
        self.eidAll = self.sb("eidAll", [128, 32, 4])
        self.ustr = self.sb("ustr", [128, 128], BF16)
        tmpu = self.sb("tmpu", [128, 128])
        dma("sp", tmpu.ap(), ustr_d, writes=[tmpu.t])
        op("dve", lambda e: e.tensor_copy(out=self.ustr.ap(), in_=tmpu.ap()), reads=[tmpu.t], writes=[self.ustr.t])
        self.iota_e = self.sb("iota_e", [128, NEXP]); dma("sp", self.iota_e.ap(), iotae_d, writes=[self.iota_e.t])
        self.iota_c = self.sb("iota_c", [128, NEXP]); dma("sp", self.iota_c.ap(), iotac_d, writes=[self.iota_c.t])
        for b in range(BL):
            self.batch(b, locals())
            if self.stop_after is not None and self.stop_after.startswith("B0"):
                return self.finalize()
        self.moe(locals())
        return self.finalize()

    def finalize(self):
        S = self.S
        tl = list(self.dbg_outs.values())
        tl.extend(self.tl_out)
        S.finish(tl)
        return self.nc


def make_in_maps(inputs):
    f = lambda a: np.ascontiguousarray(np.asarray(a, dtype=np.float32))
    x, c, ctx, c_ctx = f(inputs["x"]), f(inputs["c"]), f(inputs["ctx"]), f(inputs["c_ctx"])
    consts = host_consts()
    featT = lambda v, nk: np.ascontiguousarray(v.reshape(nk, 128).T)
    shared = {
        "w_mod": f(inputs["w_mod"][0]),
        "b_modT": featT(f(inputs["b_mod"][0]), 48),
        "g_mixT": featT(f(inputs["norm_mix_g"][0]), 8),
        "w_in": f(inputs["w_in"][0]),
        "g_qT": featT(f(inputs["mla_q_norm_g"][0]), 6),
        "w_q_b": f(inputs["w_q_b"][0]),
        "g_kvT": featT(f(inputs["mla_kv_norm_g"][0]), 2),
        "w_kv_b": f(inputs["w_kv_b"][0]),
        "lb_logits": f(inputs["hg_lb_logits"]),
        "g_hgT": featT(f(inputs["hg_norm_g"][0]), 1),
        "w_out": f(inputs["w_out"][0]),
        "g_ffn": f(inputs["norm_ffn_g"][0]).reshape(1, D),
        "w_router": f(inputs["w_router"][0]),
        "b_router": f(inputs["b_router"][0]).reshape(1, NEXP),
        "w_gate_up": f(inputs["w_gate_up"][0]),
        "b_guT": np.ascontiguousarray(f(inputs["b_gate_up"][0]).reshape(NEXP, 128, 8, 2).transpose(0, 1, 3, 2)),
        "w_down": f(inputs["w_down"][0]),
        "b_down": f(inputs["b_down"][0]),
        "g_fin": f(inputs["final_norm_g"]).reshape(1, D),
    }
    shared.update(consts)
    maps = []
    for core in range(NCORES):
        b0 = core * BL
        cv = np.stack([c[b0], c[b0 + 1], c_ctx], axis=-1)
        m = dict(shared)
        m["x"] = np.ascontiguousarray(x[b0:b0 + BL].reshape(BL * NLAT, D))
        m["ctx"] = np.ascontiguousarray(ctx[b0:b0 + BL].reshape(BL * NCTX, D))
        m["cvec"] = np.ascontiguousarray(cv.reshape(8, 128, 3).transpose(1, 0, 2))
        maps.append(m)
    return maps


def kernel(**inputs):
    prog = Prog()
    nc = prog.build()
    in_maps = make_in_maps(inputs)
    in_maps = [{k: v for k, v in m.items() if k in prog.inp} for m in in_maps]
    res = run_bass_kernel_spmd(nc, in_maps, core_ids=list(range(NCORES)))
    outs = [np.asarray(r["out"], dtype=np.float32).reshape(BL, NLAT, D) for r in res.results]
    return np.concatenate(outs, axis=0)


def _batch(self, b, L):
    nc, S = self.nc, self.S
    op, dma = S.op, S.dma
    ps = self.ps
    ident_f, ident_b, ones_f, ones_b = self.ident_f, self.ident_b, self.ones_f, self.ones_b
    G1, modT = L["G1"], L["modT"]
    x_d, ctx_d, win_d = L["x_d"], L["ctx_d"], L["win_d"]
    win_v = win_d.rearrange("(k p) c -> p k c", p=128)
    first = (b == 0)

    def psum(i):
        return ps[i % 8]

    self.open_scope()
    if True:
        self.hT = self.sb("hT", [128, 8, NTOK], BF16)
        self.hT_t = [T(f"hT{i}") for i in range(18)]
        self.open_scope()
        self.xt = [self.sb(f"xt{i}", [128, D]) for i in range(2)]
        self.sq = self.sb("sqscr", [128, D])
        self.st = [self.sb(f"st{i}", [128, 4]) for i in range(2)]
    hT, hT_t, xt, sq, st = self.hT, self.hT_t, self.xt, self.sq, self.st
    for i in range(18):
        xb, sb_ = xt[i % 2], st[i % 2]
        if i < 16:
            src, v = x_d[b * NLAT + i * 128: b * NLAT + (i + 1) * 128, :], b
        else:
            src, v = ctx_d[b * NCTX + (i - 16) * 128: b * NCTX + (i - 15) * 128, :], 2
        dma("sp", xb.ap(), src, writes=[xb.t])
        op("act", lambda e: e.activation(out=sq.ap(), in_=xb.ap(), func=AF.Square, accum_out=sb_.ap()[:, 0:1]),
           reads=[xb.t], writes=[sq.t, sb_.t])
        op("act", lambda e: e.activation(out=sb_.ap()[:, 1:2], in_=sb_.ap()[:, 0:1], func=AF.Ln, scale=1.0 / D, bias=EPS),
           reads=[sb_.t], writes=[sb_.t])
        op("act", lambda e: e.activation(out=sb_.ap()[:, 2:3], in_=sb_.ap()[:, 1:2], func=AF.Exp, scale=-0.5),
           reads=[sb_.t], writes=[sb_.t])
        op("dve", lambda e: e.tensor_scalar(out=xb.ap(), in0=xb.ap(), scalar1=sb_.ap()[:, 2:3], scalar2=None, op0=ALU.mult),
           reads=[sb_.t, xb.t], writes=[xb.t])
        for half in range(2):
            p = psum(i * 2 + half)
            for kk in range(4):
                k = half * 4 + kk
                op("pe", lambda e: e.transpose(out=p.ap()[:, kk * 128:(kk + 1) * 128], in_=xb.ap()[:, k * 128:(k + 1) * 128],
                                               identity=ident_f.ap()), reads=[xb.t, ident_f.t], writes=[p.t])
            for kk in range(4):
                k = half * 4 + kk
                eng = "act" if kk % 2 == 0 else "dve"
                if eng == "act":
                    op("act", lambda e: e.activation(out=hT.ap()[:, k, i * 128:(i + 1) * 128], in_=p.ap()[:, kk * 128:(kk + 1) * 128],
                                                     func=AF.Identity, scale=G1.ap()[:, k, v:v + 1], bias=modT.ap()[:, k, v:v + 1]),
                       reads=[p.t, G1.t, modT.t], writes=[hT_t[i]])
                else:
                    op("dve", lambda e: e.tensor_scalar(out=hT.ap()[:, k, i * 128:(i + 1) * 128], in0=p.ap()[:, kk * 128:(kk + 1) * 128],
                                                        scalar1=G1.ap()[:, k, v:v + 1], scalar2=modT.ap()[:, k, v:v + 1],
                                                        op0=ALU.mult, op1=ALU.add),
                       reads=[p.t, G1.t, modT.t], writes=[hT_t[i]])
    if self.dbg and first:
        allh = T("allh")
        o = self.dout("dbg_hT", [128, 8, NTOK], BF16)
        S.dma("sp", o, hT.ap(), reads=hT_t, writes=[allh])
        self.dbg_outs["hT"] = allh
    if self.stop_after == "B0P1":
        return
    self.close_scope()

    self.open_scope()
    if True:
        self.wA = self.sb("wA", [128, 8, 1088], BF16)
        self.wAs = self.sb("wAs", [128, 8, 64], BF16)
        dma("pool", self.wA.ap(), win_v[:, :, 0:1088], writes=[self.wA.t])
        op("act", lambda e: e.mul(out=self.wAs.ap()[:, :, 0:32], in_=self.wA.ap()[:, :, 1056:1088], mul=-1.0),
           reads=[self.wA.t], writes=[self.wAs.t])
        op("act", lambda e: e.copy(out=self.wAs.ap()[:, :, 32:64], in_=self.wA.ap()[:, :, 1024:1056]),
           reads=[self.wA.t], writes=[self.wAs.t])
        self.qaT = self.sb("qaT", [128, 6, NLAT], BF16)
        self.ckvT = self.sb("ckvT", [128, 2, NTOK], BF16)
        self.krT = self.sb("krT", [64, NTOK], BF16)
        self.cc = self.sb("cc", [64, NLAT]); dma("sp", self.cc.ap(), L["cc_d"], writes=[self.cc.t])
        self.ss = self.sb("ss", [64, NLAT]); dma("sp", self.ss.ap(), L["ss_d"], writes=[self.ss.t])
        self.sqf = [self.sb(f"sqf{i}", [128, 512]) for i in range(2)]
        self.rstd_bc = self.sb("rstd_bc", [128, 512])
        self.rtmp = [self.sb(f"rtmp{i}", [64, 512]) for i in range(2)]
        self.wkvb = self.sb("wkvb", [128, 2, 2048], BF16)
        dma("pool", self.wkvb.ap(), L["wkvb_d"].rearrange("(k p) c -> p k c", p=128), writes=[self.wkvb.t])
    wA, wAs, qaT, ckvT, krT, cc, ss, sqf, rstd_bc, rtmp, wkvb = (self.wA, self.wAs, self.qaT, self.ckvT, self.krT, self.cc,
                                                                 self.ss, self.sqf, self.rstd_bc, self.rtmp, self.wkvb)
    gqT, gkvT = L["gqT"], L["gkvT"]
    slabs = [(s * 512, 512) for s in range(4)] + [(NLAT, NCTX)]
    hdeps = lambda c0, n: hT_t[c0 // 128:(c0 + n) // 128]

    def norm_group(cols0, nch, dst, g, c0, n, nfeat, pbase):
        pbs = [psum(pbase + m) for m in range(nch)]
        pss = psum(pbase + nch)
        for m in range(nch):
            for k in range(8):
                op("pe", lambda e: e.matmul(pbs[m].ap()[:, 0:n], lhsT=wA.ap()[:, k, cols0 + m * 128: cols0 + (m + 1) * 128],
                                            rhs=hT.ap()[:, k, c0:c0 + n], start=(k == 0), stop=(k == 7)),
                   reads=[wA.t] + hdeps(c0, n), writes=[pbs[m].t])
        for m in range(nch):
            q = sqf[m % 2]
            op("act", lambda e: e.activation(out=q.ap()[:, 0:n], in_=pbs[m].ap()[:, 0:n], func=AF.Square), reads=[pbs[m].t], writes=[q.t])
            op("pe", lambda e: e.matmul(pss.ap()[:, 0:n], lhsT=ones_f.ap(), rhs=q.ap()[:, 0:n], start=(m == 0), stop=(m == nch - 1)),
               reads=[ones_f.t, q.t], writes=[pss.t])
        op("act", lambda e: e.activation(out=rstd_bc.ap()[:, 0:n], in_=pss.ap()[:, 0:n], func=AF.Ln, scale=1.0 / nfeat, bias=EPS),
           reads=[pss.t], writes=[rstd_bc.t])
        op("act", lambda e: e.activation(out=rstd_bc.ap()[:, 0:n], in_=rstd_bc.ap()[:, 0:n], func=AF.Exp, scale=-0.5),
           reads=[rstd_bc.t], writes=[rstd_bc.t])
        for m in range(nch):
            op("dve", lambda e: e.scalar_tensor_tensor(out=dst.ap()[:, m, c0:c0 + n], in0=pbs[m].ap()[:, 0:n], scalar=g.ap()[:, m:m + 1],
                                                       in1=rstd_bc.ap()[:, 0:n], op0=ALU.mult, op1=ALU.mult),
               reads=[pbs[m].t, g.t, rstd_bc.t], writes=[dst.t])

    for (c0, n) in slabs:
        if c0 < NLAT:
            norm_group(C_QA, 6, qaT, gqT, c0, n, 768, 0)
        norm_group(C_KV, 2, ckvT, gkvT, c0, n, 256, 0)
        pk, pks = psum(3), psum(4)
        for k in range(8):
            op("pe", lambda e: e.matmul(pk.ap()[0:64, 0:n], lhsT=wA.ap()[:, k, C_KR:C_KR + 64], rhs=hT.ap()[:, k, c0:c0 + n],
                                        start=(k == 0), stop=(k == 7)), reads=[wA.t] + hdeps(c0, n), writes=[pk.t])
        if c0 < NLAT:
            for k in range(8):
                op("pe", lambda e: e.matmul(pks.ap()[0:64, 0:n], lhsT=wAs.ap()[:, k, :], rhs=hT.ap()[:, k, c0:c0 + n],
                                            start=(k == 0), stop=(k == 7)), reads=[wAs.t] + hdeps(c0, n), writes=[pks.t])
            op("dve", lambda e: e.tensor_tensor(out=rtmp[0].ap()[:, 0:n], in0=pk.ap()[0:64, 0:n], in1=cc.ap()[:, c0:c0 + n], op=ALU.mult),
               reads=[pk.t, cc.t], writes=[rtmp[0].t])
            op("dve", lambda e: e.tensor_tensor(out=rtmp[1].ap()[:, 0:n], in0=pks.ap()[0:64, 0:n], in1=ss.ap()[:, c0:c0 + n], op=ALU.mult),
               reads=[pks.t, ss.t], writes=[rtmp[1].t])
            op("dve", lambda e: e.tensor_tensor(out=krT.ap()[:, c0:c0 + n], in0=rtmp[0].ap()[:, 0:n], in1=rtmp[1].ap()[:, 0:n], op=ALU.add),
               reads=[rtmp[0].t, rtmp[1].t], writes=[krT.t])
        else:
            op("act", lambda e: e.copy(out=krT.ap()[:, c0:c0 + n], in_=pk.ap()[0:64, 0:n]), reads=[pk.t], writes=[krT.t])
    if self.dbg and first:
        self.dump("qaT", qaT, [128, 6, NLAT], BF16)
        self.dump("ckvT", ckvT, [128, 2, NTOK], BF16)
        self.dump("krT", krT, [64, NTOK], BF16)
    if self.stop_after == "B0P2a":
        return

    if True:
        self.wq = [self.sb(f"wq{i}", [128, 6, 192], BF16) for i in range(2)]
        self.wqs = [self.sb(f"wqs{i}", [128, 6, 64], BF16) for i in range(2)]
        self.qT = self.sb("qT", [128, NLAT], BF16)
        self.qrT = self.sb("qrT", [64, NLAT], BF16)
        self.kT = self.sb("kT", [128, NTOK], BF16)
        self.vh = self.sb("vh", [128, 18, 128], BF16)
        self.PT = [self.sb(f"PT{i}", [128, 512], BF16) for i in range(4)]
        self.rs = self.sb("rs", [128, 512])
        self.yT = self.sb("yT", [128, 8, NLAT], BF16)
    wq, wqs, qT, qrT, kT, vh, PT, rs, yT = self.wq, self.wqs, self.qT, self.qrT, self.kT, self.vh, self.PT, self.rs, self.yT
    wqb_v = L["wqb_d"].rearrange("(k p) c -> p k c", p=128)
    scale = float(192 ** -0.5)
    for h in range(8):
        w, ws = wq[h % 2], wqs[h % 2]
        dma("pool", w.ap(), wqb_v[:, :, h * 192:(h + 1) * 192], writes=[w.t])
        op("act", lambda e: e.mul(out=ws.ap()[:, :, 0:32], in_=w.ap()[:, :, 160:192], mul=-1.0), reads=[w.t], writes=[ws.t])
        op("act", lambda e: e.copy(out=ws.ap()[:, :, 32:64], in_=w.ap()[:, :, 128:160]), reads=[w.t], writes=[ws.t])
        for s in range(4):
            c0 = s * 512
            pq, pr, prs = psum(0), psum(1), psum(2)
            for k in range(6):
                op("pe", lambda e: e.matmul(pq.ap(), lhsT=w.ap()[:, k, 0:128], rhs=qaT.ap()[:, k, c0:c0 + 512], start=(k == 0), stop=(k == 5)),
                   reads=[w.t, qaT.t], writes=[pq.t])
            for k in range(6):
                op("pe", lambda e: e.matmul(pr.ap()[0:64, :], lhsT=w.ap()[:, k, 128:192], rhs=qaT.ap()[:, k, c0:c0 + 512], start=(k == 0), stop=(k == 5)),
                   reads=[w.t, qaT.t], writes=[pr.t])
            for k in range(6):
                op("pe", lambda e: e.matmul(prs.ap()[0:64, :], lhsT=ws.ap()[:, k, :], rhs=qaT.ap()[:, k, c0:c0 + 512], start=(k == 0), stop=(k == 5)),
                   reads=[ws.t, qaT.t], writes=[prs.t])
            op("act", lambda e: e.copy(out=qT.ap()[:, c0:c0 + 512], in_=pq.ap()), reads=[pq.t], writes=[qT.t])
            op("dve", lambda e: e.tensor_tensor(out=rtmp[0].ap(), in0=pr.ap()[0:64, :], in1=cc.ap()[:, c0:c0 + 512], op=ALU.mult),
               reads=[pr.t, cc.t], writes=[rtmp[0].t])
            op("dve", lambda e: e.tensor_tensor(out=rtmp[1].ap(), in0=prs.ap()[0:64, :], in1=ss.ap()[:, c0:c0 + 512], op=ALU.mult),
               reads=[prs.t, ss.t], writes=[rtmp[1].t])
            op("dve", lambda e: e.tensor_tensor(out=qrT.ap()[:, c0:c0 + 512], in0=rtmp[0].ap(), in1=rtmp[1].ap(), op=ALU.add),
               reads=[rtmp[0].t, rtmp[1].t], writes=[qrT.t])
        for si, (c0, n) in enumerate(slabs):
            pk = psum(3 + si % 2)
            for k in range(2):
                op("pe", lambda e: e.matmul(pk.ap()[:, 0:n], lhsT=wkvb.ap()[:, k, h * 256:h * 256 + 128], rhs=ckvT.ap()[:, k, c0:c0 + n],
                                            start=(k == 0), stop=(k == 1)), reads=[wkvb.t, ckvT.t], writes=[pk.t])
            op("act", lambda e: e.copy(out=kT.ap()[:, c0:c0 + n], in_=pk.ap()[:, 0:n]), reads=[pk.t], writes=[kT.t])
        for g in range(5):
            pv = psum(5 + g % 2)
            nj = 4 if g < 4 else 2
            for jj in range(nj):
                j = g * 4 + jj
                for k in range(2):
                    op("pe", lambda e: e.matmul(pv.ap()[:, jj * 128:(jj + 1) * 128], lhsT=ckvT.ap()[:, k, j * 128:(j + 1) * 128],
                                                rhs=wkvb.ap()[:, k, h * 256 + 128:h * 256 + 256], start=(k == 0), stop=(k == 1)),
                       reads=[wkvb.t, ckvT.t], writes=[pv.t])
            op("dve", lambda e: e.tensor_copy(out=vh.ap()[:, g * 4:g * 4 + nj, :], in_=pv.ap()[:, 0:nj * 128].rearrange("p (j d) -> p j d", d=128)),
               reads=[pv.t], writes=[vh.t])
        for s in range(4):
            c0 = s * 512
            po, psm = psum(4 + s % 2), psum(6 + s % 2)

            def smm(kc):
                p = psum(kc % 4)
                op("pe", lambda e: e.matmul(p.ap(), lhsT=kT.ap()[:, kc * 128:(kc + 1) * 128], rhs=qT.ap()[:, c0:c0 + 512], start=True, stop=False),
                   reads=[kT.t, qT.t], writes=[p.t])
                op("pe", lambda e: e.matmul(p.ap(), lhsT=krT.ap()[:, kc * 128:(kc + 1) * 128], rhs=qrT.ap()[:, c0:c0 + 512], start=False, stop=True),
                   reads=[krT.t, qrT.t], writes=[p.t])
                pt = PT[kc % 4]
                op("act", lambda e: e.activation(out=pt.ap(), in_=p.ap(), func=AF.Exp, scale=scale), reads=[p.t], writes=[pt.t])

            def omm(kc):
                pt = PT[kc % 4]
                op("pe", lambda e: e.matmul(po.ap(), lhsT=vh.ap()[:, kc, :], rhs=pt.ap(), start=(kc == 0), stop=(kc == 17)),
                   reads=[vh.t, pt.t], writes=[po.t])
                op("pe", lambda e: e.matmul(psm.ap(), lhsT=ones_b.ap(), rhs=pt.ap(), start=(kc == 0), stop=(kc == 17)),
                   reads=[ones_b.t, pt.t], writes=[psm.t])

            smm(0); smm(1)
            for kc in range(18):
                if kc + 2 < 18:
                    smm(kc + 2)
                omm(kc)
            op("dve", lambda e: e.reciprocal(out=rs.ap(), in_=psm.ap()), reads=[psm.t], writes=[rs.t])
            op("dve", lambda e: e.tensor_tensor(out=yT.ap()[:, h, c0:c0 + 512], in0=po.ap(), in1=rs.ap(), op=ALU.mult),
               reads=[po.t, rs.t], writes=[yT.t])
    if self.dbg and first:
        self.dump("ymlaT", yT, [128, 8, NLAT], BF16)
    if self.stop_after == "B0P2":
        return
    ymlaD = L["ymlaD"]
    dma("sp", ymlaD[b], yT.ap(), reads=[yT.t], writes=[self.t_ymlaD])
    self.close_scope()
    self.hgrn(b, L)
    if self.stop_after == "B0P3":
        return
    self.merge_ffn_in(b, L)
    if self.stop_after == "B0P4":
        return
    self.close_scope()


def _hgrn(self, b, L):
    nc, S = self.nc, self.S
    op, dma = S.op, S.dma
    ps = self.ps
    ident_b, ones_f = self.ident_b, self.ones_f
    hT, hT_t = self.hT, self.hT_t
    win_v = L["win_d"].rearrange("(k p) c -> p k c", p=128)
    self.open_scope()
    wS = self.sb("wS", [128, 8, 4096], BF16)
    for pc in range(4):
        dma("pool", wS.ap()[:, :, pc * 1024:(pc + 1) * 1024], win_v[:, :, C_HQ + pc * 1024:C_HQ + (pc + 1) * 1024], writes=[wS.t])
    tri = self.sb("tri", [128, 4, 128]); dma("sp", tri.ap(), L["tri_d"], writes=[tri.t])
    cind = self.sb("cind", [128, NCH]); dma("sp", cind.ap(), L["cind_d"], writes=[cind.t])
    lb = [self.sb(f"lb{d}", [128, D]) for d in range(2)]
    lnoml = [self.sb(f"lnoml{d}", [128, D]) for d in range(2)]
    uL = [self.sb(f"u{d}", [128, D]) for d in range(2)]; L2L = [self.sb(f"L2{d}", [128, D]) for d in range(2)]
    tA_ = self.sb("tA", [128, D]); tB_ = self.sb("tB", [128, D])
    tAL = [tA_, tA_]; tBL = [tB_, tB_]
    u, L2, tA, tB = uL[0], L2L[0], tAL[0], tBL[0]
    lbl = L["lbl_d"]
    for d in range(2):
        dma("sp", u.ap(), lbl[d, 0, :].partition_broadcast(128), writes=[u.t])
        dma("sp", L2.ap(), lbl[d, 1, :].partition_broadcast(128), writes=[L2.t])
        op("dve", lambda e: e.tensor_tensor(out=tA.ap(), in0=L2.ap(), in1=u.ap(), op=ALU.subtract), reads=[u.t, L2.t], writes=[tA.t])
        op("act", lambda e: e.activation(out=tB.ap(), in_=tA.ap(), func=AF.Exp), reads=[tA.t], writes=[tB.t])
        op("act", lambda e: e.activation(out=tB.ap(), in_=tB.ap(), func=AF.Ln, bias=1.0), reads=[tB.t], writes=[tB.t])
        op("act", lambda e: e.activation(out=lb[d].ap(), in_=tB.ap(), func=AF.Exp, scale=-1.0), reads=[tB.t], writes=[lb[d].t])
        op("dve", lambda e: e.tensor_tensor(out=lnoml[d].ap(), in0=tA.ap(), in1=tB.ap(), op=ALU.subtract), reads=[tA.t, tB.t], writes=[lnoml[d].t])
    Kt = [self.sb(f"Kt{d}", [128, D], BF16) for d in range(2)]
    Kh = [self.sb(f"Kh{d}", [128, D], BF16) for d in range(2)]
    Qt = [self.sb(f"Qt{d}", [128, D], BF16) for d in range(2)]
    vt = [self.sb(f"vt{d}", [128, D], BF16) for d in range(2)]
    QT = [self.sb(f"QT{d}", [128, 8, 128], BF16) for d in range(2)]
    KT = [self.sb(f"KT{d}", [128, 8, 128], BF16) for d in range(2)]
    ATm = [self.sb(f"ATm{d}", [128, 8, 128], BF16) for d in range(2)]
    dec = [self.sb(f"dec{d}", [128, 8, NCH]) for d in range(2)]
    vt3 = [self.sb(f"vt3{d}", [128, D], BF16) for d in range(2)]
    Sf = [self.sb(f"Sf{d}", [128, 8, 128]) for d in range(2)]
    Sb = [self.sb(f"Sb{d}", [128, 8, 128], BF16) for d in range(2)]
    otL = [self.sb(f"ot{d}", [128, 8, 128]) for d in range(2)]
    o1 = self.sb("o1", [128, 8, 128])
    sqo = o1
    yh = self.sb("yh", [128, 8, 128], BF16)
    oD = self.dscr(f"oD{b}", [16, 128, 8, 128])
    t_oD = [T(f"oD{i}") for i in range(16)]
    yhD = L["yhD"]
    for d in range(2):
        op("pool", lambda e: e.memset(Sf[d].ap(), 0.0), writes=[Sf[d].t])
        op("pool", lambda e: e.memset(Sb[d].ap(), 0.0), writes=[Sb[d].t])
    ghgT = L["ghgT"]
    A2 = lambda i: (ps[i], ps[i + 1])

    def tokproj(tile, col0, pair):
        for half in range(2):
            p = pair[half]
            for k in range(8):
                op("pe", lambda e: e.matmul(p.ap(), lhsT=hT.ap()[:, k, tile * 128:(tile + 1) * 128],
                                            rhs=wS.ap()[:, k, col0 + half * 512: col0 + (half + 1) * 512], start=(k == 0), stop=(k == 7)),
                   reads=[hT_t[tile], wS.t], writes=[p.t])

    def two(fn, pair, reads, writes, eng):
        for half in range(2):
            p = pair[half]
            op(eng, lambda e: fn(e, p.ap(), slice(half * 512, (half + 1) * 512)), reads=[p.t] + reads, writes=writes)

    def prep(tile, d, need_o):
        pA, pB, pC, pD = A2(0), A2(2), A2(4), A2(6)
        u, L2, tA, tB = uL[d], L2L[d], tAL[d], tBL[d]
        ti_incl, ti_excl = (0, 1) if d == 0 else (2, 3)
        tokproj(tile, 1024 * (1 + d), pA)
        two(lambda e, p, c: e.activation(out=u.ap()[:, c], in_=p, func=AF.Exp, scale=-1.0), pA, [], [u.t], "act")
        op("act", lambda e: e.activation(out=L2.ap(), in_=u.ap(), func=AF.Ln, bias=1.0), reads=[u.t], writes=[L2.t])
        op("dve", lambda e: e.tensor_tensor(out=u.ap(), in0=u.ap(), in1=lb[d].ap(), op=ALU.mult), reads=[u.t, lb[d].t], writes=[u.t])
        op("act", lambda e: e.activation(out=u.ap(), in_=u.ap(), func=AF.Ln, bias=1.0), reads=[u.t], writes=[u.t])
        op("dve", lambda e: e.tensor_tensor(out=u.ap(), in0=u.ap(), in1=L2.ap(), op=ALU.subtract), reads=[u.t, L2.t], writes=[u.t])
        two(lambda e, p, c: e.scalar_tensor_tensor(out=L2.ap()[:, c], in0=p, scalar=-1.0, in1=L2.ap()[:, c], op0=ALU.mult, op1=ALU.subtract),
            pA, [L2.t], [L2.t], "dve")
        op("dve", lambda e: e.tensor_tensor(out=L2.ap(), in0=L2.ap(), in1=lnoml[d].ap(), op=ALU.add), reads=[L2.t, lnoml[d].t], writes=[L2.t])
        for half in range(2):
            op("pe", lambda e: e.matmul(pB[half].ap(), lhsT=tri.ap()[:, ti_incl, :], rhs=u.ap()[:, half * 512:(half + 1) * 512], start=True, stop=True),
               reads=[tri.t, u.t], writes=[pB[half].t])
            op("pe", lambda e: e.matmul(pC[half].ap(), lhsT=tri.ap()[:, ti_excl, :], rhs=u.ap()[:, half * 512:(half + 1) * 512], start=True, stop=True),
               reads=[tri.t, u.t], writes=[pC[half].t])
        for h in range(8):
            op("pe", lambda e: e.matmul(pD[0].ap()[:, h * NCH:(h + 1) * NCH], lhsT=u.ap()[:, h * 128:(h + 1) * 128], rhs=cind.ap(), start=True, stop=True),
               reads=[u.t, cind.t], writes=[pD[0].t])
        op("act", lambda e: e.activation(out=dec[d].ap(), in_=pD[0].ap()[:, 0:8 * NCH].rearrange("p (h c) -> p h c", c=NCH), func=AF.Exp),
           reads=[pD[0].t], writes=[dec[d].t])
        if need_o:
            two(lambda e, p, c: e.tensor_tensor(out=tA.ap()[:, c], in0=L2.ap()[:, c], in1=p, op=ALU.subtract), pB, [L2.t], [tA.t], "dve")
            op("act", lambda e: e.activation(out=Kt[d].ap(), in_=tA.ap(), func=AF.Exp), reads=[tA.t], writes=[Kt[d].t])
        two(lambda e, p, c: e.tensor_tensor(out=tB.ap()[:, c], in0=L2.ap()[:, c], in1=p, op=ALU.add), pC, [L2.t], [tB.t], "dve")
        op("act", lambda e: e.activation(out=Kh[d].ap(), in_=tB.ap(), func=AF.Exp), reads=[tB.t], writes=[Kh[d].t])
        if need_o:
            two(lambda e, p, c: e.activation(out=tA.ap()[:, c], in_=p, func=AF.Exp), pB, [], [tA.t], "act")
            tokproj(tile, 0, pA)
            two(lambda e, p, c: e.activation(out=tB.ap()[:, c], in_=p, func=AF.Exp, scale=-1.0), pA, [], [tB.t], "act")
            op("act", lambda e: e.activation(out=tB.ap(), in_=tB.ap(), func=AF.Ln, bias=1.0), reads=[tB.t], writes=[tB.t])
            op("act", lambda e: e.activation(out=tB.ap(), in_=tB.ap(), func=AF.Exp, scale=-1.0), reads=[tB.t], writes=[tB.t])
            two(lambda e, p, c: e.scalar_tensor_tensor(out=tB.ap()[:, c], in0=p, scalar=float(128 ** -0.5), in1=tB.ap()[:, c], op0=ALU.mult, op1=ALU.mult),
                pA, [tB.t], [tB.t], "dve")
            op("dve", lambda e: e.tensor_tensor(out=Qt[d].ap(), in0=tB.ap(), in1=tA.ap(), op=ALU.mult), reads=[tA.t, tB.t], writes=[Qt[d].t])
        tokproj(tile, 3072, pC)
        two(lambda e, p, c: e.copy(out=vt[d].ap()[:, c], in_=p), pC, [], [vt[d].t], "act")
        two(lambda e, p, c: e.activation(out=vt3[d].ap()[:, c], in_=p, func=AF.Identity, scale=cind.ap()[:, NCH - 1:NCH]), pC, [cind.t], [vt3[d].t], "act")
        if need_o:
            for (src, dst, pp) in ((Qt[d], QT[d], pB[0]), (Kt[d], KT[d], pB[1])):
                pv = pp.ap().bitcast(BF16)
                for h in range(8):
                    op("pe", lambda e: e.transpose(out=pv[:, h * 128:(h + 1) * 128], in_=src.ap()[:, h * 128:(h + 1) * 128], identity=ident_b.ap()),
                       reads=[src.t, ident_b.t], writes=[pp.t])
                op("act", lambda e: e.copy(out=dst.ap(), in_=pv.rearrange("p (h t) -> p h t", t=128)), reads=[pp.t], writes=[dst.t])
            for h in range(8):
                p = pA[h // 4]
                op("pe", lambda e: e.matmul(p.ap()[:, (h % 4) * 128:(h % 4 + 1) * 128], lhsT=KT[d].ap()[:, h, :], rhs=QT[d].ap()[:, h, :], start=True, stop=True),
                   reads=[KT[d].t, QT[d].t], writes=[p.t])
            for half in range(2):
                op("dve", lambda e: e.tensor_tensor(out=ATm[d].ap()[:, half * 4:(half + 1) * 4, :],
                                                    in0=pA[half].ap().rearrange("p (h t) -> p h t", t=128),
                                                    in1=tri.ap()[:, ti_incl:ti_incl + 1, :].to_broadcast([128, 4, 128]), op=ALU.mult),
                   reads=[pA[half].t, tri.t], writes=[ATm[d].t])

    def rec_parts(tile, d, need_o):
        pO = A2(6) if d == 0 else A2(0)
        pS = A2(2) if d == 0 else A2(4)
        ot = otL[d]
        corder = tuple(range(NCH)) if d == 0 else tuple(range(NCH - 1, -1, -1))

        def pre():
            if not need_o:
                return
            for h in range(8):
                p = pO[h // 4]
                cs = slice((h % 4) * 128, (h % 4 + 1) * 128)
                op("pe", lambda e: e.matmul(p.ap()[:, cs], lhsT=vt[d].ap()[:, h * 128:(h + 1) * 128], rhs=ATm[d].ap()[:, h, :], start=True, stop=True),
                   reads=[vt[d].t, ATm[d].t], writes=[p.t])
            for half in range(2):
                op("act", lambda e: e.copy(out=ot.ap()[:, half * 4:(half + 1) * 4, :], in_=pO[half].ap().rearrange("p (h t) -> p h t", t=128)),
                   reads=[pO[half].t], writes=[ot.t])

        def chunk(c):
            if need_o:
                for h in range(8):
                    p = pO[h // 4]
                    cs = slice((h % 4) * 128 + c * HC, (h % 4) * 128 + (c + 1) * HC)
                    op("pe", lambda e: e.matmul(p.ap()[:, cs], lhsT=Sb[d].ap()[:, h, :], rhs=QT[d].ap()[:, h, c * HC:(c + 1) * HC], start=True, stop=True),
                       reads=[Sb[d].t, QT[d].t], writes=[p.t])
            for h in range(8):
                p = pS[h // 4]
                if c < NCH - 1:
                    op("pe", lambda e: e.matmul(p.ap()[:, (h % 4) * 128:(h % 4 + 1) * 128], lhsT=Kh[d].ap()[c * HC:(c + 1) * HC, h * 128:(h + 1) * 128],
                                                rhs=vt[d].ap()[c * HC:(c + 1) * HC, h * 128:(h + 1) * 128], start=True, stop=True),
                       reads=[Kh[d].t, vt[d].t], writes=[p.t])
                else:
                    op("pe", lambda e: e.matmul(p.ap()[:, (h % 4) * 128:(h % 4 + 1) * 128], lhsT=Kh[d].ap()[:, h * 128:(h + 1) * 128],
                                                rhs=vt3[d].ap()[:, h * 128:(h + 1) * 128], start=True, stop=True),
                       reads=[Kh[d].t, vt3[d].t], writes=[p.t])
            op("dve", lambda e: e.tensor_tensor(out=Sf[d].ap(), in0=Sf[d].ap(), in1=dec[d].ap()[:, :, c:c + 1].to_broadcast([128, 8, 128]), op=ALU.mult),
               reads=[Sf[d].t, dec[d].t], writes=[Sf[d].t])
            for half in range(2):
                op("dve", lambda e: e.tensor_tensor(out=Sf[d].ap()[:, half * 4:(half + 1) * 4, :], in0=Sf[d].ap()[:, half * 4:(half + 1) * 4, :],
                                                    in1=pS[half].ap().rearrange("p (h v) -> p h v", v=128), op=ALU.add),
                   reads=[Sf[d].t, pS[half].t], writes=[Sf[d].t])
            op("act", lambda e: e.copy(out=Sb[d].ap(), in_=Sf[d].ap()), reads=[Sf[d].t], writes=[Sb[d].t])

        def post():
            if not need_o:
                return
            lt = tile
            second = (d == 0 and lt >= 8) or (d == 1 and lt < 8)
            for half in range(2):
                op("dve", lambda e: e.tensor_tensor(out=ot.ap()[:, half * 4:(half + 1) * 4, :], in0=ot.ap()[:, half * 4:(half + 1) * 4, :],
                                                    in1=pO[half].ap().rearrange("p (h t) -> p h t", t=128), op=ALU.add),
                   reads=[ot.t, pO[half].t], writes=[ot.t])
            if not second:
                dma("sp", oD[lt], ot.ap(), reads=[ot.t], writes=[t_oD[lt]])
                return
            dma("sp", o1.ap(), oD[lt], reads=[t_oD[lt]], writes=[o1.t])
            op("dve", lambda e: e.tensor_tensor(out=ot.ap(), in0=ot.ap(), in1=o1.ap(), op=ALU.add), reads=[ot.t, o1.t], writes=[ot.t])
            op("act", lambda e: e.activation(out=sqo.ap(), in_=ot.ap(), func=AF.Square), reads=[ot.t], writes=[sqo.t])
            pN = pO
            for half in range(2):
                op("pe", lambda e: e.matmul(pN[half].ap(), lhsT=ones_f.ap(), rhs=sqo.ap()[:, half * 4:(half + 1) * 4, :], start=True, stop=True),
                   reads=[ones_f.t, sqo.t], writes=[pN[half].t])
                op("act", lambda e: e.activation(out=sqo.ap()[:, half * 4:(half + 1) * 4, :], in_=pN[half].ap().rearrange("p (h t) -> p h t", t=128),
                                                 func=AF.Ln, scale=1.0 / 128, bias=EPS), reads=[pN[half].t], writes=[sqo.t])
            op("act", lambda e: e.activation(out=sqo.ap(), in_=sqo.ap(), func=AF.Exp, scale=-0.5), reads=[sqo.t], writes=[sqo.t])
            op("dve", lambda e: e.scalar_tensor_tensor(out=yh.ap(), in0=ot.ap(), scalar=ghgT.ap()[:, 0:1], in1=sqo.ap(), op0=ALU.mult, op1=ALU.mult),
               reads=[ot.t, sqo.t, ghgT.t], writes=[yh.t])
            dma("sp", yhD[b, :, :, lt * 128:(lt + 1) * 128], yh.ap(), reads=[yh.t], writes=[self.t_yhD])

        return [pre] + [(lambda c=c: chunk(c)) for c in corder] + [post]

    forder = [16, 17] + list(range(16))
    border = [17, 16] + list(range(15, -1, -1))
    for step in range(18):
        tf, tb = forder[step], border[step]
        prep(tf, 0, tf < 16)
        prep(tb, 1, tb < 16)
        for fa, fb in zip(rec_parts(tf, 0, tf < 16), rec_parts(tb, 1, tb < 16)):
            fa()
            fb()
    if self.dbg and b == 0:
        o = self.dout("dbg_yh", [128, 8, NLAT], BF16)
        t = T("dbg_yh")
        S.dma("sp", o, yhD[0], reads=[self.t_yhD], writes=[t])
        self.dbg_outs["yh"] = t
    self.close_scope()


def _merge_ffn_in(self, b, L):
    nc, S = self.nc, self.S
    op, dma = S.op, S.dma
    ps = self.ps
    ident_b, ones_b = self.ident_b, self.ones_b
    hT, hT_t = self.hT, self.hT_t
    win_v = L["win_d"].rearrange("(k p) c -> p k c", p=128)
    modD = self.modD
    self.open_scope()
    yT = self.sb("yTm", [128, 8, NLAT], BF16)
    self.open_scope()
    wG = self.sb("wG", [128, 8, 3072], BF16)
    for pc in range(3):
        dma("pool", wG.ap()[:, :, pc * 1024:(pc + 1) * 1024], win_v[:, :, C_HG + pc * 1024:C_HG + (pc + 1) * 1024], writes=[wG.t])
    ym = [self.sb(f"ym{i}", [128, 8, 512], BF16) for i in range(2)]
    yhs = [self.sb(f"yhs{i}", [128, 8, 512], BF16) for i in range(2)]
    sg = [self.sb(f"sg{i}", [128, 512]) for i in range(3)]
    t1 = self.sb("t1", [128, 512]); t2 = self.sb("t2", [128, 512])
    for s in range(4):
        c0 = s * 512
        dma("sp", ym[s % 2].ap(), L["ymlaD"][b, :, :, c0:c0 + 512], reads=[self.t_ymlaD], writes=[ym[s % 2].t])
        dma("sp", yhs[s % 2].ap(), L["yhD"][b, :, :, c0:c0 + 512], reads=[self.t_yhD], writes=[yhs[s % 2].t])
        for m in range(8):
            pg = [ps[(m * 3 + g) % 8] for g in range(3)]
            for g in range(3):
                for k in range(8):
                    op("pe", lambda e: e.matmul(pg[g].ap(), lhsT=wG.ap()[:, k, g * 1024 + m * 128: g * 1024 + (m + 1) * 128],
                                                rhs=hT.ap()[:, k, c0:c0 + 512], start=(k == 0), stop=(k == 7)),
                       reads=[wG.t] + hT_t[c0 // 128:(c0 + 512) // 128], writes=[pg[g].t])
                op("act", lambda e: e.activation(out=sg[g].ap(), in_=pg[g].ap(), func=AF.Sigmoid), reads=[pg[g].t], writes=[sg[g].t])
            op("dve", lambda e: e.tensor_tensor(out=t1.ap(), in0=pg[0].ap(), in1=sg[0].ap(), op=ALU.mult), reads=[pg[0].t, sg[0].t], writes=[t1.t])
            op("dve", lambda e: e.tensor_tensor(out=t1.ap(), in0=t1.ap(), in1=yhs[s % 2].ap()[:, m, :], op=ALU.mult), reads=[t1.t, yhs[s % 2].t], writes=[t1.t])
            op("dve", lambda e: e.tensor_tensor(out=t1.ap(), in0=t1.ap(), in1=sg[2].ap(), op=ALU.mult), reads=[t1.t, sg[2].t], writes=[t1.t])
            op("dve", lambda e: e.tensor_tensor(out=t2.ap(), in0=sg[1].ap(), in1=ym[s % 2].ap()[:, m, :], op=ALU.mult), reads=[sg[1].t, ym[s % 2].t], writes=[t2.t])
            op("dve", lambda e: e.tensor_tensor(out=yT.ap()[:, m, c0:c0 + 512], in0=t1.ap(), in1=t2.ap(), op=ALU.add), reads=[t1.t, t2.t], writes=[yT.t])
    self.close_scope()
    wo = self.sb("wo", [128, 8, D], BF16)
    dma("pool", wo.ap(), L["wout_d"].rearrange("(k p) c -> p k c", p=128), writes=[wo.t])
    wr = self.sb("wr", [128, 8, NEXP], BF16)
    dma("pool", wr.ap(), L["wr_d"].rearrange("(k p) c -> p k c", p=128), writes=[wr.t])
    brb = self.sb("brb", [1, NEXP], BF16)
    dma("pool", brb.ap(), L["br_d"], writes=[brb.t])
    mod2 = self.sb("mod2", [128, D]); dma("sp", mod2.ap(), modD[b, 2 * D:3 * D].partition_broadcast(128), reads=[self.t_modD], writes=[mod2.t])
    S2 = self.sb("S2", [128, D]); dma("sp", S2.ap(), modD[b, 3 * D:4 * D].partition_broadcast(128), reads=[self.t_modD], writes=[S2.t])
    G2 = self.sb("G2", [128, D]); dma("sp", G2.ap(), modD[b, 4 * D:5 * D].partition_broadcast(128), reads=[self.t_modD], writes=[G2.t])
    gf = self.sb("gf", [128, D]); dma("sp", gf.ap(), L["gffn_d"][0, :].partition_broadcast(128), writes=[gf.t])
    op("dve", lambda e: e.scalar_tensor_tensor(out=G2.ap(), in0=G2.ap(), scalar=1.0, in1=gf.ap(), op0=ALU.add, op1=ALU.mult),
       reads=[G2.t, gf.t], writes=[G2.t])
    xr = [self.sb(f"xr{i}", [128, D]) for i in range(2)]
    tm = self.sb("tm", [128, D])
    sq = self.sb("sq4", [128, D])
    st = self.sb("st4", [128, 4])
    h2 = [self.sb(f"h2_{i}", [128, D], BF16) for i in range(2)]
    h2T = self.sb("h2T", [128, 8, 128], BF16)
    lg = self.sb("lg", [128, NEXP])
    mx = self.sb("mx", [128, 8]); ix = self.sb("ix", [128, 8], U32); ixf = self.sb("ixf", [128, 8])
    oh = [self.sb(f"oh{k}", [128, NEXP]) for k in range(4)]
    msk = self.sb("msk", [128, NEXP]); mskb = self.sb("mskb", [128, NEXP], BF16)
    ex = self.sb("ex", [128, 4]); sm = self.sb("sm", [128, 2])
    pos = self.sb("pos", [128, NEXP]); tmp32 = self.sb("tmp32", [128, NEXP]); dstf = self.sb("dstf", [128, 4])
    x_d, x1D, XeD = L["x_d"], L["x1D"], L["XeD"]
    base_bc, destAll, probAll, ustr, iota_e, iota_c = self.base_bc, self.destAll, self.probAll, self.ustr, self.iota_e, self.iota_c
    for i in range(16):
        gt = b * 16 + i
        r0 = b * NLAT + i * 128
        xb, hb = xr[i % 2], h2[i % 2]
        dma("sp", xb.ap(), x_d[r0:r0 + 128, :], writes=[xb.t])
        pm = (ps[0], ps[1])
        for half in range(2):
            for k in range(8):
                op("pe", lambda e: e.matmul(pm[half].ap(), lhsT=yT.ap()[:, k, i * 128:(i + 1) * 128], rhs=wo.ap()[:, k, half * 512:(half + 1) * 512],
                                            start=(k == 0), stop=(k == 7)), reads=[yT.t, wo.t], writes=[pm[half].t])
            op("dve", lambda e: e.tensor_tensor(out=tm.ap()[:, half * 512:(half + 1) * 512], in0=pm[half].ap(), in1=mod2.ap()[:, half * 512:(half + 1) * 512], op=ALU.mult),
               reads=[pm[half].t, mod2.t], writes=[tm.t])
        op("dve", lambda e: e.tensor_tensor(out=xb.ap(), in0=xb.ap(), in1=tm.ap(), op=ALU.add), reads=[xb.t, tm.t], writes=[xb.t])
        dma("sp", x1D[r0:r0 + 128, :], xb.ap(), reads=[xb.t], writes=[self.tl_x1[gt]])
        op("act", lambda e: e.activation(out=sq.ap(), in_=xb.ap(), func=AF.Square, accum_out=st.ap()[:, 0:1]), reads=[xb.t], writes=[sq.t, st.t])
        op("act", lambda e: e.activation(out=st.ap()[:, 1:2], in_=st.ap()[:, 0:1], func=AF.Ln, scale=1.0 / D, bias=EPS), reads=[st.t], writes=[st.t])
        op("act", lambda e: e.activation(out=st.ap()[:, 2:3], in_=st.ap()[:, 1:2], func=AF.Exp, scale=-0.5), reads=[st.t], writes=[st.t])
        op("dve", lambda e: e.scalar_tensor_tensor(out=tm.ap(), in0=xb.ap(), scalar=st.ap()[:, 2:3], in1=G2.ap(), op0=ALU.mult, op1=ALU.mult),
           reads=[xb.t, st.t, G2.t], writes=[tm.t])
        op("dve", lambda e: e.tensor_tensor(out=hb.ap(), in0=tm.ap(), in1=S2.ap(), op=ALU.add), reads=[tm.t, S2.t], writes=[hb.t])
        pt = ps[2]
        ptv = pt.ap().bitcast(BF16)
        for k in range(8):
            op("pe", lambda e: e.transpose(out=ptv[:, k * 128:(k + 1) * 128], in_=hb.ap()[:, k * 128:(k + 1) * 128], identity=ident_b.ap()),
               reads=[hb.t, ident_b.t], writes=[pt.t])
        op("act", lambda e: e.copy(out=h2T.ap(), in_=ptv.rearrange("p (k t) -> p k t", t=128)), reads=[pt.t], writes=[h2T.t])
        pl = ps[3]
        for k in range(8):
            op("pe", lambda e: e.matmul(pl.ap()[:, 0:NEXP], lhsT=h2T.ap()[:, k, :], rhs=wr.ap()[:, k, :], start=(k == 0), stop=False),
               reads=[h2T.t, wr.t], writes=[pl.t])
        op("pe", lambda e: e.matmul(pl.ap()[:, 0:NEXP], lhsT=ones_b.ap()[0:1, :], rhs=brb.ap(), start=False, stop=True),
           reads=[ones_b.t, brb.t], writes=[pl.t])
        op("act", lambda e: e.copy(out=lg.ap(), in_=pl.ap()[:, 0:NEXP]), reads=[pl.t], writes=[lg.t])
        op("dve", lambda e: e.max(out=mx.ap(), in_=lg.ap()), reads=[lg.t], writes=[mx.t])
        op("dve", lambda e: e.max_index(out=ix.ap(), in_max=mx.ap(), in_values=lg.ap()), reads=[mx.t, lg.t], writes=[ix.t])
        op("dve", lambda e: e.tensor_copy(out=ixf.ap(), in_=ix.ap()), reads=[ix.t], writes=[ixf.t])
        for k in range(4):
            op("dve", lambda e: e.tensor_scalar(out=oh[k].ap(), in0=iota_e.ap(), scalar1=ixf.ap()[:, k:k + 1], scalar2=None, op0=ALU.is_equal),
               reads=[iota_e.t, ixf.t], writes=[oh[k].t])
        op("dve", lambda e: e.tensor_tensor(out=msk.ap(), in0=oh[0].ap(), in1=oh[1].ap(), op=ALU.add), reads=[oh[0].t, oh[1].t], writes=[msk.t])
        op("dve", lambda e: e.tensor_tensor(out=msk.ap(), in0=msk.ap(), in1=oh[2].ap(), op=ALU.add), reads=[msk.t, oh[2].t], writes=[msk.t])
        op("dve", lambda e: e.tensor_tensor(out=mskb.ap(), in0=msk.ap(), in1=oh[3].ap(), op=ALU.add), reads=[msk.t, oh[3].t], writes=[mskb.t])
        op("dve", lambda e: e.tensor_scalar(out=sm.ap()[:, 0:1], in0=mx.ap()[:, 0:1], scalar1=-1.0, scalar2=None, op0=ALU.mult), reads=[mx.t], writes=[sm.t])
        op("act", lambda e: e.activation(out=ex.ap(), in_=mx.ap()[:, 0:4], func=AF.Exp, bias=sm.ap()[:, 0:1], accum_out=sm.ap()[:, 1:2]),
           reads=[mx.t, sm.t], writes=[ex.t, sm.t])
        op("dve", lambda e: e.reciprocal(out=sm.ap()[:, 1:2], in_=sm.ap()[:, 1:2]), reads=[sm.t], writes=[sm.t])
        op("dve", lambda e: e.tensor_scalar(out=probAll.ap()[:, gt, :], in0=ex.ap(), scalar1=sm.ap()[:, 1:2], scalar2=None, op0=ALU.mult),
           reads=[ex.t, sm.t], writes=[probAll.t])
        pp, pc = ps[4], ps[5]
        op("pe", lambda e: e.matmul(pp.ap()[:, 0:NEXP], lhsT=ustr.ap(), rhs=mskb.ap(), start=True, stop=True), reads=[ustr.t, mskb.t], writes=[pp.t])
        op("pe", lambda e: e.matmul(pc.ap()[:, 0:NEXP], lhsT=ones_b.ap(), rhs=mskb.ap(), start=True, stop=True), reads=[ones_b.t, mskb.t], writes=[pc.t])
        op("dve", lambda e: e.tensor_tensor(out=pos.ap(), in0=pp.ap()[:, 0:NEXP], in1=base_bc.ap(), op=ALU.add), reads=[pp.t, base_bc.t], writes=[pos.t])
        op("dve", lambda e: e.tensor_tensor(out=base_bc.ap(), in0=pc.ap()[:, 0:NEXP], in1=base_bc.ap(), op=ALU.add), reads=[pc.t, base_bc.t], writes=[base_bc.t])
        op("dve", lambda e: e.tensor_tensor(out=pos.ap(), in0=pos.ap(), in1=iota_c.ap(), op=ALU.add), reads=[pos.t, iota_c.t], writes=[pos.t])
        for k in range(4):
            op("dve", lambda e: e.tensor_tensor(out=tmp32.ap(), in0=pos.ap(), in1=oh[k].ap(), op=ALU.mult), reads=[pos.t, oh[k].t], writes=[tmp32.t])
            op("dve", lambda e: e.reduce_sum(out=dstf.ap()[:, k:k + 1], in_=tmp32.ap(), axis=AX.X), reads=[tmp32.t], writes=[dstf.t])
        op("dve", lambda e: e.tensor_copy(out=destAll.ap()[:, gt, :], in_=dstf.ap()), reads=[dstf.t], writes=[destAll.t])
        op("dve", lambda e: e.tensor_copy(out=self.dstfAll.ap()[:, gt, :], in_=dstf.ap()), reads=[dstf.t], writes=[self.dstfAll.t])
        op("dve", lambda e: e.tensor_copy(out=self.eidAll.ap()[:, gt, :], in_=ixf.ap()[:, 0:4]), reads=[ixf.t], writes=[self.eidAll.t])
        for k in range(4):
            tsc = T("Xsc"); self.tl_Xe.append(tsc)
            dma("pool", None, None, reads=[hb.t, destAll.t, self.t_XeD], writes=[tsc],
                fn=lambda e: e.indirect_dma_start(out=XeD, out_offset=bass.IndirectOffsetOnAxis(ap=destAll.ap()[:, gt, k:k + 1], axis=0),
                                                  in_=hb.ap(), in_offset=None))
    if self.dbg and b == 0:
        o = self.dout("dbg_x1", [NLAT, D]); t = T("dbg_x1")
        S.dma("sp", o, x1D[0:NLAT, :], reads=self.tl_x1[0:16], writes=[t]); self.dbg_outs["x1"] = t
        self.dump("yTm", yT, [128, 8, NLAT], BF16)
        self.dump("destAll", destAll, [128, 32, 4], I32)
        self.dump("probAll", probAll, [128, 32, 4])
        self.dump("base_bc", base_bc, [128, NEXP])
        self.dump("lg", lg, [128, NEXP])
        self.dump("h2last", h2[1], [128, D], BF16)
    self.close_scope()


def _moe(self, L):
    nc, S = self.nc, self.S
    op, dma = S.op, S.dma
    ps = self.ps
    ident_b, ones_b = self.ident_b, self.ones_b
    XeD, YeD, x1D, modD = L["XeD"], L["YeD"], L["x1D"], self.modD
    cnt, iota_e = self.base_bc, self.iota_e
    eI = self.sb("eI", [128, NSL], I32)
    rI = self.sb("rI", [128, NSL], I32)
    wI = self.sb("wI", [128, NSL], I32)
    xI = self.sb("xI", [128, NSL, NB], I32)
    ydest = self.sb("ydest", [128, 32, 4], I32)
    self.open_scope()
    jota = self.sb("jota", [128, NSL]); dma("sp", jota.ap(), L["jota_d"], writes=[jota.t])
    nsl = self.sb("nsl", [128, NEXP]); tmp = self.sb("tmpe", [128, NEXP])
    ca = self.sb("ca", [128, NEXP]); cb = self.sb("cb", [128, NEXP]); cstart = self.sb("cstart", [128, NEXP]); adj = self.sb("adj", [128, NEXP])
    op("dve", lambda e: e.tensor_scalar(out=nsl.ap(), in0=cnt.ap(), scalar1=0.0, scalar2=None, op0=ALU.is_gt), reads=[cnt.t], writes=[nsl.t])
    for m in range(1, CAP // SLAB):
        op("dve", lambda e: e.tensor_scalar(out=tmp.ap(), in0=cnt.ap(), scalar1=float(m * SLAB), scalar2=None, op0=ALU.is_gt), reads=[cnt.t], writes=[tmp.t])
        op("dve", lambda e: e.tensor_tensor(out=nsl.ap(), in0=nsl.ap(), in1=tmp.ap(), op=ALU.add), reads=[nsl.t, tmp.t], writes=[nsl.t])
    op("dve", lambda e: e.tensor_copy(out=ca.ap(), in_=nsl.ap()), reads=[nsl.t], writes=[ca.t])
    src, dst = ca, cb
    sh = 1
    while sh < NEXP:
        op("dve", lambda e: e.tensor_copy(out=dst.ap()[:, 0:sh], in_=src.ap()[:, 0:sh]), reads=[src.t], writes=[dst.t])
        op("dve", lambda e: e.tensor_tensor(out=dst.ap()[:, sh:], in0=src.ap()[:, sh:], in1=src.ap()[:, 0:NEXP - sh], op=ALU.add), reads=[src.t], writes=[dst.t])
        src, dst = dst, src
        sh *= 2
    cend = src
    op("dve", lambda e: e.tensor_tensor(out=cstart.ap(), in0=cend.ap(), in1=nsl.ap(), op=ALU.subtract), reads=[cend.t, nsl.t], writes=[cstart.t])
    big = self.sb("big3", [128, 128, NEXP])
    ej = self.sb("ej", [128, NSL]); csj = self.sb("csj", [128, NSL]); vj = self.sb("vj", [128, NSL]); rj = self.sb("rj", [128, NSL])
    b3 = big.ap()[:, 0:NSL, :]
    op("dve", lambda e: e.tensor_tensor(out=b3, in0=cend.ap().unsqueeze(1).to_broadcast([128, NSL, NEXP]),
                                        in1=jota.ap().unsqueeze(2).to_broadcast([128, NSL, NEXP]), op=ALU.is_le), reads=[cend.t, jota.t], writes=[big.t])
    op("dve", lambda e: e.reduce_sum(out=ej.ap(), in_=b3, axis=AX.X), reads=[big.t], writes=[ej.t])
    op("dve", lambda e: e.tensor_scalar_min(out=ej.ap(), in0=ej.ap(), scalar1=float(NEXP - 1)), reads=[ej.t], writes=[ej.t])
    op("dve", lambda e: e.tensor_tensor(out=b3, in0=iota_e.ap().unsqueeze(1).to_broadcast([128, NSL, NEXP]),
                                        in1=ej.ap().unsqueeze(2).to_broadcast([128, NSL, NEXP]), op=ALU.is_equal), reads=[iota_e.t, ej.t], writes=[big.t])
    op("dve", lambda e: e.tensor_tensor(out=b3, in0=b3, in1=cstart.ap().unsqueeze(1).to_broadcast([128, NSL, NEXP]), op=ALU.mult), reads=[big.t, cstart.t], writes=[big.t])
    op("dve", lambda e: e.reduce_sum(out=csj.ap(), in_=b3, axis=AX.X), reads=[big.t], writes=[csj.t])
    op("dve", lambda e: e.tensor_scalar(out=vj.ap(), in0=jota.ap(), scalar1=cend.ap()[:, NEXP - 1:NEXP], scalar2=None, op0=ALU.is_lt), reads=[jota.t, cend.t], writes=[vj.t])
    op("dve", lambda e: e.tensor_tensor(out=rj.ap(), in0=jota.ap(), in1=csj.ap(), op=ALU.subtract), reads=[jota.t, csj.t], writes=[rj.t])
    op("dve", lambda e: e.tensor_scalar(out=rj.ap(), in0=rj.ap(), scalar1=float(SLAB), scalar2=None, op0=ALU.mult), reads=[rj.t], writes=[rj.t])
    cntj = self.sb("cntj", [128, NSL]); off4 = self.sb("off4", [128, NB]); dma("sp", off4.ap(), L["off4_d"], writes=[off4.t])
    op("dve", lambda e: e.tensor_tensor(out=b3, in0=iota_e.ap().unsqueeze(1).to_broadcast([128, NSL, NEXP]),
                                        in1=ej.ap().unsqueeze(2).to_broadcast([128, NSL, NEXP]), op=ALU.is_equal), reads=[iota_e.t, ej.t], writes=[big.t])
    op("dve", lambda e: e.tensor_tensor(out=b3, in0=b3, in1=cnt.ap().unsqueeze(1).to_broadcast([128, NSL, NEXP]), op=ALU.mult), reads=[big.t, cnt.t], writes=[big.t])
    op("dve", lambda e: e.reduce_sum(out=cntj.ap(), in_=b3, axis=AX.X), reads=[big.t], writes=[cntj.t])
    op("dve", lambda e: e.tensor_tensor(out=cntj.ap(), in0=cntj.ap(), in1=vj.ap(), op=ALU.mult), reads=[cntj.t, vj.t], writes=[cntj.t])
    op("dve", lambda e: e.tensor_tensor(out=ej.ap(), in0=ej.ap(), in1=vj.ap(), op=ALU.mult), reads=[ej.t, vj.t], writes=[ej.t])
    posb = self.sb("posb", [128, NSL, NB]); val4 = self.sb("val4", [128, NSL, NB])
    op("dve", lambda e: e.tensor_tensor(out=posb.ap(), in0=rj.ap().unsqueeze(2).to_broadcast([128, NSL, NB]),
                                        in1=off4.ap().unsqueeze(1).to_broadcast([128, NSL, NB]), op=ALU.add), reads=[rj.t, off4.t], writes=[posb.t])
    op("dve", lambda e: e.tensor_tensor(out=val4.ap(), in0=posb.ap(), in1=cntj.ap().unsqueeze(2).to_broadcast([128, NSL, NB]), op=ALU.is_lt),
       reads=[posb.t, cntj.t], writes=[val4.t])
    op("dve", lambda e: e.tensor_tensor(out=posb.ap(), in0=posb.ap(), in1=val4.ap(), op=ALU.mult), reads=[posb.t, val4.t], writes=[posb.t])
    op("dve", lambda e: e.tensor_scalar(out=rj.ap(), in0=ej.ap(), scalar1=float(CAP), scalar2=None, op0=ALU.mult), reads=[ej.t], writes=[rj.t])
    op("dve", lambda e: e.tensor_tensor(out=posb.ap(), in0=posb.ap(), in1=rj.ap().unsqueeze(2).to_broadcast([128, NSL, NB]), op=ALU.add), reads=[posb.t, rj.t], writes=[posb.t])
    op("dve", lambda e: e.tensor_copy(out=xI.ap(), in_=posb.ap()), reads=[posb.t], writes=[xI.t])
    pidx = self.sb("pidx", [128, NSL]); dma("sp", pidx.ap(), L["pidx_d"], writes=[pidx.t])
    OOB = 1.0e6
    ld = self.sb("ld", [128, NSL]); tmpj = self.sb("tmpj", [128, NSL])
    op("dve", lambda e: e.memset(ld.ap(), 1.0), writes=[ld.t])
    op("dve", lambda e: e.tensor_tensor(out=ld.ap()[:, 2:], in0=ej.ap()[:, 2:], in1=ej.ap()[:, 0:NSL - 2], op=ALU.not_equal), reads=[ej.t], writes=[ld.t])
    op("dve", lambda e: e.tensor_scalar(out=tmpj.ap(), in0=ej.ap(), scalar1=-OOB, scalar2=None, op0=ALU.add), reads=[ej.t], writes=[tmpj.t])
    op("dve", lambda e: e.tensor_tensor(out=tmpj.ap(), in0=tmpj.ap(), in1=ld.ap(), op=ALU.mult), reads=[tmpj.t, ld.t], writes=[tmpj.t])
    op("dve", lambda e: e.tensor_scalar(out=tmpj.ap(), in0=tmpj.ap(), scalar1=OOB, scalar2=None, op0=ALU.add), reads=[tmpj.t], writes=[tmpj.t])
    op("dve", lambda e: e.tensor_copy(out=eI.ap(), in_=tmpj.ap()), reads=[tmpj.t], writes=[eI.t])
    op("dve", lambda e: e.scalar_tensor_tensor(out=ej.ap(), in0=ej.ap(), scalar=128.0, in1=pidx.ap(), op0=ALU.mult, op1=ALU.add), reads=[ej.t, pidx.t], writes=[ej.t])
    op("dve", lambda e: e.tensor_scalar(out=tmpj.ap(), in0=ej.ap(), scalar1=-OOB, scalar2=None, op0=ALU.add), reads=[ej.t], writes=[tmpj.t])
    op("dve", lambda e: e.tensor_tensor(out=tmpj.ap(), in0=tmpj.ap(), in1=ld.ap(), op=ALU.mult), reads=[tmpj.t, ld.t], writes=[tmpj.t])
    op("dve", lambda e: e.tensor_scalar(out=tmpj.ap(), in0=tmpj.ap(), scalar1=OOB, scalar2=None, op0=ALU.add), reads=[tmpj.t], writes=[tmpj.t])
    op("dve", lambda e: e.tensor_copy(out=wI.ap(), in_=tmpj.ap()), reads=[tmpj.t], writes=[wI.t])
    op("dve", lambda e: e.tensor_scalar(out=adj.ap(), in0=cstart.ap(), scalar1=float(SLAB), scalar2=None, op0=ALU.mult), reads=[cstart.t], writes=[adj.t])
    op("dve", lambda e: e.tensor_tensor(out=adj.ap(), in0=adj.ap(), in1=self.iota_c.ap(), op=ALU.subtract), reads=[adj.t, self.iota_c.t], writes=[adj.t])
    eid2 = self.eidAll.ap().rearrange("p g k -> p (g k)")
    op("dve", lambda e: e.tensor_tensor(out=big.ap(), in0=iota_e.ap().unsqueeze(1).to_broadcast([128, 128, NEXP]),
                                        in1=eid2.unsqueeze(2).to_broadcast([128, 128, NEXP]), op=ALU.is_equal), reads=[iota_e.t, self.eidAll.t], writes=[big.t])
    op("dve", lambda e: e.tensor_tensor(out=big.ap(), in0=big.ap(), in1=adj.ap().unsqueeze(1).to_broadcast([128, 128, NEXP]), op=ALU.mult), reads=[big.t, adj.t], writes=[big.t])
    adjp = self.sb("adjp", [128, 128])
    op("dve", lambda e: e.reduce_sum(out=adjp.ap(), in_=big.ap(), axis=AX.X), reads=[big.t], writes=[adjp.t])
    op("dve", lambda e: e.tensor_tensor(out=adjp.ap(), in0=adjp.ap(), in1=self.dstfAll.ap().rearrange("p g k -> p (g k)"), op=ALU.add), reads=[adjp.t, self.dstfAll.t], writes=[adjp.t])
    op("dve", lambda e: e.tensor_copy(out=ydest.ap().rearrange("p g k -> p (g k)"), in_=adjp.ap()), reads=[adjp.t], writes=[ydest.t])
    if self.dbg:
        self.dump("eI", eI, [128, NSL], I32); self.dump("xI", xI, [128, NSL, NB], I32); self.dump("ydest", ydest, [128, 32, 4], I32)
    self.close_scope()
    self.ydest = ydest
    self.open_scope()
    wgu = [self.sb(f"wgu{i}", [128, 8, 2 * D], BF16) for i in range(2)]
    wd = [self.sb(f"wd{i}", [128, 8, D], BF16) for i in range(2)]
    bgu = [self.sb(f"bgu{i}", [128, 2, 8]) for i in range(2)]
    xblk = [self.sb(f"xblk{i}", [128, NB, D], BF16) for i in range(2)]
    xT = self.sb("xTe", [128, 8, SLAB], BF16)
    aT = self.sb("aTe", [128, 8, SLAB], BF16)
    xg = self.sb("xg", [128, SLAB]); sgm = self.sb("sgm", [128, SLAB]); xl = self.sb("xl", [128, SLAB])
    yo = [self.sb(f"yo{i}", [128, D]) for i in range(2)]
    Yv = YeD.rearrange("(n p) d -> n p d", p=128)
    bdf = [self.sb(f"bdf{i}", [128, D]) for i in range(2)]
    Xg = XeD.rearrange("(r j) d -> r (j d)", j=4)
    Wg = L["wgu_d"].rearrange("e (p k) c -> (e p) (k c)", k=8)
    Wd = L["wd_d"].rearrange("e (p k) c -> (e p) (k c)", k=8)
    Bg = L["bguT_d"].rearrange("e p t c -> (e p) (t c)")
    IOA = bass.IndirectOffsetOnAxis
    bregs = {}

    def gather(dst_ap, src, idx_ap, reads, writes):
        bound = src.shape[0] - 1
        if bound not in bregs:
            bregs[bound] = nc.gpsimd.alloc_register(f"bound{bound}")
            nc.gpsimd.reg_mov(bregs[bound], bound)
        dma("pool", None, None, reads=reads, writes=writes,
            fn=lambda e: e.indirect_dma_start(out=dst_ap, out_offset=None, in_=src, in_offset=IOA(ap=idx_ap, axis=0),
                                              bounds_check=bregs[bound], oob_is_err=False))

    for j in range(NSL):
        wg, wdn, bd, bg, xb = wgu[j % 2], wd[j % 2], bdf[j % 2], bgu[j % 2], xblk[j % 2]
        for jb in range(NB):
            gather(xb.ap()[:, jb, :], XeD, xI.ap()[:, j, jb:jb + 1], [self.t_XeD, xI.t] + self.tl_Xe, [xb.t])
        gather(wg.ap().rearrange("p k c -> p (k c)"), Wg, wI.ap()[:, j:j + 1], [wI.t], [wg.t])
        gather(wdn.ap().rearrange("p k c -> p (k c)"), Wd, wI.ap()[:, j:j + 1], [wI.t], [wdn.t])
        gather(bg.ap().rearrange("p t c -> p (t c)"), Bg, wI.ap()[:, j:j + 1], [wI.t], [bg.t])
        gather(bd.ap(), L["bd_d"], eI.ap()[:, j:j + 1], [eI.t], [bd.t])
        for jb in range(NB):
            pt = ps[jb % 2]
            ptv = pt.ap().bitcast(BF16)
            for k in range(8):
                op("pe", lambda e: e.transpose(out=ptv[:, k * 128:(k + 1) * 128], in_=xb.ap()[:, jb, k:D:8], identity=ident_b.ap()),
                   reads=[xb.t, ident_b.t], writes=[pt.t])
            if jb % 2 == 0:
                op("act", lambda e: e.copy(out=xT.ap()[:, :, jb * 128:(jb + 1) * 128], in_=ptv.rearrange("p (k t) -> p k t", t=128)), reads=[pt.t], writes=[xT.t])
            else:
                op("dve", lambda e: e.tensor_copy(out=xT.ap()[:, :, jb * 128:(jb + 1) * 128], in_=ptv.rearrange("p (k t) -> p k t", t=128)), reads=[pt.t], writes=[xT.t])
        for c in range(8):
            pg, pl = ps[2 + (c % 2) * 2], ps[3 + (c % 2) * 2]
            for k in range(8):
                op("pe", lambda e: e.matmul(pg.ap()[:, 0:SLAB], lhsT=wg.ap()[:, k, 2 * c:2 * D:16], rhs=xT.ap()[:, k, :], start=(k == 0), stop=(k == 7)),
                   reads=[wg.t, xT.t], writes=[pg.t])
            for k in range(8):
                op("pe", lambda e: e.matmul(pl.ap()[:, 0:SLAB], lhsT=wg.ap()[:, k, 2 * c + 1:2 * D:16], rhs=xT.ap()[:, k, :], start=(k == 0), stop=(k == 7)),
                   reads=[wg.t, xT.t], writes=[pl.t])
            op("dve", lambda e: e.tensor_scalar(out=xg.ap(), in0=pg.ap()[:, 0:SLAB], scalar1=bg.ap()[:, 0, c:c + 1], scalar2=7.0, op0=ALU.add, op1=ALU.min),
               reads=[pg.t, bg.t], writes=[xg.t])
            op("act", lambda e: e.activation(out=sgm.ap(), in_=xg.ap(), func=AF.Sigmoid, scale=1.702), reads=[xg.t], writes=[sgm.t])
            op("dve", lambda e: e.tensor_scalar(out=xl.ap(), in0=pl.ap()[:, 0:SLAB], scalar1=bg.ap()[:, 1, c:c + 1], scalar2=7.0, op0=ALU.add, op1=ALU.min),
               reads=[pl.t, bg.t], writes=[xl.t])
            op("dve", lambda e: e.tensor_scalar(out=xl.ap(), in0=xl.ap(), scalar1=-7.0, scalar2=1.0, op0=ALU.max, op1=ALU.add), reads=[xl.t], writes=[xl.t])
            op("dve", lambda e: e.tensor_tensor(out=xg.ap(), in0=xg.ap(), in1=sgm.ap(), op=ALU.mult), reads=[xg.t, sgm.t], writes=[xg.t])
            op("dve", lambda e: e.tensor_tensor(out=aT.ap()[:, c, :], in0=xg.ap(), in1=xl.ap(), op=ALU.mult), reads=[xg.t, xl.t], writes=[aT.t])
        for jb in range(NB):
            y = yo[jb % 2]
            py = (ps[6], ps[7])
            for half in range(2):
                for k in range(8):
                    op("pe", lambda e: e.matmul(py[half].ap(), lhsT=aT.ap()[:, k, jb * 128:(jb + 1) * 128], rhs=wdn.ap()[:, k, half * 512:(half + 1) * 512],
                                                start=(k == 0), stop=(k == 7)), reads=[aT.t, wdn.t], writes=[py[half].t])
                op("dve", lambda e: e.tensor_tensor(out=y.ap()[:, half * 512:(half + 1) * 512], in0=py[half].ap(), in1=bd.ap()[:, half * 512:(half + 1) * 512], op=ALU.add),
                   reads=[py[half].t, bd.t], writes=[y.t])
            tys = T("Yst"); self.tl_Ye.append(tys)
            dma("sp", Yv[j * NB + jb], y.ap(), reads=[y.t], writes=[tys])
    self.close_scope()
    if self.dbg:
        for nm, src, n, tt, dt in (("Xe0", XeD, 1024, self.tl_Xe, BF16), ("Ye0", YeD, 1024, self.tl_Ye, F32), ("x1all", x1D, BL * NLAT, self.tl_x1, F32)):
            o = self.dout("dbg_" + nm, [n, D], dt); t = T("dbg_" + nm)
            S.dma("sp", o, src[0:n, :], reads=tt, writes=[t]); self.dbg_outs[nm] = t
        self.dump("destAll2", self.destAll, [128, 32, 4], I32)
        self.dump("probAll2", self.probAll, [128, 32, 4])
    self.open_scope()
    gfin = self.sb("gfin", [128, D]); dma("sp", gfin.ap(), L["gfin_d"][0, :].partition_broadcast(128), writes=[gfin.t])
    mod5 = self.sb("mod5", [128, D])
    x1 = [self.sb(f"x1_{i}", [128, D]) for i in range(2)]
    yk = [self.sb(f"yk{i}", [128, D]) for i in range(4)]
    acc = self.sb("acc", [128, D]); sq = self.sb("sqF", [128, D]); st = self.sb("stF", [128, 4])
    ob = [self.sb(f"ob{i}", [128, D]) for i in range(2)]
    destAll, probAll = self.destAll, self.probAll
    out_d = L["out_d"]
    for gt in range(32):
        b, i = gt // 16, gt % 16
        if i == 0:
            dma("sp", mod5.ap(), modD[b, 5 * D:6 * D].partition_broadcast(128), reads=[self.t_modD], writes=[mod5.t])
        r0 = gt * 128
        xb, o = x1[gt % 2], ob[gt % 2]
        dma("sp", xb.ap(), x1D[r0:r0 + 128, :], reads=[self.tl_x1[gt]], writes=[xb.t])
        for k in range(4):
            dma("pool", None, None, reads=self.tl_Ye + [self.ydest.t], writes=[yk[k].t],
                fn=lambda e: e.indirect_dma_start(out=yk[k].ap(), out_offset=None, in_=YeD,
                                                  in_offset=bass.IndirectOffsetOnAxis(ap=self.ydest.ap()[:, gt, k:k + 1], axis=0)))
        op("dve", lambda e: e.tensor_scalar(out=acc.ap(), in0=yk[0].ap(), scalar1=probAll.ap()[:, gt, 0:1], scalar2=None, op0=ALU.mult),
           reads=[yk[0].t, probAll.t], writes=[acc.t])
        for k in range(1, 4):
            op("dve", lambda e: e.scalar_tensor_tensor(out=acc.ap(), in0=yk[k].ap(), scalar=probAll.ap()[:, gt, k:k + 1], in1=acc.ap(), op0=ALU.mult, op1=ALU.add),
               reads=[yk[k].t, probAll.t, acc.t], writes=[acc.t])
        op("dve", lambda e: e.tensor_tensor(out=acc.ap(), in0=acc.ap(), in1=mod5.ap(), op=ALU.mult), reads=[acc.t, mod5.t], writes=[acc.t])
        op("dve", lambda e: e.tensor_tensor(out=xb.ap(), in0=xb.ap(), in1=acc.ap(), op=ALU.add), reads=[acc.t, xb.t], writes=[xb.t])
        op("act", lambda e: e.activation(out=sq.ap(), in_=xb.ap(), func=AF.Square, accum_out=st.ap()[:, 0:1]), reads=[xb.t], writes=[sq.t, st.t])
        op("act", lambda e: e.activation(out=st.ap()[:, 1:2], in_=st.ap()[:, 0:1], func=AF.Ln, scale=1.0 / D, bias=EPS), reads=[st.t], writes=[st.t])
        op("act", lambda e: e.activation(out=st.ap()[:, 2:3], in_=st.ap()[:, 1:2], func=AF.Exp, scale=-0.5), reads=[st.t], writes=[st.t])
        op("dve", lambda e: e.scalar_tensor_tensor(out=o.ap(), in0=xb.ap(), scalar=st.ap()[:, 2:3], in1=gfin.ap(), op0=ALU.mult, op1=ALU.mult),
           reads=[xb.t, st.t, gfin.t], writes=[o.t])
        tos = T("outst"); self.tl_out.append(tos)
        dma("sp", out_d[r0:r0 + 128, :], o.ap(), reads=[o.t], writes=[tos])


Prog.batch = _batch
Prog.hgrn = _hgrn
Prog.merge_ffn_in = _merge_ffn_in
Prog.moe = _moe
```

```python
from contextlib import ExitStack
import numpy as np
import ml_dtypes
import concourse.bass as bass
import concourse.mybir as mybir
from concourse.bass_utils import run_bass_kernel_spmd

F32 = mybir.dt.float32
BF16 = mybir.dt.bfloat16
I32 = mybir.dt.int32
U32 = mybir.dt.uint32
AF = mybir.ActivationFunctionType
ALU = mybir.AluOpType
AX = mybir.AxisListType

NCORES = 8
BL = 2
NLAT = 2048
NCTX = 256
NTOK = NLAT + NCTX
D = 1024
DIN = 8256
EPS = 1e-6
NEXP = 32
HC = 32
NCH = 128 // HC
CAP = 4096
SLAB = 256
NB = SLAB // 128
NSL = BL * NLAT * 4 // SLAB + NEXP


class T:
    __slots__ = ("name", "w", "r")

    def __init__(self, name=""):
        self.name = name
        self.w = None
        self.r = {}


class Sched:
    def __init__(self, nc, n_dma_sems=16):
        self.nc = nc
        self.eng = {}
        for nm, h in (("pe", nc.tensor), ("act", nc.scalar), ("dve", nc.vector), ("pool", nc.gpsimd), ("sp", nc.sync)):
            sem = nc.alloc_semaphore(f"s_{nm}")
            self.eng[nm] = dict(h=h, sem=sem, cnt=0, seen={}, name=nm)
        self.dq = {}
        for nm, nq in (("sp", 32), ("pool", 40), ("act", 2)):
            sems = [nc.alloc_semaphore(f"d_{nm}{i}") for i in range(nq)]
            self.dq[nm] = dict(sems=sems, tgt=[0] * nq, i=0)
        self.nwaits = 0
        self.nops = 0

    def _wait(self, E, deps, skip_same=False):
        best = {}
        for d in deps:
            if d is None:
                continue
            sem, cnt = d
            if skip_same and sem is E["sem"]:
                continue
            k = id(sem)
            if k not in best or best[k][1] < cnt:
                best[k] = (sem, cnt)
        for k, (sem, cnt) in best.items():
            if E["seen"].get(k, 0) >= cnt:
                continue
            E["h"].wait_ge(sem, cnt)
            E["seen"][k] = cnt
            self.nwaits += 1

    @staticmethod
    def _deps(reads, writes):
        deps = []
        for t in reads:
            deps.append(t.w)
        for t in writes:
            deps.append(t.w)
            deps.extend(t.r.values())
        return deps

    @staticmethod
    def _commit(tok, reads, writes):
        for t in writes:
            t.w = tok
            t.r = {}
        for t in reads:
            if t.w is not tok:
                t.r[id(tok[0])] = tok

    def op(self, e, fn, reads=(), writes=()):
        E = self.eng[e]
        self._wait(E, self._deps(reads, writes), skip_same=(e == "pe"))
        ins = fn(E["h"])
        E["cnt"] += 1
        ins.then_inc(E["sem"], 1)
        self.nops += 1
        self._commit((E["sem"], E["cnt"]), reads, writes)
        return ins

    def dma(self, q, out, in_, reads=(), writes=(), fn=None, **kw):
        E = self.eng[q]
        Q = self.dq[q]
        i = Q["i"] % len(Q["sems"])
        Q["i"] += 1
        sem = Q["sems"][i]
        deps = self._deps(reads, writes)
        if Q["tgt"][i] > 0:
            deps.append((sem, Q["tgt"][i]))
        self._wait(E, deps)
        if fn is not None:
            ins = fn(E["h"])
        else:
            ins = E["h"].dma_start(out=out, in_=in_, **kw)
        Q["tgt"][i] += 16
        ins.then_inc(sem, 16)
        self.nops += 1
        self._commit((sem, Q["tgt"][i]), reads, writes)
        return ins

    def barrier(self):
        toks = [(E["sem"], E["cnt"]) for E in self.eng.values() if E["cnt"] > 0]
        for Q in self.dq.values():
            for s, t in zip(Q["sems"], Q["tgt"]):
                if t > 0:
                    toks.append((s, t))
        for E in self.eng.values():
            self._wait(E, toks)

    def finish(self, tiles):
        self._wait(self.eng["sp"], [t.w for t in tiles])


class Buf:
    def __init__(self, h, name):
        self.h = h
        self.t = T(name)

    def ap(self):
        return self.h.ap()


def host_consts():
    c = {}
    c["ident_f"] = np.eye(128, dtype=np.float32)
    rows = NLAT // 64
    row = np.repeat(np.arange(rows), 64).astype(np.float32)
    col = np.tile(np.arange(64), rows).astype(np.float32)
    inv = (np.float32(10000.0) ** (-np.arange(16, dtype=np.float32) / np.float32(16))).astype(np.float32)
    ang = np.concatenate([row[:, None] * inv, col[:, None] * inv], axis=-1).astype(np.float32)
    cos = np.cos(ang).astype(np.float32).T
    sin = np.sin(ang).astype(np.float32).T
    c["rope_cc"] = np.ascontiguousarray(np.concatenate([cos, cos], 0))
    c["rope_ss"] = np.ascontiguousarray(np.concatenate([sin, sin], 0))
    s = np.arange(128)[:, None]
    t = np.arange(128)[None, :]
    same = (s // HC) == (t // HC)
    tri = np.stack([
        same & (s <= t),
        same & (s > t),
        same & (s >= t),
        same & (s < t),
    ]).astype(np.float32)
    c["tri"] = np.ascontiguousarray(tri.transpose(1, 0, 2))
    ind = np.zeros((128, NCH), np.float32)
    for cix in range(NCH):
        ind[cix * HC:(cix + 1) * HC, cix] = 1
    c["chunk_ind"] = ind
    c["ustrict"] = (s < t).astype(np.float32)
    c["iota_e"] = np.tile(np.arange(NEXP, dtype=np.float32)[None, :], (128, 1))
    c["iota_cap"] = np.ascontiguousarray(c["iota_e"] * CAP)
    c["jota"] = np.tile(np.arange(NSL, dtype=np.float32)[None, :], (128, 1))
    c["pidx"] = np.tile(np.arange(128, dtype=np.float32)[:, None], (1, NSL))
    c["off4"] = (np.arange(128, dtype=np.float32)[:, None] + 128.0 * np.arange(NB, dtype=np.float32)[None, :])
    return c


C_QA, C_KV, C_KR, C_HQ, C_FF, C_FB, C_HI, C_HG, C_MM, C_MH = 0, 768, 1024, 1088, 2112, 3136, 4160, 5184, 6208, 7232


class Prog:
    def __init__(self, stop_after=None, dbg=False):
        self.stop_after = stop_after
        self.dbg = dbg
        nc = self.nc = bass.Bass("TRN2", target_bir_lowering=False)
        self.S = Sched(nc)
        self.inp = {}
        self.dbg_outs = {}
        self.psn = 0
        self.scopes = []

    def din(self, name, shape, dt=F32):
        ap = self.nc.dram_tensor(name, list(shape), dt, kind="ExternalInput").ap()
        self.inp[name] = ap
        return ap

    def dscr(self, name, shape, dt=F32):
        return self.nc.dram_tensor(name, list(shape), dt, kind="Internal").ap()

    def dout(self, name, shape, dt=F32):
        return self.nc.dram_tensor(name, list(shape), dt, kind="ExternalOutput").ap()

    def sb(self, name, shape, dt=F32):
        self.uid = getattr(self, "uid", 0) + 1
        nm = f"s_{name}_{self.uid}"
        if self.scopes:
            h = self.scopes[-1].enter_context(self.nc.sbuf_tensor(nm, list(shape), dt))
        else:
            h = self.nc.alloc_sbuf_tensor(nm, list(shape), dt)
        return Buf(h, name)

    def open_scope(self):
        self.scopes.append(ExitStack())

    def close_scope(self):
        self.S.barrier()
        self.scopes.pop().close()

    def dump(self, name, buf, shape, dt=F32):
        if not self.dbg:
            return
        o = self.dout("dbg_" + name, shape, dt)
        t = T("dbg_" + name)
        self.S.dma("sp", o, buf.ap(), reads=[buf.t], writes=[t])
        self.dbg_outs[name] = t

    def build(self):
        nc, S = self.nc, self.S
        op, dma = S.op, S.dma
        x_d = self.din("x", [BL * NLAT, D])
        ctx_d = self.din("ctx", [BL * NCTX, D])
        cvec_d = self.din("cvec", [128, 8, 3])
        wmod_d = self.din("w_mod", [D, 6 * D])
        bmodT_d = self.din("b_modT", [128, 48])
        gmixT_d = self.din("g_mixT", [128, 8])
        win_d = self.din("w_in", [D, DIN])
        gqT_d = self.din("g_qT", [128, 6])
        wqb_d = self.din("w_q_b", [768, 1536])
        gkvT_d = self.din("g_kvT", [128, 2])
        wkvb_d = self.din("w_kv_b", [256, 2048])
        lbl_d = self.din("lb_logits", [2, 2, D])
        ghgT_d = self.din("g_hgT", [128, 1])
        wout_d = self.din("w_out", [D, D])
        gffn_d = self.din("g_ffn", [1, D])
        wr_d = self.din("w_router", [D, NEXP])
        br_d = self.din("b_router", [1, NEXP])
        wgu_d = self.din("w_gate_up", [NEXP, D, 2 * D])
        bguT_d = self.din("b_guT", [NEXP, 128, 2, 8])
        wd_d = self.din("w_down", [NEXP, D, D])
        bd_d = self.din("b_down", [NEXP, D])
        gfin_d = self.din("g_fin", [1, D])
        identf_d = self.din("ident_f", [128, 128])
        cc_d = self.din("rope_cc", [64, NLAT])
        ss_d = self.din("rope_ss", [64, NLAT])
        tri_d = self.din("tri", [128, 4, 128])
        cind_d = self.din("chunk_ind", [128, NCH])
        ustr_d = self.din("ustrict", [128, 128])
        iotae_d = self.din("iota_e", [128, NEXP])
        iotac_d = self.din("iota_cap", [128, NEXP])
        jota_d = self.din("jota", [128, NSL])
        pidx_d = self.din("pidx", [128, NSL])
        off4_d = self.din("off4", [128, NB])
        out_d = self.dout("out", [BL * NLAT, D])
        self.t_out = T("out")
        modD = self.dscr("modD", [3, 6 * D])
        ymlaD = self.dscr("ymlaD", [BL, 128, 8, NLAT], BF16)
        self.t_ymlaD = T("ymlaD")
        yhD = self.dscr("yhD", [BL, 128, 8, NLAT], BF16)
        self.t_yhD = T("yhD")
        x1D = self.dscr("x1D", [BL * NLAT, D])
        self.t_x1D = T("x1D")
        XeD = self.dscr("XeD", [NEXP * CAP, D], BF16)
        self.t_XeD = T("XeD")
        YeD = self.dscr("YeD", [NSL * SLAB, D])
        self.t_YeD = T("YeD")
        self.tl_Xe, self.tl_Ye, self.tl_out = [], [], []
        self.tl_x1 = [T(f"x1D{i}") for i in range(32)]

        ident_f = self.sb("ident_f_sb", [128, 128])
        ident_b = self.sb("ident_b", [128, 128], BF16)
        ones_f = self.sb("ones_f", [128, 128])
        ones_b = self.sb("ones_b", [128, 128], BF16)
        dma("sp", ident_f.ap(), identf_d, writes=[ident_f.t])
        op("dve", lambda e: e.tensor_copy(out=ident_b.ap(), in_=ident_f.ap()), reads=[ident_f.t], writes=[ident_b.t])
        op("pool", lambda e: e.memset(ones_f.ap(), 1.0), writes=[ones_f.t])
        op("pool", lambda e: e.memset(ones_b.ap(), 1.0), writes=[ones_b.t])
        self.ident_f, self.ident_b, self.ones_f, self.ones_b = ident_f, ident_b, ones_f, ones_b
        self.ps = [Buf(nc.alloc_psum_tensor(f"ps{i}", [128, 512], F32), f"ps{i}") for i in range(8)]

        bmodT = self.sb("bmodT", [128, 48])
        modT = self.sb("modT", [128, 48, 3])
        gmixT = self.sb("gmixT", [128, 8])
        G1 = self.sb("G1", [128, 8, 3])
        self.open_scope()
        zt = self.sb("zeros", [128, 2048], BF16)
        op("pool", lambda e: e.memset(zt.ap(), 0.0), writes=[zt.t])
        zsem = nc.alloc_semaphore("zfill")
        Xv = XeD.rearrange("(a p j) d -> a p (j d)", p=128, j=2)
        S._wait(S.eng["act"], [zt.t.w])
        NZ = SLAB // 256
        for a in range(NZ):
            nc.scalar.dma_start(out=Xv[a], in_=zt.ap()).then_inc(zsem, 16)
        self.t_XeD.w = (zsem, 16 * NZ)
        cvec = self.sb("cvec", [128, 8, 3])
        sig = self.sb("csig", [128, 8, 3])
        scb = self.sb("scb", [128, 8, 3], BF16)
        dma("sp", cvec.ap(), cvec_d, writes=[cvec.t])
        op("act", lambda e: e.activation(out=sig.ap(), in_=cvec.ap(), func=AF.Sigmoid), reads=[cvec.t], writes=[sig.t])
        op("dve", lambda e: e.tensor_mul(out=scb.ap(), in0=cvec.ap(), in1=sig.ap()), reads=[cvec.t, sig.t], writes=[scb.t])
        dma("sp", bmodT.ap(), bmodT_d, writes=[bmodT.t])
        wm = [self.sb(f"wmod{i}", [128, 8, 1536], BF16) for i in range(2)]
        pm = self.ps[0]
        wmod_v = wmod_d.rearrange("(k p) c -> p k c", p=128)
        for pc in range(4):
            w = wm[pc % 2]
            dma("pool", w.ap(), wmod_v[:, :, pc * 1536:(pc + 1) * 1536], writes=[w.t])
            for jj in range(12):
                j = pc * 12 + jj
                for k in range(8):
                    op("pe", lambda e: e.matmul(pm.ap()[:, j * 3:(j + 1) * 3], lhsT=w.ap()[:, k, jj * 128:(jj + 1) * 128],
                                                rhs=scb.ap()[:, k, :], start=(k == 0), stop=(k == 7)),
                       reads=[w.t, scb.t], writes=[pm.t])
        op("dve", lambda e: e.tensor_tensor(out=modT.ap(), in0=pm.ap()[:, 0:144].rearrange("p (j v) -> p j v", v=3),
                                            in1=bmodT.ap().unsqueeze(2).to_broadcast([128, 48, 3]), op=ALU.add),
           reads=[pm.t, bmodT.t], writes=[modT.t])
        t_modD = T("modD")
        with nc.allow_non_contiguous_dma(reason="one-time tiny modulation transpose"):
            for v in range(3):
                dma("sp", modD[v, :].rearrange("(j p) -> p j", p=128), modT.ap()[:, :, v], reads=[modT.t], writes=[t_modD])
        self.modD, self.t_modD = modD, t_modD
        dma("sp", gmixT.ap(), gmixT_d, writes=[gmixT.t])
        op("dve", lambda e: e.scalar_tensor_tensor(out=G1.ap(), in0=modT.ap()[:, 8:16, :], scalar=1.0,
                                                   in1=gmixT.ap().unsqueeze(2).to_broadcast([128, 8, 3]), op0=ALU.add, op1=ALU.mult),
           reads=[modT.t, gmixT.t], writes=[G1.t])
        self.dump("modT", modT, [128, 48, 3])
        for E in S.eng.values():
            S._wait(E, [self.t_XeD.w])
        self.close_scope()
        if self.stop_after == "P0":
            return self.finalize()

        gqT = self.sb("gqT", [128, 6]); dma("sp", gqT.ap(), gqT_d, writes=[gqT.t])
        gkvT = self.sb("gkvT", [128, 2]); dma("sp", gkvT.ap(), gkvT_d, writes=[gkvT.t])
        ghgT = self.sb("ghgT", [128, 1]); dma("sp", ghgT.ap(), ghgT_d, writes=[ghgT.t])

        self.base_bc = self.sb("base_bc", [128, NEXP])
        op("pool", lambda e: e.memset(self.base_bc.ap(), 0.0), writes=[self.base_bc.t])
        self.destAll = self.sb("destAll", [128, 32, 4], I32)
        self.probAll = self.sb("probAll", [128, 32, 4])
        self.dstfAll = self.sb("dstfAll", [128, 32, 4])
        self.eidAll = self.sb("eidAll", [128, 32, 4])
        self.ustr = self.sb("ustr", [128, 128], BF16)
        tmpu = self.sb("tmpu", [128, 128])
        dma("sp", tmpu.ap(), ustr_d, writes=[tmpu.t])
        op("dve", lambda e: e.tensor_copy(out=self.ustr.ap(), in_=tmpu.ap()), reads=[tmpu.t], writes=[self.ustr.t])
        self.iota_e = self.sb("iota_e", [128, NEXP]); dma("sp", self.iota_e.ap(), iotae_d, writes=[self.iota_e.t])
        self.iota_c = self.sb("iota_c", [128, NEXP]); dma("sp", self.iota_c.ap(), iotac_d, writes=[self.iota_c.t])
        for b in range(BL):
            self.batch(b, locals())
            if self.stop_after is not None and self.stop_after.startswith("B0"):
                return self.finalize()
        self.moe(locals())
        return self.finalize()

    def finalize(self):
        S = self.S
        tl = list(self.dbg_outs.values())
        tl.extend(self.tl_out)
        S.finish(tl)
        return self.nc


def make_in_maps(inputs):
    f = lambda a: np.ascontiguousarray(np.asarray(a, dtype=np.float32))
    x, c, ctx, c_ctx = f(inputs["x"]), f(inputs["c"]), f(inputs["ctx"]), f(inputs["c_ctx"])
    consts = host_consts()
    featT = lambda v, nk: np.ascontiguousarray(v.reshape(nk, 128).T)
    shared = {
        "w_mod": f(inputs["w_mod"][0]),
        "b_modT": featT(f(inputs["b_mod"][0]), 48),
        "g_mixT": featT(f(inputs["norm_mix_g"][0]), 8),
        "w_in": f(inputs["w_in"][0]),
        "g_qT": featT(f(inputs["mla_q_norm_g"][0]), 6),
        "w_q_b": f(inputs["w_q_b"][0]),
        "g_kvT": featT(f(inputs["mla_kv_norm_g"][0]), 2),
        "w_kv_b": f(inputs["w_kv_b"][0]),
        "lb_logits": f(inputs["hg_lb_logits"]),
        "g_hgT": featT(f(inputs["hg_norm_g"][0]), 1),
        "w_out": f(inputs["w_out"][0]),
        "g_ffn": f(inputs["norm_ffn_g"][0]).reshape(1, D),
        "w_router": f(inputs["w_router"][0]),
        "b_router": f(inputs["b_router"][0]).reshape(1, NEXP),
        "w_gate_up": f(inputs["w_gate_up"][0]),
        "b_guT": np.ascontiguousarray(f(inputs["b_gate_up"][0]).reshape(NEXP, 128, 8, 2).transpose(0, 1, 3, 2)),
        "w_down": f(inputs["w_down"][0]),
        "b_down": f(inputs["b_down"][0]),
        "g_fin": f(inputs["final_norm_g"]).reshape(1, D),
    }
    shared.update(consts)
    maps = []
    for core in range(NCORES):
        b0 = core * BL
        cv = np.stack([c[b0], c[b0 + 1], c_ctx], axis=-1)
        m = dict(shared)
        m["x"] = np.ascontiguousarray(x[b0:b0 + BL].reshape(BL * NLAT, D))
        m["ctx"] = np.ascontiguousarray(ctx[b0:b0 + BL].reshape(BL * NCTX, D))
        m["cvec"] = np.ascontiguousarray(cv.reshape(8, 128, 3).transpose(1, 0, 2))
        maps.append(m)
    return maps


def kernel(**inputs):
    prog = Prog()
    nc = prog.build()
    in_maps = make_in_maps(inputs)
    in_maps = [{k: v for k, v in m.items() if k in prog.inp} for m in in_maps]
    res = run_bass_kernel_spmd(nc, in_maps, core_ids=list(range(NCORES)))
    outs = [np.asarray(r["out"], dtype=np.float32).reshape(BL, NLAT, D) for r in res.results]
    return np.concatenate(outs, axis=0)


def _batch(self, b, L):
    nc, S = self.nc, self.S
    op, dma = S.op, S.dma
    ps = self.ps
    ident_f, ident_b, ones_f, ones_b = self.ident_f, self.ident_b, self.ones_f, self.ones_b
    G1, modT = L["G1"], L["modT"]
    x_d, ctx_d, win_d = L["x_d"], L["ctx_d"], L["win_d"]
    win_v = win_d.rearrange("(k p) c -> p k c", p=128)
    first = (b == 0)

    def psum(i):
        return ps[i % 8]

    self.open_scope()
    if True:
        self.hT = self.sb("hT", [128, 8, NTOK], BF16)
        self.hT_t = [T(f"hT{i}") for i in range(18)]
        self.open_scope()
        self.xt = [self.sb(f"xt{i}", [128, D]) for i in range(2)]
        self.sq = self.sb("sqscr", [128, D])
        self.st = [self.sb(f"st{i}", [128, 4]) for i in range(2)]
    hT, hT_t, xt, sq, st = self.hT, self.hT_t, self.xt, self.sq, self.st
    for i in range(18):
        xb, sb_ = xt[i % 2], st[i % 2]
        if i < 16:
            src, v = x_d[b * NLAT + i * 128: b * NLAT + (i + 1) * 128, :], b
        else:
            src, v = ctx_d[b * NCTX + (i - 16) * 128: b * NCTX + (i - 15) * 128, :], 2
        dma("sp", xb.ap(), src, writes=[xb.t])
        op("act", lambda e: e.activation(out=sq.ap(), in_=xb.ap(), func=AF.Square, accum_out=sb_.ap()[:, 0:1]),
           reads=[xb.t], writes=[sq.t, sb_.t])
        op("act", lambda e: e.activation(out=sb_.ap()[:, 1:2], in_=sb_.ap()[:, 0:1], func=AF.Ln, scale=1.0 / D, bias=EPS),
           reads=[sb_.t], writes=[sb_.t])
        op("act", lambda e: e.activation(out=sb_.ap()[:, 2:3], in_=sb_.ap()[:, 1:2], func=AF.Exp, scale=-0.5),
           reads=[sb_.t], writes=[sb_.t])
        op("dve", lambda e: e.tensor_scalar(out=xb.ap(), in0=xb.ap(), scalar1=sb_.ap()[:, 2:3], scalar2=None, op0=ALU.mult),
           reads=[sb_.t, xb.t], writes=[xb.t])
        for half in range(2):
            p = psum(i * 2 + half)
            for kk in range(4):
                k = half * 4 + kk
                op("pe", lambda e: e.transpose(out=p.ap()[:, kk * 128:(kk + 1) * 128], in_=xb.ap()[:, k * 128:(k + 1) * 128],
                                               identity=ident_f.ap()), reads=[xb.t, ident_f.t], writes=[p.t])
            for kk in range(4):
                k = half * 4 + kk
                eng = "act" if kk % 2 == 0 else "dve"
                if eng == "act":
                    op("act", lambda e: e.activation(out=hT.ap()[:, k, i * 128:(i + 1) * 128], in_=p.ap()[:, kk * 128:(kk + 1) * 128],
                                                     func=AF.Identity, scale=G1.ap()[:, k, v:v + 1], bias=modT.ap()[:, k, v:v + 1]),
                       reads=[p.t, G1.t, modT.t], writes=[hT_t[i]])
                else:
                    op("dve", lambda e: e.tensor_scalar(out=hT.ap()[:, k, i * 128:(i + 1) * 128], in0=p.ap()[:, kk * 128:(kk + 1) * 128],
                                                        scalar1=G1.ap()[:, k, v:v + 1], scalar2=modT.ap()[:, k, v:v + 1],
                                                        op0=ALU.mult, op1=ALU.add),
                       reads=[p.t, G1.t, modT.t], writes=[hT_t[i]])
    if self.dbg and first:
        allh = T("allh")
        o = self.dout("dbg_hT", [128, 8, NTOK], BF16)
        S.dma("sp", o, hT.ap(), reads=hT_t, writes=[allh])
        self.dbg_outs["hT"] = allh
    if self.stop_after == "B0P1":
        return
    self.close_scope()

    self.open_scope()
    if True:
        self.wA = self.sb("wA", [128, 8, 1088], BF16)
        self.wAs = self.sb("wAs", [128, 8, 64], BF16)
        dma("pool", self.wA.ap(), win_v[:, :, 0:1088], writes=[self.wA.t])
        op("act", lambda e: e.mul(out=self.wAs.ap()[:, :, 0:32], in_=self.wA.ap()[:, :, 1056:1088], mul=-1.0),
           reads=[self.wA.t], writes=[self.wAs.t])
        op("act", lambda e: e.copy(out=self.wAs.ap()[:, :, 32:64], in_=self.wA.ap()[:, :, 1024:1056]),
           reads=[self.wA.t], writes=[self.wAs.t])
        self.qaT = self.sb("qaT", [128, 6, NLAT], BF16)
        self.ckvT = self.sb("ckvT", [128, 2, NTOK], BF16)
        self.krT = self.sb("krT", [64, NTOK], BF16)
        self.cc = self.sb("cc", [64, NLAT]); dma("sp", self.cc.ap(), L["cc_d"], writes=[self.cc.t])
        self.ss = self.sb("ss", [64, NLAT]); dma("sp", self.ss.ap(), L["ss_d"], writes=[self.ss.t])
        self.sqf = [self.sb(f"sqf{i}", [128, 512]) for i in range(2)]
        self.rstd_bc = self.sb("rstd_bc", [128, 512])
        self.rtmp = [self.sb(f"rtmp{i}", [64, 512]) for i in range(2)]
        self.wkvb = self.sb("wkvb", [128, 2, 2048], BF16)
        dma("pool", self.wkvb.ap(), L["wkvb_d"].rearrange("(k p) c -> p k c", p=128), writes=[self.wkvb.t])
    wA, wAs, qaT, ckvT, krT, cc, ss, sqf, rstd_bc, rtmp, wkvb = (self.wA, self.wAs, self.qaT, self.ckvT, self.krT, self.cc,
                                                                 self.ss, self.sqf, self.rstd_bc, self.rtmp, self.wkvb)
    gqT, gkvT = L["gqT"], L["gkvT"]
    slabs = [(s * 512, 512) for s in range(4)] + [(NLAT, NCTX)]
    hdeps = lambda c0, n: hT_t[c0 // 128:(c0 + n) // 128]

    def norm_group(cols0, nch, dst, g, c0, n, nfeat, pbase):
        pbs = [psum(pbase + m) for m in range(nch)]
        pss = psum(pbase + nch)
        for m in range(nch):
            for k in range(8):
                op("pe", lambda e: e.matmul(pbs[m].ap()[:, 0:n], lhsT=wA.ap()[:, k, cols0 + m * 128: cols0 + (m + 1) * 128],
                                            rhs=hT.ap()[:, k, c0:c0 + n], start=(k == 0), stop=(k == 7)),
                   reads=[wA.t] + hdeps(c0, n), writes=[pbs[m].t])
        for m in range(nch):
            q = sqf[m % 2]
            op("act", lambda e: e.activation(out=q.ap()[:, 0:n], in_=pbs[m].ap()[:, 0:n], func=AF.Square), reads=[pbs[m].t], writes=[q.t])
            op("pe", lambda e: e.matmul(pss.ap()[:, 0:n], lhsT=ones_f.ap(), rhs=q.ap()[:, 0:n], start=(m == 0), stop=(m == nch - 1)),
               reads=[ones_f.t, q.t], writes=[pss.t])
        op("act", lambda e: e.activation(out=rstd_bc.ap()[:, 0:n], in_=pss.ap()[:, 0:n], func=AF.Ln, scale=1.0 / nfeat, bias=EPS),
           reads=[pss.t], writes=[rstd_bc.t])
        op("act", lambda e: e.activation(out=rstd_bc.ap()[:, 0:n], in_=rstd_bc.ap()[:, 0:n], func=AF.Exp, scale=-0.5),
           reads=[rstd_bc.t], writes=[rstd_bc.t])
        for m in range(nch):
            op("dve", lambda e: e.scalar_tensor_tensor(out=dst.ap()[:, m, c0:c0 + n], in0=pbs[m].ap()[:, 0:n], scalar=g.ap()[:, m:m + 1],
                                                       in1=rstd_bc.ap()[:, 0:n], op0=ALU.mult, op1=ALU.mult),
               reads=[pbs[m].t, g.t, rstd_bc.t], writes=[dst.t])

    for (c0, n) in slabs:
        if c0 < NLAT:
            norm_group(C_QA, 6, qaT, gqT, c0, n, 768, 0)
        norm_group(C_KV, 2, ckvT, gkvT, c0, n, 256, 0)
        pk, pks = psum(3), psum(4)
        for k in range(8):
            op("pe", lambda e: e.matmul(pk.ap()[0:64, 0:n], lhsT=wA.ap()[:, k, C_KR:C_KR + 64], rhs=hT.ap()[:, k, c0:c0 + n],
                                        start=(k == 0), stop=(k == 7)), reads=[wA.t] + hdeps(c0, n), writes=[pk.t])
        if c0 < NLAT:
            for k in range(8):
                op("pe", lambda e: e.matmul(pks.ap()[0:64, 0:n], lhsT=wAs.ap()[:, k, :], rhs=hT.ap()[:, k, c0:c0 + n],
                                            start=(k == 0), stop=(k == 7)), reads=[wAs.t] + hdeps(c0, n), writes=[pks.t])
            op("dve", lambda e: e.tensor_tensor(out=rtmp[0].ap()[:, 0:n], in0=pk.ap()[0:64, 0:n], in1=cc.ap()[:, c0:c0 + n], op=ALU.mult),
               reads=[pk.t, cc.t], writes=[rtmp[0].t])
            op("dve", lambda e: e.tensor_tensor(out=rtmp[1].ap()[:, 0:n], in0=pks.ap()[0:64, 0:n], in1=ss.ap()[:, c0:c0 + n], op=ALU.mult),
               reads=[pks.t, ss.t], writes=[rtmp[1].t])
            op("dve", lambda e: e.tensor_tensor(out=krT.ap()[:, c0:c0 + n], in0=rtmp[0].ap()[:, 0:n], in1=rtmp[1].ap()[:, 0:n], op=ALU.add),
               reads=[rtmp[0].t, rtmp[1].t], writes=[krT.t])
        else:
            op("act", lambda e: e.copy(out=krT.ap()[:, c0:c0 + n], in_=pk.ap()[0:64, 0:n]), reads=[pk.t], writes=[krT.t])
    if self.dbg and first:
        self.dump("qaT", qaT, [128, 6, NLAT], BF16)
        self.dump("ckvT", ckvT, [128, 2, NTOK], BF16)
        self.dump("krT", krT, [64, NTOK], BF16)
    if self.stop_after == "B0P2a":
        return

    if True:
        self.wq = [self.sb(f"wq{i}", [128, 6, 192], BF16) for i in range(2)]
        self.wqs = [self.sb(f"wqs{i}", [128, 6, 64], BF16) for i in range(2)]
        self.qT = self.sb("qT", [128, NLAT], BF16)
        self.qrT = self.sb("qrT", [64, NLAT], BF16)
        self.kT = self.sb("kT", [128, NTOK], BF16)
        self.vh = self.sb("vh", [128, 18, 128], BF16)
        self.PT = [self.sb(f"PT{i}", [128, 512], BF16) for i in range(4)]
        self.rs = self.sb("rs", [128, 512])
        self.yT = self.sb("yT", [128, 8, NLAT], BF16)
    wq, wqs, qT, qrT, kT, vh, PT, rs, yT = self.wq, self.wqs, self.qT, self.qrT, self.kT, self.vh, self.PT, self.rs, self.yT
    wqb_v = L["wqb_d"].rearrange("(k p) c -> p k c", p=128)
    scale = float(192 ** -0.5)
    for h in range(8):
        w, ws = wq[h % 2], wqs[h % 2]
        dma("pool", w.ap(), wqb_v[:, :, h * 192:(h + 1) * 192], writes=[w.t])
        op("act", lambda e: e.mul(out=ws.ap()[:, :, 0:32], in_=w.ap()[:, :, 160:192], mul=-1.0), reads=[w.t], writes=[ws.t])
        op("act", lambda e: e.copy(out=ws.ap()[:, :, 32:64], in_=w.ap()[:, :, 128:160]), reads=[w.t], writes=[ws.t])
        for s in range(4):
            c0 = s * 512
            pq, pr, prs = psum(0), psum(1), psum(2)
            for k in range(6):
                op("pe", lambda e: e.matmul(pq.ap(), lhsT=w.ap()[:, k, 0:128], rhs=qaT.ap()[:, k, c0:c0 + 512], start=(k == 0), stop=(k == 5)),
                   reads=[w.t, qaT.t], writes=[pq.t])
            for k in range(6):
                op("pe", lambda e: e.matmul(pr.ap()[0:64, :], lhsT=w.ap()[:, k, 128:192], rhs=qaT.ap()[:, k, c0:c0 + 512], start=(k == 0), stop=(k == 5)),
                   reads=[w.t, qaT.t], writes=[pr.t])
            for k in range(6):
                op("pe", lambda e: e.matmul(prs.ap()[0:64, :], lhsT=ws.ap()[:, k, :], rhs=qaT.ap()[:, k, c0:c0 + 512], start=(k == 0), stop=(k == 5)),
                   reads=[ws.t, qaT.t], writes=[prs.t])
            op("act", lambda e: e.copy(out=qT.ap()[:, c0:c0 + 512], in_=pq.ap()), reads=[pq.t], writes=[qT.t])
            op("dve", lambda e: e.tensor_tensor(out=rtmp[0].ap(), in0=pr.ap()[0:64, :], in1=cc.ap()[:, c0:c0 + 512], op=ALU.mult),
               reads=[pr.t, cc.t], writes=[rtmp[0].t])
            op("dve", lambda e: e.tensor_tensor(out=rtmp[1].ap(), in0=prs.ap()[0:64, :], in1=ss.ap()[:, c0:c0 + 512], op=ALU.mult),
               reads=[prs.t, ss.t], writes=[rtmp[1].t])
            op("dve", lambda e: e.tensor_tensor(out=qrT.ap()[:, c0:c0 + 512], in0=rtmp[0].ap(), in1=rtmp[1].ap(), op=ALU.add),
               reads=[rtmp[0].t, rtmp[1].t], writes=[qrT.t])
        for si, (c0, n) in enumerate(slabs):
            pk = psum(3 + si % 2)
            for k in range(2):
                op("pe", lambda e: e.matmul(pk.ap()[:, 0:n], lhsT=wkvb.ap()[:, k, h * 256:h * 256 + 128], rhs=ckvT.ap()[:, k, c0:c0 + n],
                                            start=(k == 0), stop=(k == 1)), reads=[wkvb.t, ckvT.t], writes=[pk.t])
            op("act", lambda e: e.copy(out=kT.ap()[:, c0:c0 + n], in_=pk.ap()[:, 0:n]), reads=[pk.t], writes=[kT.t])
        for g in range(5):
            pv = psum(5 + g % 2)
            nj = 4 if g < 4 else 2
            for jj in range(nj):
                j = g * 4 + jj
                for k in range(2):
                    op("pe", lambda e: e.matmul(pv.ap()[:, jj * 128:(jj + 1) * 128], lhsT=ckvT.ap()[:, k, j * 128:(j + 1) * 128],
                                                rhs=wkvb.ap()[:, k, h * 256 + 128:h * 256 + 256], start=(k == 0), stop=(k == 1)),
                       reads=[wkvb.t, ckvT.t], writes=[pv.t])
            op("dve", lambda e: e.tensor_copy(out=vh.ap()[:, g * 4:g * 4 + nj, :], in_=pv.ap()[:, 0:nj * 128].rearrange("p (j d) -> p j d", d=128)),
               reads=[pv.t], writes=[vh.t])
        for s in range(4):
            c0 = s * 512
            po, psm = psum(4 + s % 2), psum(6 + s % 2)

            def smm(kc):
                p = psum(kc % 4)
                op("pe", lambda e: e.matmul(p.ap(), lhsT=kT.ap()[:, kc * 128:(kc + 1) * 128], rhs=qT.ap()[:, c0:c0 + 512], start=True, stop=False),
                   reads=[kT.t, qT.t], writes=[p.t])
                op("pe", lambda e: e.matmul(p.ap(), lhsT=krT.ap()[:, kc * 128:(kc + 1) * 128], rhs=qrT.ap()[:, c0:c0 + 512], start=False, stop=True),
                   reads=[krT.t, qrT.t], writes=[p.t])
                pt = PT[kc % 4]
                op("act", lambda e: e.activation(out=pt.ap(), in_=p.ap(), func=AF.Exp, scale=scale), reads=[p.t], writes=[pt.t])

            def omm(kc):
                pt = PT[kc % 4]
                op("pe", lambda e: e.matmul(po.ap(), lhsT=vh.ap()[:, kc, :], rhs=pt.ap(), start=(kc == 0), stop=(kc == 17)),
                   reads=[vh.t, pt.t], writes=[po.t])
                op("pe", lambda e: e.matmul(psm.ap(), lhsT=ones_b.ap(), rhs=pt.ap(), start=(kc == 0), stop=(kc == 17)),
                   reads=[ones_b.t, pt.t], writes=[psm.t])

            smm(0); smm(1)
            for kc in range(18):
                if kc + 2 < 18:
                    smm(kc + 2)
                omm(kc)
            op("dve", lambda e: e.reciprocal(out=rs.ap(), in_=psm.ap()), reads=[psm.t], writes=[rs.t])
            op("dve", lambda e: e.tensor_tensor(out=yT.ap()[:, h, c0:c0 + 512], in0=po.ap(), in1=rs.ap(), op=ALU.mult),
               reads=[po.t, rs.t], writes=[yT.t])
    if self.dbg and first:
        self.dump("ymlaT", yT, [128, 8, NLAT], BF16)
    if self.stop_after == "B0P2":
        return
    ymlaD = L["ymlaD"]
    dma("sp", ymlaD[b], yT.ap(), reads=[yT.t], writes=[self.t_ymlaD])
    self.close_scope()
    self.hgrn(b, L)
    if self.stop_after == "B0P3":
        return
    self.merge_ffn_in(b, L)
    if self.stop_after == "B0P4":
        return
    self.close_scope()


def _hgrn(self, b, L):
    nc, S = self.nc, self.S
    op, dma = S.op, S.dma
    ps = self.ps
    ident_b, ones_f = self.ident_b, self.ones_f
    hT, hT_t = self.hT, self.hT_t
    win_v = L["win_d"].rearrange("(k p) c -> p k c", p=128)
    self.open_scope()
    wS = self.sb("wS", [128, 8, 4096], BF16)
    for pc in range(4):
        dma("pool", wS.ap()[:, :, pc * 1024:(pc + 1) * 1024], win_v[:, :, C_HQ + pc * 1024:C_HQ + (pc + 1) * 1024], writes=[wS.t])
    tri = self.sb("tri", [128, 4, 128]); dma("sp", tri.ap(), L["tri_d"], writes=[tri.t])
    cind = self.sb("cind", [128, NCH]); dma("sp", cind.ap(), L["cind_d"], writes=[cind.t])
    lb = [self.sb(f"lb{d}", [128, D]) for d in range(2)]
    lnoml = [self.sb(f"lnoml{d}", [128, D]) for d in range(2)]
    uL = [self.sb(f"u{d}", [128, D]) for d in range(2)]; L2L = [self.sb(f"L2{d}", [128, D]) for d in range(2)]
    tA_ = self.sb("tA", [128, D]); tB_ = self.sb("tB", [128, D])
    tAL = [tA_, tA_]; tBL = [tB_, tB_]
    u, L2, tA, tB = uL[0], L2L[0], tAL[0], tBL[0]
    lbl = L["lbl_d"]
    for d in range(2):
        dma("sp", u.ap(), lbl[d, 0, :].partition_broadcast(128), writes=[u.t])
        dma("sp", L2.ap(), lbl[d, 1, :].partition_broadcast(128), writes=[L2.t])
        op("dve", lambda e: e.tensor_tensor(out=tA.ap(), in0=L2.ap(), in1=u.ap(), op=ALU.subtract), reads=[u.t, L2.t], writes=[tA.t])
        op("act", lambda e: e.activation(out=tB.ap(), in_=tA.ap(), func=AF.Exp), reads=[tA.t], writes=[tB.t])
        op("act", lambda e: e.activation(out=tB.ap(), in_=tB.ap(), func=AF.Ln, bias=1.0), reads=[tB.t], writes=[tB.t])
        op("act", lambda e: e.activation(out=lb[d].ap(), in_=tB.ap(), func=AF.Exp, scale=-1.0), reads=[tB.t], writes=[lb[d].t])
        op("dve", lambda e: e.tensor_tensor(out=lnoml[d].ap(), in0=tA.ap(), in1=tB.ap(), op=ALU.subtract), reads=[tA.t, tB.t], writes=[lnoml[d].t])
    Kt = [self.sb(f"Kt{d}", [128, D], BF16) for d in range(2)]
    Kh = [self.sb(f"Kh{d}", [128, D], BF16) for d in range(2)]
    Qt = [self.sb(f"Qt{d}", [128, D], BF16) for d in range(2)]
    vt = [self.sb(f"vt{d}", [128, D], BF16) for d in range(2)]
    QT = [self.sb(f"QT{d}", [128, 8, 128], BF16) for d in range(2)]
    KT = [self.sb(f"KT{d}", [128, 8, 128], BF16) for d in range(2)]
    ATm = [self.sb(f"ATm{d}", [128, 8, 128], BF16) for d in range(2)]
    dec = [self.sb(f"dec{d}", [128, 8, NCH]) for d in range(2)]
    vt3 = [self.sb(f"vt3{d}", [128, D], BF16) for d in range(2)]
    Sf = [self.sb(f"Sf{d}", [128, 8, 128]) for d in range(2)]
    Sb = [self.sb(f"Sb{d}", [128, 8, 128], BF16) for d in range(2)]
    otL = [self.sb(f"ot{d}", [128, 8, 128]) for d in range(2)]
    o1 = self.sb("o1", [128, 8, 128])
    sqo = o1
    yh = self.sb("yh", [128, 8, 128], BF16)
    oD = self.dscr(f"oD{b}", [16, 128, 8, 128])
    t_oD = [T(f"oD{i}") for i in range(16)]
    yhD = L["yhD"]
    for d in range(2):
        op("pool", lambda e: e.memset(Sf[d].ap(), 0.0), writes=[Sf[d].t])
        op("pool", lambda e: e.memset(Sb[d].ap(), 0.0), writes=[Sb[d].t])
    ghgT = L["ghgT"]
    A2 = lambda i: (ps[i], ps[i + 1])

    def tokproj(tile, col0, pair):
        for half in range(2):
            p = pair[half]
            for k in range(8):
                op("pe", lambda e: e.matmul(p.ap(), lhsT=hT.ap()[:, k, tile * 128:(tile + 1) * 128],
                                            rhs=wS.ap()[:, k, col0 + half * 512: col0 + (half + 1) * 512], start=(k == 0), stop=(k == 7)),
                   reads=[hT_t[tile], wS.t], writes=[p.t])

    def two(fn, pair, reads, writes, eng):
        for half in range(2):
            p = pair[half]
            op(eng, lambda e: fn(e, p.ap(), slice(half * 512, (half + 1) * 512)), reads=[p.t] + reads, writes=writes)

    def prep(tile, d, need_o):
        pA, pB, pC, pD = A2(0), A2(2), A2(4), A2(6)
        u, L2, tA, tB = uL[d], L2L[d], tAL[d], tBL[d]
        ti_incl, ti_excl = (0, 1) if d == 0 else (2, 3)
        tokproj(tile, 1024 * (1 + d), pA)
        two(lambda e, p, c: e.activation(out=u.ap()[:, c], in_=p, func=AF.Exp, scale=-1.0), pA, [], [u.t], "act")
        op("act", lambda e: e.activation(out=L2.ap(), in_=u.ap(), func=AF.Ln, bias=1.0), reads=[u.t], writes=[L2.t])
        op("dve", lambda e: e.tensor_tensor(out=u.ap(), in0=u.ap(), in1=lb[d].ap(), op=ALU.mult), reads=[u.t, lb[d].t], writes=[u.t])
        op("act", lambda e: e.activation(out=u.ap(), in_=u.ap(), func=AF.Ln, bias=1.0), reads=[u.t], writes=[u.t])
        op("dve", lambda e: e.tensor_tensor(out=u.ap(), in0=u.ap(), in1=L2.ap(), op=ALU.subtract), reads=[u.t, L2.t], writes=[u.t])
        two(lambda e, p, c: e.scalar_tensor_tensor(out=L2.ap()[:, c], in0=p, scalar=-1.0, in1=L2.ap()[:, c], op0=ALU.mult, op1=ALU.subtract),
            pA, [L2.t], [L2.t], "dve")
        op("dve", lambda e: e.tensor_tensor(out=L2.ap(), in0=L2.ap(), in1=lnoml[d].ap(), op=ALU.add), reads=[L2.t, lnoml[d].t], writes=[L2.t])
        for half in range(2):
            op("pe", lambda e: e.matmul(pB[half].ap(), lhsT=tri.ap()[:, ti_incl, :], rhs=u.ap()[:, half * 512:(half + 1) * 512], start=True, stop=True),
               reads=[tri.t, u.t], writes=[pB[half].t])
            op("pe", lambda e: e.matmul(pC[half].ap(), lhsT=tri.ap()[:, ti_excl, :], rhs=u.ap()[:, half * 512:(half + 1) * 512], start=True, stop=True),
               reads=[tri.t, u.t], writes=[pC[half].t])
        for h in range(8):
            op("pe", lambda e: e.matmul(pD[0].ap()[:, h * NCH:(h + 1) * NCH], lhsT=u.ap()[:, h * 128:(h + 1) * 128], rhs=cind.ap(), start=True, stop=True),
               reads=[u.t, cind.t], writes=[pD[0].t])
        op("act", lambda e: e.activation(out=dec[d].ap(), in_=pD[0].ap()[:, 0:8 * NCH].rearrange("p (h c) -> p h c", c=NCH), func=AF.Exp),
           reads=[pD[0].t], writes=[dec[d].t])
        if need_o:
            two(lambda e, p, c: e.tensor_tensor(out=tA.ap()[:, c], in0=L2.ap()[:, c], in1=p, op=ALU.subtract), pB, [L2.t], [tA.t], "dve")
            op("act", lambda e: e.activation(out=Kt[d].ap(), in_=tA.ap(), func=AF.Exp), reads=[tA.t], writes=[Kt[d].t])
        two(lambda e, p, c: e.tensor_tensor(out=tB.ap()[:, c], in0=L2.ap()[:, c], in1=p, op=ALU.add), pC, [L2.t], [tB.t], "dve")
        op("act", lambda e: e.activation(out=Kh[d].ap(), in_=tB.ap(), func=AF.Exp), reads=[tB.t], writes=[Kh[d].t])
        if need_o:
            two(lambda e, p, c: e.activation(out=tA.ap()[:, c], in_=p, func=AF.Exp), pB, [], [tA.t], "act")
            tokproj(tile, 0, pA)
            two(lambda e, p, c: e.activation(out=tB.ap()[:, c], in_=p, func=AF.Exp, scale=-1.0), pA, [], [tB.t], "act")
            op("act", lambda e: e.activation(out=tB.ap(), in_=tB.ap(), func=AF.Ln, bias=1.0), reads=[tB.t], writes=[tB.t])
            op("act", lambda e: e.activation(out=tB.ap(), in_=tB.ap(), func=AF.Exp, scale=-1.0), reads=[tB.t], writes=[tB.t])
            two(lambda e, p, c: e.scalar_tensor_tensor(out=tB.ap()[:, c], in0=p, scalar=float(128 ** -0.5), in1=tB.ap()[:, c], op0=ALU.mult, op1=ALU.mult),
                pA, [tB.t], [tB.t], "dve")
            op("dve", lambda e: e.tensor_tensor(out=Qt[d].ap(), in0=tB.ap(), in1=tA.ap(), op=ALU.mult), reads=[tA.t, tB.t], writes=[Qt[d].t])
        tokproj(tile, 3072, pC)
        two(lambda e, p, c: e.copy(out=vt[d].ap()[:, c], in_=p), pC, [], [vt[d].t], "act")
        two(lambda e, p, c: e.activation(out=vt3[d].ap()[:, c], in_=p, func=AF.Identity, scale=cind.ap()[:, NCH - 1:NCH]), pC, [cind.t], [vt3[d].t], "act")
        if need_o:
            for (src, dst, pp) in ((Qt[d], QT[d], pB[0]), (Kt[d], KT[d], pB[1])):
                pv = pp.ap().bitcast(BF16)
                for h in range(8):
                    op("pe", lambda e: e.transpose(out=pv[:, h * 128:(h + 1) * 128], in_=src.ap()[:, h * 128:(h + 1) * 128], identity=ident_b.ap()),
                       reads=[src.t, ident_b.t], writes=[pp.t])
                op("act", lambda e: e.copy(out=dst.ap(), in_=pv.rearrange("p (h t) -> p h t", t=128)), reads=[pp.t], writes=[dst.t])
            for h in range(8):
                p = pA[h // 4]
                op("pe", lambda e: e.matmul(p.ap()[:, (h % 4) * 128:(h % 4 + 1) * 128], lhsT=KT[d].ap()[:, h, :], rhs=QT[d].ap()[:, h, :], start=True, stop=True),
                   reads=[KT[d].t, QT[d].t], writes=[p.t])
            for half in range(2):
                op("dve", lambda e: e.tensor_tensor(out=ATm[d].ap()[:, half * 4:(half + 1) * 4, :],
                                                    in0=pA[half].ap().rearrange("p (h t) -> p h t", t=128),
                                                    in1=tri.ap()[:, ti_incl:ti_incl + 1, :].to_broadcast([128, 4, 128]), op=ALU.mult),
                   reads=[pA[half].t, tri.t], writes=[ATm[d].t])

    def rec_parts(tile, d, need_o):
        pO = A2(6) if d == 0 else A2(0)
        pS = A2(2) if d == 0 else A2(4)
        ot = otL[d]
        corder = tuple(range(NCH)) if d == 0 else tuple(range(NCH - 1, -1, -1))

        def pre():
            if not need_o:
                return
            for h in range(8):
                p = pO[h // 4]
                cs = slice((h % 4) * 128, (h % 4 + 1) * 128)
                op("pe", lambda e: e.matmul(p.ap()[:, cs], lhsT=vt[d].ap()[:, h * 128:(h + 1) * 128], rhs=ATm[d].ap()[:, h, :], start=True, stop=True),
                   reads=[vt[d].t, ATm[d].t], writes=[p.t])
            for half in range(2):
                op("act", lambda e: e.copy(out=ot.ap()[:, half * 4:(half + 1) * 4, :], in_=pO[half].ap().rearrange("p (h t) -> p h t", t=128)),
                   reads=[pO[half].t], writes=[ot.t])

        def chunk(c):
            if need_o:
                for h in range(8):
                    p = pO[h // 4]
                    cs = slice((h % 4) * 128 + c * HC, (h % 4) * 128 + (c + 1) * HC)
                    op("pe", lambda e: e.matmul(p.ap()[:, cs], lhsT=Sb[d].ap()[:, h, :], rhs=QT[d].ap()[:, h, c * HC:(c + 1) * HC], start=True, stop=True),
                       reads=[Sb[d].t, QT[d].t], writes=[p.t])
            for h in range(8):
                p = pS[h // 4]
                if c < NCH - 1:
                    op("pe", lambda e: e.matmul(p.ap()[:, (h % 4) * 128:(h % 4 + 1) * 128], lhsT=Kh[d].ap()[c * HC:(c + 1) * HC, h * 128:(h + 1) * 128],
                                                rhs=vt[d].ap()[c * HC:(c + 1) * HC, h * 128:(h + 1) * 128], start=True, stop=True),
                       reads=[Kh[d].t, vt[d].t], writes=[p.t])
                else:
                    op("pe", lambda e: e.matmul(p.ap()[:, (h % 4) * 128:(h % 4 + 1) * 128], lhsT=Kh[d].ap()[:, h * 128:(h + 1) * 128],
                                                rhs=vt3[d].ap()[:, h * 128:(h + 1) * 128], start=True, stop=True),
                       reads=[Kh[d].t, vt3[d].t], writes=[p.t])
            op("dve", lambda e: e.tensor_tensor(out=Sf[d].ap(), in0=Sf[d].ap(), in1=dec[d].ap()[:, :, c:c + 1].to_broadcast([128, 8, 128]), op=ALU.mult),
               reads=[Sf[d].t, dec[d].t], writes=[Sf[d].t])
            for half in range(2):
                op("dve", lambda e: e.tensor_tensor(out=Sf[d].ap()[:, half * 4:(half + 1) * 4, :], in0=Sf[d].ap()[:, half * 4:(half + 1) * 4, :],
                                                    in1=pS[half].ap().rearrange("p (h v) -> p h v", v=128), op=ALU.add),
                   reads=[Sf[d].t, pS[half].t], writes=[Sf[d].t])
            op("act", lambda e: e.copy(out=Sb[d].ap(), in_=Sf[d].ap()), reads=[Sf[d].t], writes=[Sb[d].t])

        def post():
            if not need_o:
                return
            lt = tile
            second = (d == 0 and lt >= 8) or (d == 1 and lt < 8)
            for half in range(2):
                op("dve", lambda e: e.tensor_tensor(out=ot.ap()[:, half * 4:(half + 1) * 4, :], in0=ot.ap()[:, half * 4:(half + 1) * 4, :],
                                                    in1=pO[half].ap().rearrange("p (h t) -> p h t", t=128), op=ALU.add),
                   reads=[ot.t, pO[half].t], writes=[ot.t])
            if not second:
                dma("sp", oD[lt], ot.ap(), reads=[ot.t], writes=[t_oD[lt]])
                return
            dma("sp", o1.ap(), oD[lt], reads=[t_oD[lt]], writes=[o1.t])
            op("dve", lambda e: e.tensor_tensor(out=ot.ap(), in0=ot.ap(), in1=o1.ap(), op=ALU.add), reads=[ot.t, o1.t], writes=[ot.t])
            op("act", lambda e: e.activation(out=sqo.ap(), in_=ot.ap(), func=AF.Square), reads=[ot.t], writes=[sqo.t])
            pN = pO
            for half in range(2):
                op("pe", lambda e: e.matmul(pN[half].ap(), lhsT=ones_f.ap(), rhs=sqo.ap()[:, half * 4:(half + 1) * 4, :], start=True, stop=True),
                   reads=[ones_f.t, sqo.t], writes=[pN[half].t])
                op("act", lambda e: e.activation(out=sqo.ap()[:, half * 4:(half + 1) * 4, :], in_=pN[half].ap().rearrange("p (h t) -> p h t", t=128),
                                                 func=AF.Ln, scale=1.0 / 128, bias=EPS), reads=[pN[half].t], writes=[sqo.t])
            op("act", lambda e: e.activation(out=sqo.ap(), in_=sqo.ap(), func=AF.Exp, scale=-0.5), reads=[sqo.t], writes=[sqo.t])
            op("dve", lambda e: e.scalar_tensor_tensor(out=yh.ap(), in0=ot.ap(), scalar=ghgT.ap()[:, 0:1], in1=sqo.ap(), op0=ALU.mult, op1=ALU.mult),
               reads=[ot.t, sqo.t, ghgT.t], writes=[yh.t])
            dma("sp", yhD[b, :, :, lt * 128:(lt + 1) * 128], yh.ap(), reads=[yh.t], writes=[self.t_yhD])

        return [pre] + [(lambda c=c: chunk(c)) for c in corder] + [post]

    forder = [16, 17] + list(range(16))
    border = [17, 16] + list(range(15, -1, -1))
    for step in range(18):
        tf, tb = forder[step], border[step]
        prep(tf, 0, tf < 16)
        prep(tb, 1, tb < 16)
        for fa, fb in zip(rec_parts(tf, 0, tf < 16), rec_parts(tb, 1, tb < 16)):
            fa()
            fb()
    if self.dbg and b == 0:
        o = self.dout("dbg_yh", [128, 8, NLAT], BF16)
        t = T("dbg_yh")
        S.dma("sp", o, yhD[0], reads=[self.t_yhD], writes=[t])
        self.dbg_outs["yh"] = t
    self.close_scope()


def _merge_ffn_in(self, b, L):
    nc, S = self.nc, self.S
    op, dma = S.op, S.dma
    ps = self.ps
    ident_b, ones_b = self.ident_b, self.ones_b
    hT, hT_t = self.hT, self.hT_t
    win_v = L["win_d"].rearrange("(k p) c -> p k c", p=128)
    modD = self.modD
    self.open_scope()
    yT = self.sb("yTm", [128, 8, NLAT], BF16)
    self.open_scope()
    wG = self.sb("wG", [128, 8, 3072], BF16)
    for pc in range(3):
        dma("pool", wG.ap()[:, :, pc * 1024:(pc + 1) * 1024], win_v[:, :, C_HG + pc * 1024:C_HG + (pc + 1) * 1024], writes=[wG.t])
    ym = [self.sb(f"ym{i}", [128, 8, 512], BF16) for i in range(2)]
    yhs = [self.sb(f"yhs{i}", [128, 8, 512], BF16) for i in range(2)]
    sg = [self.sb(f"sg{i}", [128, 512]) for i in range(3)]
    t1 = self.sb("t1", [128, 512]); t2 = self.sb("t2", [128, 512])
    for s in range(4):
        c0 = s * 512
        dma("sp", ym[s % 2].ap(), L["ymlaD"][b, :, :, c0:c0 + 512], reads=[self.t_ymlaD], writes=[ym[s % 2].t])
        dma("sp", yhs[s % 2].ap(), L["yhD"][b, :, :, c0:c0 + 512], reads=[self.t_yhD], writes=[yhs[s % 2].t])
        for m in range(8):
            pg = [ps[(m * 3 + g) % 8] for g in range(3)]
            for g in range(3):
                for k in range(8):
                    op("pe", lambda e: e.matmul(pg[g].ap(), lhsT=wG.ap()[:, k, g * 1024 + m * 128: g * 1024 + (m + 1) * 128],
                                                rhs=hT.ap()[:, k, c0:c0 + 512], start=(k == 0), stop=(k == 7)),
                       reads=[wG.t] + hT_t[c0 // 128:(c0 + 512) // 128], writes=[pg[g].t])
                op("act", lambda e: e.activation(out=sg[g].ap(), in_=pg[g].ap(), func=AF.Sigmoid), reads=[pg[g].t], writes=[sg[g].t])
            op("dve", lambda e: e.tensor_tensor(out=t1.ap(), in0=pg[0].ap(), in1=sg[0].ap(), op=ALU.mult), reads=[pg[0].t, sg[0].t], writes=[t1.t])
            op("dve", lambda e: e.tensor_tensor(out=t1.ap(), in0=t1.ap(), in1=yhs[s % 2].ap()[:, m, :], op=ALU.mult), reads=[t1.t, yhs[s % 2].t], writes=[t1.t])
            op("dve", lambda e: e.tensor_tensor(out=t1.ap(), in0=t1.ap(), in1=sg[2].ap(), op=ALU.mult), reads=[t1.t, sg[2].t], writes=[t1.t])
            op("dve", lambda e: e.tensor_tensor(out=t2.ap(), in0=sg[1].ap(), in1=ym[s % 2].ap()[:, m, :], op=ALU.mult), reads=[sg[1].t, ym[s % 2].t], writes=[t2.t])
            op("dve", lambda e: e.tensor_tensor(out=yT.ap()[:, m, c0:c0 + 512], in0=t1.ap(), in1=t2.ap(), op=ALU.add), reads=[t1.t, t2.t], writes=[yT.t])
    self.close_scope()
    wo = self.sb("wo", [128, 8, D], BF16)
    dma("pool", wo.ap(), L["wout_d"].rearrange("(k p) c -> p k c", p=128), writes=[wo.t])
    wr = self.sb("wr", [128, 8, NEXP], BF16)
    dma("pool", wr.ap(), L["wr_d"].rearrange("(k p) c -> p k c", p=128), writes=[wr.t])
    brb = self.sb("brb", [1, NEXP], BF16)
    dma("pool", brb.ap(), L["br_d"], writes=[brb.t])
    mod2 = self.sb("mod2", [128, D]); dma("sp", mod2.ap(), modD[b, 2 * D:3 * D].partition_broadcast(128), reads=[self.t_modD], writes=[mod2.t])
    S2 = self.sb("S2", [128, D]); dma("sp", S2.ap(), modD[b, 3 * D:4 * D].partition_broadcast(128), reads=[self.t_modD], writes=[S2.t])
    G2 = self.sb("G2", [128, D]); dma("sp", G2.ap(), modD[b, 4 * D:5 * D].partition_broadcast(128), reads=[self.t_modD], writes=[G2.t])
    gf = self.sb("gf", [128, D]); dma("sp", gf.ap(), L["gffn_d"][0, :].partition_broadcast(128), writes=[gf.t])
    op("dve", lambda e: e.scalar_tensor_tensor(out=G2.ap(), in0=G2.ap(), scalar=1.0, in1=gf.ap(), op0=ALU.add, op1=ALU.mult),
       reads=[G2.t, gf.t], writes=[G2.t])
    xr = [self.sb(f"xr{i}", [128, D]) for i in range(2)]
    tm = self.sb("tm", [128, D])
    sq = self.sb("sq4", [128, D])
    st = self.sb("st4", [128, 4])
    h2 = [self.sb(f"h2_{i}", [128, D], BF16) for i in range(2)]
    h2T = self.sb("h2T", [128, 8, 128], BF16)
    lg = self.sb("lg", [128, NEXP])
    mx = self.sb("mx", [128, 8]); ix = self.sb("ix", [128, 8], U32); ixf = self.sb("ixf", [128, 8])
    oh = [self.sb(f"oh{k}", [128, NEXP]) for k in range(4)]
    msk = self.sb("msk", [128, NEXP]); mskb = self.sb("mskb", [128, NEXP], BF16)
    ex = self.sb("ex", [128, 4]); sm = self.sb("sm", [128, 2])
    pos = self.sb("pos", [128, NEXP]); tmp32 = self.sb("tmp32", [128, NEXP]); dstf = self.sb("dstf", [128, 4])
    x_d, x1D, XeD = L["x_d"], L["x1D"], L["XeD"]
    base_bc, destAll, probAll, ustr, iota_e, iota_c = self.base_bc, self.destAll, self.probAll, self.ustr, self.iota_e, self.iota_c
    for i in range(16):
        gt = b * 16 + i
        r0 = b * NLAT + i * 128
        xb, hb = xr[i % 2], h2[i % 2]
        dma("sp", xb.ap(), x_d[r0:r0 + 128, :], writes=[xb.t])
        pm = (ps[0], ps[1])
        for half in range(2):
            for k in range(8):
                op("pe", lambda e: e.matmul(pm[half].ap(), lhsT=yT.ap()[:, k, i * 128:(i + 1) * 128], rhs=wo.ap()[:, k, half * 512:(half + 1) * 512],
                                            start=(k == 0), stop=(k == 7)), reads=[yT.t, wo.t], writes=[pm[half].t])
            op("dve", lambda e: e.tensor_tensor(out=tm.ap()[:, half * 512:(half + 1) * 512], in0=pm[half].ap(), in1=mod2.ap()[:, half * 512:(half + 1) * 512], op=ALU.mult),
               reads=[pm[half].t, mod2.t], writes=[tm.t])
        op("dve", lambda e: e.tensor_tensor(out=xb.ap(), in0=xb.ap(), in1=tm.ap(), op=ALU.add), reads=[xb.t, tm.t], writes=[xb.t])
        dma("sp", x1D[r0:r0 + 128, :], xb.ap(), reads=[xb.t], writes=[self.tl_x1[gt]])
        op("act", lambda e: e.activation(out=sq.ap(), in_=xb.ap(), func=AF.Square, accum_out=st.ap()[:, 0:1]), reads=[xb.t], writes=[sq.t, st.t])
        op("act", lambda e: e.activation(out=st.ap()[:, 1:2], in_=st.ap()[:, 0:1], func=AF.Ln, scale=1.0 / D, bias=EPS), reads=[st.t], writes=[st.t])
        op("act", lambda e: e.activation(out=st.ap()[:, 2:3], in_=st.ap()[:, 1:2], func=AF.Exp, scale=-0.5), reads=[st.t], writes=[st.t])
        op("dve", lambda e: e.scalar_tensor_tensor(out=tm.ap(), in0=xb.ap(), scalar=st.ap()[:, 2:3], in1=G2.ap(), op0=ALU.mult, op1=ALU.mult),
           reads=[xb.t, st.t, G2.t], writes=[tm.t])
        op("dve", lambda e: e.tensor_tensor(out=hb.ap(), in0=tm.ap(), in1=S2.ap(), op=ALU.add), reads=[tm.t, S2.t], writes=[hb.t])
        pt = ps[2]
        ptv = pt.ap().bitcast(BF16)
        for k in range(8):
            op("pe", lambda e: e.transpose(out=ptv[:, k * 128:(k + 1) * 128], in_=hb.ap()[:, k * 128:(k + 1) * 128], identity=ident_b.ap()),
               reads=[hb.t, ident_b.t], writes=[pt.t])
        op("act", lambda e: e.copy(out=h2T.ap(), in_=ptv.rearrange("p (k t) -> p k t", t=128)), reads=[pt.t], writes=[h2T.t])
        pl = ps[3]
        for k in range(8):
            op("pe", lambda e: e.matmul(pl.ap()[:, 0:NEXP], lhsT=h2T.ap()[:, k, :], rhs=wr.ap()[:, k, :], start=(k == 0), stop=False),
               reads=[h2T.t, wr.t], writes=[pl.t])
        op("pe", lambda e: e.matmul(pl.ap()[:, 0:NEXP], lhsT=ones_b.ap()[0:1, :], rhs=brb.ap(), start=False, stop=True),
           reads=[ones_b.t, brb.t], writes=[pl.t])
        op("act", lambda e: e.copy(out=lg.ap(), in_=pl.ap()[:, 0:NEXP]), reads=[pl.t], writes=[lg.t])
        op("dve", lambda e: e.max(out=mx.ap(), in_=lg.ap()), reads=[lg.t], writes=[mx.t])
        op("dve", lambda e: e.max_index(out=ix.ap(), in_max=mx.ap(), in_values=lg.ap()), reads=[mx.t, lg.t], writes=[ix.t])
        op("dve", lambda e: e.tensor_copy(out=ixf.ap(), in_=ix.ap()), reads=[ix.t], writes=[ixf.t])
        for k in range(4):
            op("dve", lambda e: e.tensor_scalar(out=oh[k].ap(), in0=iota_e.ap(), scalar1=ixf.ap()[:, k:k + 1], scalar2=None, op0=ALU.is_equal),
               reads=[iota_e.t, ixf.t], writes=[oh[k].t])
        op("dve", lambda e: e.tensor_tensor(out=msk.ap(), in0=oh[0].ap(), in1=oh[1].ap(), op=ALU.add), reads=[oh[0].t, oh[1].t], writes=[msk.t])
        op("dve", lambda e: e.tensor_tensor(out=msk.ap(), in0=msk.ap(), in1=oh[2].ap(), op=ALU.add), reads=[msk.t, oh[2].t], writes=[msk.t])
        op("dve", lambda e: e.tensor_tensor(out=mskb.ap(), in0=msk.ap(), in1=oh[3].ap(), op=ALU.add), reads=[msk.t, oh[3].t], writes=[mskb.t])
        op("dve", lambda e: e.tensor_scalar(out=sm.ap()[:, 0:1], in0=mx.ap()[:, 0:1], scalar1=-1.0, scalar2=None, op0=ALU.mult), reads=[mx.t], writes=[sm.t])
        op("act", lambda e: e.activation(out=ex.ap(), in_=mx.ap()[:, 0:4], func=AF.Exp, bias=sm.ap()[:, 0:1], accum_out=sm.ap()[:, 1:2]),
           reads=[mx.t, sm.t], writes=[ex.t, sm.t])
        op("dve", lambda e: e.reciprocal(out=sm.ap()[:, 1:2], in_=sm.ap()[:, 1:2]), reads=[sm.t], writes=[sm.t])
        op("dve", lambda e: e.tensor_scalar(out=probAll.ap()[:, gt, :], in0=ex.ap(), scalar1=sm.ap()[:, 1:2], scalar2=None, op0=ALU.mult),
           reads=[ex.t, sm.t], writes=[probAll.t])
        pp, pc = ps[4], ps[5]
        op("pe", lambda e: e.matmul(pp.ap()[:, 0:NEXP], lhsT=ustr.ap(), rhs=mskb.ap(), start=True, stop=True), reads=[ustr.t, mskb.t], writes=[pp.t])
        op("pe", lambda e: e.matmul(pc.ap()[:, 0:NEXP], lhsT=ones_b.ap(), rhs=mskb.ap(), start=True, stop=True), reads=[ones_b.t, mskb.t], writes=[pc.t])
        op("dve", lambda e: e.tensor_tensor(out=pos.ap(), in0=pp.ap()[:, 0:NEXP], in1=base_bc.ap(), op=ALU.add), reads=[pp.t, base_bc.t], writes=[pos.t])
        op("dve", lambda e: e.tensor_tensor(out=base_bc.ap(), in0=pc.ap()[:, 0:NEXP], in1=base_bc.ap(), op=ALU.add), reads=[pc.t, base_bc.t], writes=[base_bc.t])
        op("dve", lambda e: e.tensor_tensor(out=pos.ap(), in0=pos.ap(), in1=iota_c.ap(), op=ALU.add), reads=[pos.t, iota_c.t], writes=[pos.t])
        for k in range(4):
            op("dve", lambda e: e.tensor_tensor(out=tmp32.ap(), in0=pos.ap(), in1=oh[k].ap(), op=ALU.mult), reads=[pos.t, oh[k].t], writes=[tmp32.t])
            op("dve", lambda e: e.reduce_sum(out=dstf.ap()[:, k:k + 1], in_=tmp32.ap(), axis=AX.X), reads=[tmp32.t], writes=[dstf.t])
        op("dve", lambda e: e.tensor_copy(out=destAll.ap()[:, gt, :], in_=dstf.ap()), reads=[dstf.t], writes=[destAll.t])
        op("dve", lambda e: e.tensor_copy(out=self.dstfAll.ap()[:, gt, :], in_=dstf.ap()), reads=[dstf.t], writes=[self.dstfAll.t])
        op("dve", lambda e: e.tensor_copy(out=self.eidAll.ap()[:, gt, :], in_=ixf.ap()[:, 0:4]), reads=[ixf.t], writes=[self.eidAll.t])
        for k in range(4):
            tsc = T("Xsc"); self.tl_Xe.append(tsc)
            dma("pool", None, None, reads=[hb.t, destAll.t, self.t_XeD], writes=[tsc],
                fn=lambda e: e.indirect_dma_start(out=XeD, out_offset=bass.IndirectOffsetOnAxis(ap=destAll.ap()[:, gt, k:k + 1], axis=0),
                                                  in_=hb.ap(), in_offset=None))
    if self.dbg and b == 0:
        o = self.dout("dbg_x1", [NLAT, D]); t = T("dbg_x1")
        S.dma("sp", o, x1D[0:NLAT, :], reads=self.tl_x1[0:16], writes=[t]); self.dbg_outs["x1"] = t
        self.dump("yTm", yT, [128, 8, NLAT], BF16)
        self.dump("destAll", destAll, [128, 32, 4], I32)
        self.dump("probAll", probAll, [128, 32, 4])
        self.dump("base_bc", base_bc, [128, NEXP])
        self.dump("lg", lg, [128, NEXP])
        self.dump("h2last", h2[1], [128, D], BF16)
    self.close_scope()


def _moe(self, L):
    nc, S = self.nc, self.S
    op, dma = S.op, S.dma
    ps = self.ps
    ident_b, ones_b = self.ident_b, self.ones_b
    XeD, YeD, x1D, modD = L["XeD"], L["YeD"], L["x1D"], self.modD
    cnt, iota_e = self.base_bc, self.iota_e
    eI = self.sb("eI", [128, NSL], I32)
    rI = self.sb("rI", [128, NSL], I32)
    wI = self.sb("wI", [128, NSL], I32)
    xI = self.sb("xI", [128, NSL, NB], I32)
    ydest = self.sb("ydest", [128, 32, 4], I32)
    self.open_scope()
    jota = self.sb("jota", [128, NSL]); dma("sp", jota.ap(), L["jota_d"], writes=[jota.t])
    nsl = self.sb("nsl", [128, NEXP]); tmp = self.sb("tmpe", [128, NEXP])
    ca = self.sb("ca", [128, NEXP]); cb = self.sb("cb", [128, NEXP]); cstart = self.sb("cstart", [128, NEXP]); adj = self.sb("adj", [128, NEXP])
    op("dve", lambda e: e.tensor_scalar(out=nsl.ap(), in0=cnt.ap(), scalar1=0.0, scalar2=None, op0=ALU.is_gt), reads=[cnt.t], writes=[nsl.t])
    for m in range(1, CAP // SLAB):
        op("dve", lambda e: e.tensor_scalar(out=tmp.ap(), in0=cnt.ap(), scalar1=float(m * SLAB), scalar2=None, op0=ALU.is_gt), reads=[cnt.t], writes=[tmp.t])
        op("dve", lambda e: e.tensor_tensor(out=nsl.ap(), in0=nsl.ap(), in1=tmp.ap(), op=ALU.add), reads=[nsl.t, tmp.t], writes=[nsl.t])
    op("dve", lambda e: e.tensor_copy(out=ca.ap(), in_=nsl.ap()), reads=[nsl.t], writes=[ca.t])
    src, dst = ca, cb
    sh = 1
    while sh < NEXP:
        op("dve", lambda e: e.tensor_copy(out=dst.ap()[:, 0:sh], in_=src.ap()[:, 0:sh]), reads=[src.t], writes=[dst.t])
        op("dve", lambda e: e.tensor_tensor(out=dst.ap()[:, sh:], in0=src.ap()[:, sh:], in1=src.ap()[:, 0:NEXP - sh], op=ALU.add), reads=[src.t], writes=[dst.t])
        src, dst = dst, src
        sh *= 2
    cend = src
    op("dve", lambda e: e.tensor_tensor(out=cstart.ap(), in0=cend.ap(), in1=nsl.ap(), op=ALU.subtract), reads=[cend.t, nsl.t], writes=[cstart.t])
    big = self.sb("big3", [128, 128, NEXP])
    ej = self.sb("ej", [128, NSL]); csj = self.sb("csj", [128, NSL]); vj = self.sb("vj", [128, NSL]); rj = self.sb("rj", [128, NSL])
    b3 = big.ap()[:, 0:NSL, :]
    op("dve", lambda e: e.tensor_tensor(out=b3, in0=cend.ap().unsqueeze(1).to_broadcast([128, NSL, NEXP]),
                                        in1=jota.ap().unsqueeze(2).to_broadcast([128, NSL, NEXP]), op=ALU.is_le), reads=[cend.t, jota.t], writes=[big.t])
    op("dve", lambda e: e.reduce_sum(out=ej.ap(), in_=b3, axis=AX.X), reads=[big.t], writes=[ej.t])
    op("dve", lambda e: e.tensor_scalar_min(out=ej.ap(), in0=ej.ap(), scalar1=float(NEXP - 1)), reads=[ej.t], writes=[ej.t])
    op("dve", lambda e: e.tensor_tensor(out=b3, in0=iota_e.ap().unsqueeze(1).to_broadcast([128, NSL, NEXP]),
                                        in1=ej.ap().unsqueeze(2).to_broadcast([128, NSL, NEXP]), op=ALU.is_equal), reads=[iota_e.t, ej.t], writes=[big.t])
    op("dve", lambda e: e.tensor_tensor(out=b3, in0=b3, in1=cstart.ap().unsqueeze(1).to_broadcast([128, NSL, NEXP]), op=ALU.mult), reads=[big.t, cstart.t], writes=[big.t])
    op("dve", lambda e: e.reduce_sum(out=csj.ap(), in_=b3, axis=AX.X), reads=[big.t], writes=[csj.t])
    op("dve", lambda e: e.tensor_scalar(out=vj.ap(), in0=jota.ap(), scalar1=cend.ap()[:, NEXP - 1:NEXP], scalar2=None, op0=ALU.is_lt), reads=[jota.t, cend.t], writes=[vj.t])
    op("dve", lambda e: e.tensor_tensor(out=rj.ap(), in0=jota.ap(), in1=csj.ap(), op=ALU.subtract), reads=[jota.t, csj.t], writes=[rj.t])
    op("dve", lambda e: e.tensor_scalar(out=rj.ap(), in0=rj.ap(), scalar1=float(SLAB), scalar2=None, op0=ALU.mult), reads=[rj.t], writes=[rj.t])
    cntj = self.sb("cntj", [128, NSL]); off4 = self.sb("off4", [128, NB]); dma("sp", off4.ap(), L["off4_d"], writes=[off4.t])
    op("dve", lambda e: e.tensor_tensor(out=b3, in0=iota_e.ap().unsqueeze(1).to_broadcast([128, NSL, NEXP]),
                                        in1=ej.ap().unsqueeze(2).to_broadcast([128, NSL, NEXP]), op=ALU.is_equal), reads=[iota_e.t, ej.t], writes=[big.t])
    op("dve", lambda e: e.tensor_tensor(out=b3, in0=b3, in1=cnt.ap().unsqueeze(1).to_broadcast([128, NSL, NEXP]), op=ALU.mult), reads=[big.t, cnt.t], writes=[big.t])
    op("dve", lambda e: e.reduce_sum(out=cntj.ap(), in_=b3, axis=AX.X), reads=[big.t], writes=[cntj.t])
    op("dve", lambda e: e.tensor_tensor(out=cntj.ap(), in0=cntj.ap(), in1=vj.ap(), op=ALU.mult), reads=[cntj.t, vj.t], writes=[cntj.t])
    op("dve", lambda e: e.tensor_tensor(out=ej.ap(), in0=ej.ap(), in1=vj.ap(), op=ALU.mult), reads=[ej.t, vj.t], writes=[ej.t])
    posb = self.sb("posb", [128, NSL, NB]); val4 = self.sb("val4", [128, NSL, NB])
    op("dve", lambda e: e.tensor_tensor(out=posb.ap(), in0=rj.ap().unsqueeze(2).to_broadcast([128, NSL, NB]),
                                        in1=off4.ap().unsqueeze(1).to_broadcast([128, NSL, NB]), op=ALU.add), reads=[rj.t, off4.t], writes=[posb.t])
    op("dve", lambda e: e.tensor_tensor(out=val4.ap(), in0=posb.ap(), in1=cntj.ap().unsqueeze(2).to_broadcast([128, NSL, NB]), op=ALU.is_lt),
       reads=[posb.t, cntj.t], writes=[val4.t])
    op("dve", lambda e: e.tensor_tensor(out=posb.ap(), in0=posb.ap(), in1=val4.ap(), op=ALU.mult), reads=[posb.t, val4.t], writes=[posb.t])
    op("dve", lambda e: e.tensor_scalar(out=rj.ap(), in0=ej.ap(), scalar1=float(CAP), scalar2=None, op0=ALU.mult), reads=[ej.t], writes=[rj.t])
    op("dve", lambda e: e.tensor_tensor(out=posb.ap(), in0=posb.ap(), in1=rj.ap().unsqueeze(2).to_broadcast([128, NSL, NB]), op=ALU.add), reads=[posb.t, rj.t], writes=[posb.t])
    op("dve", lambda e: e.tensor_copy(out=xI.ap(), in_=posb.ap()), reads=[posb.t], writes=[xI.t])
    pidx = self.sb("pidx", [128, NSL]); dma("sp", pidx.ap(), L["pidx_d"], writes=[pidx.t])
    OOB = 1.0e6
    ld = self.sb("ld", [128, NSL]); tmpj = self.sb("tmpj", [128, NSL])
    op("dve", lambda e: e.memset(ld.ap(), 1.0), writes=[ld.t])
    op("dve", lambda e: e.tensor_tensor(out=ld.ap()[:, 2:], in0=ej.ap()[:, 2:], in1=ej.ap()[:, 0:NSL - 2], op=ALU.not_equal), reads=[ej.t], writes=[ld.t])
    op("dve", lambda e: e.tensor_scalar(out=tmpj.ap(), in0=ej.ap(), scalar1=-OOB, scalar2=None, op0=ALU.add), reads=[ej.t], writes=[tmpj.t])
    op("dve", lambda e: e.tensor_tensor(out=tmpj.ap(), in0=tmpj.ap(), in1=ld.ap(), op=ALU.mult), reads=[tmpj.t, ld.t], writes=[tmpj.t])
    op("dve", lambda e: e.tensor_scalar(out=tmpj.ap(), in0=tmpj.ap(), scalar1=OOB, scalar2=None, op0=ALU.add), reads=[tmpj.t], writes=[tmpj.t])
    op("dve", lambda e: e.tensor_copy(out=eI.ap(), in_=tmpj.ap()), reads=[tmpj.t], writes=[eI.t])
    op("dve", lambda e: e.scalar_tensor_tensor(out=ej.ap(), in0=ej.ap(), scalar=128.0, in1=pidx.ap(), op0=ALU.mult, op1=ALU.add), reads=[ej.t, pidx.t], writes=[ej.t])
    op("dve", lambda e: e.tensor_scalar(out=tmpj.ap(), in0=ej.ap(), scalar1=-OOB, scalar2=None, op0=ALU.add), reads=[ej.t], writes=[tmpj.t])
    op("dve", lambda e: e.tensor_tensor(out=tmpj.ap(), in0=tmpj.ap(), in1=ld.ap(), op=ALU.mult), reads=[tmpj.t, ld.t], writes=[tmpj.t])
    op("dve", lambda e: e.tensor_scalar(out=tmpj.ap(), in0=tmpj.ap(), scalar1=OOB, scalar2=None, op0=ALU.add), reads=[tmpj.t], writes=[tmpj.t])
    op("dve", lambda e: e.tensor_copy(out=wI.ap(), in_=tmpj.ap()), reads=[tmpj.t], writes=[wI.t])
    op("dve", lambda e: e.tensor_scalar(out=adj.ap(), in0=cstart.ap(), scalar1=float(SLAB), scalar2=None, op0=ALU.mult), reads=[cstart.t], writes=[adj.t])
    op("dve", lambda e: e.tensor_tensor(out=adj.ap(), in0=adj.ap(), in1=self.iota_c.ap(), op=ALU.subtract), reads=[adj.t, self.iota_c.t], writes=[adj.t])
    eid2 = self.eidAll.ap().rearrange("p g k -> p (g k)")
    op("dve", lambda e: e.tensor_tensor(out=big.ap(), in0=iota_e.ap().unsqueeze(1).to_broadcast([128, 128, NEXP]),
                                        in1=eid2.unsqueeze(2).to_broadcast([128, 128, NEXP]), op=ALU.is_equal), reads=[iota_e.t, self.eidAll.t], writes=[big.t])
    op("dve", lambda e: e.tensor_tensor(out=big.ap(), in0=big.ap(), in1=adj.ap().unsqueeze(1).to_broadcast([128, 128, NEXP]), op=ALU.mult), reads=[big.t, adj.t], writes=[big.t])
    adjp = self.sb("adjp", [128, 128])
    op("dve", lambda e: e.reduce_sum(out=adjp.ap(), in_=big.ap(), axis=AX.X), reads=[big.t], writes=[adjp.t])
    op("dve", lambda e: e.tensor_tensor(out=adjp.ap(), in0=adjp.ap(), in1=self.dstfAll.ap().rearrange("p g k -> p (g k)"), op=ALU.add), reads=[adjp.t, self.dstfAll.t], writes=[adjp.t])
    op("dve", lambda e: e.tensor_copy(out=ydest.ap().rearrange("p g k -> p (g k)"), in_=adjp.ap()), reads=[adjp.t], writes=[ydest.t])
    if self.dbg:
        self.dump("eI", eI, [128, NSL], I32); self.dump("xI", xI, [128, NSL, NB], I32); self.dump("ydest", ydest, [128, 32, 4], I32)
    self.close_scope()
    self.ydest = ydest
    self.open_scope()
    wgu = [self.sb(f"wgu{i}", [128, 8, 2 * D], BF16) for i in range(2)]
    wd = [self.sb(f"wd{i}", [128, 8, D], BF16) for i in range(2)]
    bgu = [self.sb(f"bgu{i}", [128, 2, 8]) for i in range(2)]
    xblk = [self.sb(f"xblk{i}", [128, NB, D], BF16) for i in range(2)]
    xT = self.sb("xTe", [128, 8, SLAB], BF16)
    aT = self.sb("aTe", [128, 8, SLAB], BF16)
    xg = self.sb("xg", [128, SLAB]); sgm = self.sb("sgm", [128, SLAB]); xl = self.sb("xl", [128, SLAB])
    yo = [self.sb(f"yo{i}", [128, D]) for i in range(2)]
    Yv = YeD.rearrange("(n p) d -> n p d", p=128)
    bdf = [self.sb(f"bdf{i}", [128, D]) for i in range(2)]
    Xg = XeD.rearrange("(r j) d -> r (j d)", j=4)
    Wg = L["wgu_d"].rearrange("e (p k) c -> (e p) (k c)", k=8)
    Wd = L["wd_d"].rearrange("e (p k) c -> (e p) (k c)", k=8)
    Bg = L["bguT_d"].rearrange("e p t c -> (e p) (t c)")
    IOA = bass.IndirectOffsetOnAxis
    bregs = {}

    def gather(dst_ap, src, idx_ap, reads, writes):
        bound = src.shape[0] - 1
        if bound not in bregs:
            bregs[bound] = nc.gpsimd.alloc_register(f"bound{bound}")
            nc.gpsimd.reg_mov(bregs[bound], bound)
        dma("pool", None, None, reads=reads, writes=writes,
            fn=lambda e: e.indirect_dma_start(out=dst_ap, out_offset=None, in_=src, in_offset=IOA(ap=idx_ap, axis=0),
                                              bounds_check=bregs[bound], oob_is_err=False))

    for j in range(NSL):
        wg, wdn, bd, bg, xb = wgu[j % 2], wd[j % 2], bdf[j % 2], bgu[j % 2], xblk[j % 2]
        for jb in range(NB):
            gather(xb.ap()[:, jb, :], XeD, xI.ap()[:, j, jb:jb + 1], [self.t_XeD, xI.t] + self.tl_Xe, [xb.t])
        gather(wg.ap().rearrange("p k c -> p (k c)"), Wg, wI.ap()[:, j:j + 1], [wI.t], [wg.t])
        gather(wdn.ap().rearrange("p k c -> p (k c)"), Wd, wI.ap()[:, j:j + 1], [wI.t], [wdn.t])
        gather(bg.ap().rearrange("p t c -> p (t c)"), Bg, wI.ap()[:, j:j + 1], [wI.t], [bg.t])
        gather(bd.ap(), L["bd_d"], eI.ap()[:, j:j + 1], [eI.t], [bd.t])
        for jb in range(NB):
            pt = ps[jb % 2]
            ptv = pt.ap().bitcast(BF16)
            for k in range(8):
                op("pe", lambda e: e.transpose(out=ptv[:, k * 128:(k + 1) * 128], in_=xb.ap()[:, jb, k:D:8], identity=ident_b.ap()),
                   reads=[xb.t, ident_b.t], writes=[pt.t])
            if jb % 2 == 0:
                op("act", lambda e: e.copy(out=xT.ap()[:, :, jb * 128:(jb + 1) * 128], in_=ptv.rearrange("p (k t) -> p k t", t=128)), reads=[pt.t], writes=[xT.t])
            else:
                op("dve", lambda e: e.tensor_copy(out=xT.ap()[:, :, jb * 128:(jb + 1) * 128], in_=ptv.rearrange("p (k t) -> p k t", t=128)), reads=[pt.t], writes=[xT.t])
        for c in range(8):
            pg, pl = ps[2 + (c % 2) * 2], ps[3 + (c % 2) * 2]
            for k in range(8):
                op("pe", lambda e: e.matmul(pg.ap()[:, 0:SLAB], lhsT=wg.ap()[:, k, 2 * c:2 * D:16], rhs=xT.ap()[:, k, :], start=(k == 0), stop=(k == 7)),
                   reads=[wg.t, xT.t], writes=[pg.t])
            for k in range(8):
                op("pe", lambda e: e.matmul(pl.ap()[:, 0:SLAB], lhsT=wg.ap()[:, k, 2 * c + 1:2 * D:16], rhs=xT.ap()[:, k, :], start=(k == 0), stop=(k == 7)),
                   reads=[wg.t, xT.t], writes=[pl.t])
            op("dve", lambda e: e.tensor_scalar(out=xg.ap(), in0=pg.ap()[:, 0:SLAB], scalar1=bg.ap()[:, 0, c:c + 1], scalar2=7.0, op0=ALU.add, op1=ALU.min),
               reads=[pg.t, bg.t], writes=[xg.t])
            op("act", lambda e: e.activation(out=sgm.ap(), in_=xg.ap(), func=AF.Sigmoid, scale=1.702), reads=[xg.t], writes=[sgm.t])
            op("dve", lambda e: e.tensor_scalar(out=xl.ap(), in0=pl.ap()[:, 0:SLAB], scalar1=bg.ap()[:, 1, c:c + 1], scalar2=7.0, op0=ALU.add, op1=ALU.min),
               reads=[pl.t, bg.t], writes=[xl.t])
            op("dve", lambda e: e.tensor_scalar(out=xl.ap(), in0=xl.ap(), scalar1=-7.0, scalar2=1.0, op0=ALU.max, op1=ALU.add), reads=[xl.t], writes=[xl.t])
            op("dve", lambda e: e.tensor_tensor(out=xg.ap(), in0=xg.ap(), in1=sgm.ap(), op=ALU.mult), reads=[xg.t, sgm.t], writes=[xg.t])
            op("dve", lambda e: e.tensor_tensor(out=aT.ap()[:, c, :], in0=xg.ap(), in1=xl.ap(), op=ALU.mult), reads=[xg.t, xl.t], writes=[aT.t])
        for jb in range(NB):
            y = yo[jb % 2]
            py = (ps[6], ps[7])
            for half in range(2):
                for k in range(8):
                    op("pe", lambda e: e.matmul(py[half].ap(), lhsT=aT.ap()[:, k, jb * 128:(jb + 1) * 128], rhs=wdn.ap()[:, k, half * 512:(half + 1) * 512],
                                                start=(k == 0), stop=(k == 7)), reads=[aT.t, wdn.t], writes=[py[half].t])
                op("dve", lambda e: e.tensor_tensor(out=y.ap()[:, half * 512:(half + 1) * 512], in0=py[half].ap(), in1=bd.ap()[:, half * 512:(half + 1) * 512], op=ALU.add),
                   reads=[py[half].t, bd.t], writes=[y.t])
            tys = T("Yst"); self.tl_Ye.append(tys)
            dma("sp", Yv[j * NB + jb], y.ap(), reads=[y.t], writes=[tys])
    self.close_scope()
    if self.dbg:
        for nm, src, n, tt, dt in (("Xe0", XeD, 1024, self.tl_Xe, BF16), ("Ye0", YeD, 1024, self.tl_Ye, F32), ("x1all", x1D, BL * NLAT, self.tl_x1, F32)):
            o = self.dout("dbg_" + nm, [n, D], dt); t = T("dbg_" + nm)
            S.dma("sp", o, src[0:n, :], reads=tt, writes=[t]); self.dbg_outs[nm] = t
        self.dump("destAll2", self.destAll, [128, 32, 4], I32)
        self.dump("probAll2", self.probAll, [128, 32, 4])
    self.open_scope()
    gfin = self.sb("gfin", [128, D]); dma("sp", gfin.ap(), L["gfin_d"][0, :].partition_broadcast(128), writes=[gfin.t])
    mod5 = self.sb("mod5", [128, D])
    x1 = [self.sb(f"x1_{i}", [128, D]) for i in range(2)]
    yk = [self.sb(f"yk{i}", [128, D]) for i in range(4)]
    acc = self.sb("acc", [128, D]); sq = self.sb("sqF", [128, D]); st = self.sb("stF", [128, 4])
    ob = [self.sb(f"ob{i}", [128, D]) for i in range(2)]
    destAll, probAll = self.destAll, self.probAll
    out_d = L["out_d"]
    for gt in range(32):
        b, i = gt // 16, gt % 16
        if i == 0:
            dma("sp", mod5.ap(), modD[b, 5 * D:6 * D].partition_broadcast(128), reads=[self.t_modD], writes=[mod5.t])
        r0 = gt * 128
        xb, o = x1[gt % 2], ob[gt % 2]
        dma("sp", xb.ap(), x1D[r0:r0 + 128, :], reads=[self.tl_x1[gt]], writes=[xb.t])
        for k in range(4):
            dma("pool", None, None, reads=self.tl_Ye + [self.ydest.t], writes=[yk[k].t],
                fn=lambda e: e.indirect_dma_start(out=yk[k].ap(), out_offset=None, in_=YeD,
                                                  in_offset=bass.IndirectOffsetOnAxis(ap=self.ydest.ap()[:, gt, k:k + 1], axis=0)))
        op("dve", lambda e: e.tensor_scalar(out=acc.ap(), in0=yk[0].ap(), scalar1=probAll.ap()[:, gt, 0:1], scalar2=None, op0=ALU.mult),
           reads=[yk[0].t, probAll.t], writes=[acc.t])
        for k in range(1, 4):
            op("dve", lambda e: e.scalar_tensor_tensor(out=acc.ap(), in0=yk[k].ap(), scalar=probAll.ap()[:, gt, k:k + 1], in1=acc.ap(), op0=ALU.mult, op1=ALU.add),
               reads=[yk[k].t, probAll.t, acc.t], writes=[acc.t])
        op("dve", lambda e: e.tensor_tensor(out=acc.ap(), in0=acc.ap(), in1=mod5.ap(), op=ALU.mult), reads=[acc.t, mod5.t], writes=[acc.t])
        op("dve", lambda e: e.tensor_tensor(out=xb.ap(), in0=xb.ap(), in1=acc.ap(), op=ALU.add), reads=[acc.t, xb.t], writes=[xb.t])
        op("act", lambda e: e.activation(out=sq.ap(), in_=xb.ap(), func=AF.Square, accum_out=st.ap()[:, 0:1]), reads=[xb.t], writes=[sq.t, st.t])
        op("act", lambda e: e.activation(out=st.ap()[:, 1:2], in_=st.ap()[:, 0:1], func=AF.Ln, scale=1.0 / D, bias=EPS), reads=[st.t], writes=[st.t])
        op("act", lambda e: e.activation(out=st.ap()[:, 2:3], in_=st.ap()[:, 1:2], func=AF.Exp, scale=-0.5), reads=[st.t], writes=[st.t])
        op("dve", lambda e: e.scalar_tensor_tensor(out=o.ap(), in0=xb.ap(), scalar=st.ap()[:, 2:3], in1=gfin.ap(), op0=ALU.mult, op1=ALU.mult),
           reads=[xb.t, st.t, gfin.t], writes=[o.t])
        tos = T("outst"); self.tl_out.append(tos)
        dma("sp", out_d[r0:r0 + 128, :], o.ap(), reads=[o.t], writes=[tos])


Prog.batch = _batch
Prog.hgrn = _hgrn
Prog.merge_ffn_in = _merge_ffn_in
Prog.moe = _moe
```
